# Optimizing a Trainium2 kernel written in Bass

```python
import math
import jax, jax.numpy as jnp
from jax import lax
import numpy as np

D_MODEL = 1024
BATCH = 16
SEQ = 4096
DEPTH = 2

A_PAIRS = ((128, 1), (512, 4), (2048, 16))
A_GROUPS = len(A_PAIRS)
A_HEADS = 4
A_HEAD_DIM = 128
A_GROUP_WIDTH = A_HEADS * A_HEAD_DIM
A_QKV_WIDTH = A_GROUPS * A_GROUP_WIDTH
A_OUT = A_GROUP_WIDTH
A_BLOCK = 128
ROPE_THETA = 500000.0
ROT_DIM = A_HEAD_DIM // 4

B_HEAD_DIM = 128
B_HEADS = D_MODEL // B_HEAD_DIM
B_WIDTH = B_HEADS * B_HEAD_DIM
CONV_K = 4
CHUNK = 64

IN_WIDTHS = (A_QKV_WIDTH, A_QKV_WIDTH, A_QKV_WIDTH,
             3 * B_WIDTH,
             B_WIDTH,
             B_HEADS,
             B_HEADS,
             D_MODEL, D_MODEL)
N_IN = int(sum(IN_WIDTHS))
IN_SPLITS = [int(s) for s in np.cumsum(IN_WIDTHS)[:-1]]

D_FF = 2816
N_EXPERTS = 8
TOP_K = 2
D_EXPERT = 3584
N_DENSE = (DEPTH + 1) // 2
N_MOE = DEPTH // 2

DN_ALPHA = (2 * DEPTH) ** 0.25
DN_BETA = (8 * DEPTH) ** -0.25
LN_EPS = 1e-5
RMS_EPS = 1e-6

kernel_name = "hybrid_dilated_attn_gated_deltanet_moe_deepnorm"


def layer_norm(x, g, b):
    xf = x.astype(jnp.float32)
    mu = xf.mean(-1, keepdims=True)
    var = jnp.square(xf - mu).mean(-1, keepdims=True)
    return ((xf - mu) * lax.rsqrt(var + LN_EPS) * g.astype(jnp.float32) + b.astype(jnp.float32)).astype(x.dtype)


def rotary_tables(positions):
    inv_freq = ROPE_THETA ** (-jnp.arange(0, ROT_DIM, 2, dtype=jnp.float32) / ROT_DIM)
    ang = positions.astype(jnp.float32)[..., None] * inv_freq
    return jnp.cos(ang), jnp.sin(ang)


def apply_partial_rotary(t, cos, sin):
    tr, tp = t[..., :ROT_DIM].astype(jnp.float32), t[..., ROT_DIM:]
    t1, t2 = tr[..., :ROT_DIM // 2], tr[..., ROT_DIM // 2:]
    c, s = cos[:, :, None, None, :], sin[:, :, None, None, :]
    rot = jnp.concatenate([t1 * c - t2 * s, t2 * c + t1 * s], axis=-1).astype(t.dtype)
    return jnp.concatenate([rot, tp], axis=-1)


def dilated_window_attention(q, k, v, window, dilation):
    bsz, seq, nh, dh = q.shape
    span = window // dilation
    sub_len = seq // dilation
    blk = min(A_BLOCK, sub_len)
    n_blk = -(-sub_len // blk)
    pad_len = n_blk * blk

    def to_sub(t):
        return t.reshape(bsz, sub_len, dilation, nh, dh).transpose(0, 2, 3, 1, 4)

    qs = jnp.pad(to_sub(q), ((0, 0),) * 3 + ((0, pad_len - sub_len), (0, 0)))
    kv_pad = ((0, 0),) * 3 + ((span, pad_len - sub_len), (0, 0))
    ks = jnp.pad(to_sub(k), kv_pad)
    vs = jnp.pad(to_sub(v), kv_pad)
    qb = qs.reshape(bsz, dilation, nh, n_blk, blk, dh)
    key_idx = np.arange(n_blk)[:, None] * blk + np.arange(blk + span)[None, :]
    kb = jnp.take(ks, key_idx, axis=3)
    vb = jnp.take(vs, key_idx, axis=3)
    qpos = np.arange(n_blk)[:, None] * blk + np.arange(blk)[None, :]
    kpos = key_idx - span
    dist = qpos[:, :, None] - kpos[:, None, :]
    mask = (dist >= 0) & (dist <= span) & (kpos[:, None, :] >= 0)

    scores = jnp.einsum('brhnqe,brhnke->brhnqk', qb, kb,
                        preferred_element_type=jnp.float32) / math.sqrt(dh)
    scores = jnp.where(mask, scores, -jnp.inf)
    m = scores.max(-1, keepdims=True)
    p = jnp.exp(scores - m)
    den = p.sum(-1, keepdims=True)
    o = jnp.einsum('brhnqk,brhnke->brhnqe', p, vb.astype(jnp.float32)) / den
    lse = (m + jnp.log(den))[..., 0]
    o = o.reshape(bsz, dilation, nh, pad_len, dh)[:, :, :, :sub_len]
    lse = lse.reshape(bsz, dilation, nh, pad_len)[..., :sub_len]
    o = o.transpose(0, 3, 1, 2, 4).reshape(bsz, seq, nh, dh)
    lse = lse.transpose(0, 3, 1, 2).reshape(bsz, seq, nh)
    return o, lse


def mixer_dilated(q, k, v, cos, sin):
    bsz, seq = q.shape[:2]
    q = apply_partial_rotary(q, cos, sin)
    k = apply_partial_rotary(k, cos, sin)
    outs, lses = [], []
    for g, (window, dilation) in enumerate(A_PAIRS):
        o, l = dilated_window_attention(q[:, :, g], k[:, :, g], v[:, :, g], window, dilation)
        outs.append(o)
        lses.append(l)
    o = jnp.stack(outs, axis=2)
    w = jax.nn.softmax(jnp.stack(lses, axis=2), axis=2)
    y = jnp.sum(o * w[..., None], axis=2)
    return y.reshape(bsz, seq, A_OUT)


def short_conv(t, w):
    y = lax.conv_general_dilated(t, w[:, None, :].astype(t.dtype), window_strides=(1,),
                                 padding=((CONV_K - 1, 0),),
                                 dimension_numbers=('NWC', 'WIO', 'NWC'),
                                 feature_group_count=t.shape[-1])
    return jax.nn.silu(y)


def l2_normalize(t):
    tf = t.astype(jnp.float32)
    return tf * lax.rsqrt(jnp.sum(tf * tf, -1, keepdims=True) + RMS_EPS)


def gated_delta_rule(q, k, v, g, beta):
    bsz, seq, nh, dk = q.shape
    dv = v.shape[-1]
    nc = seq // CHUNK

    def chunks(t):
        t = jnp.moveaxis(t.astype(jnp.float32), 2, 1)
        return t.reshape(bsz, nh, nc, CHUNK, *t.shape[3:])

    q, k, v, g, beta = chunks(q), chunks(k), chunks(v), chunks(g), chunks(beta)
    g = jnp.cumsum(g, axis=-1)
    strict = np.tril(np.ones((CHUNK, CHUNK), bool), -1)
    incl = np.tril(np.ones((CHUNK, CHUNK), bool), 0)
    decay = jnp.exp(jnp.where(incl, g[..., :, None] - g[..., None, :], -jnp.inf))
    k_beta = k * beta[..., None]
    v_beta = v * beta[..., None]
    lower = jnp.where(strict, jnp.einsum('bhnie,bhnje->bhnij', k_beta, k) * decay, 0.0)
    eye = jnp.eye(CHUNK, dtype=jnp.float32)
    t_inv = lax.linalg.triangular_solve(eye + lower, jnp.broadcast_to(eye, lower.shape),
                                        left_side=True, lower=True, unit_diagonal=True)
    u = t_inv @ v_beta
    w = t_inv @ (k_beta * jnp.exp(g)[..., None])
    attn_intra = jnp.where(incl, jnp.einsum('bhnie,bhnje->bhnij', q, k) * decay, 0.0)
    q_dec = q * jnp.exp(g)[..., None]
    g_last = g[..., -1]
    k_tail = k * jnp.exp(g_last[..., None] - g)[..., None]
    state_decay = jnp.exp(g_last)[..., None, None]

    def step(state, inp):
        q_c, k_c, u_c, w_c, a_c, sd_c = inp
        v_new = u_c - w_c @ state
        o_c = q_c @ state + a_c @ v_new
        state = state * sd_c + jnp.einsum('bhce,bhcv->bhev', k_c, v_new)
        return state, o_c

    xs = tuple(jnp.moveaxis(t, 2, 0) for t in (q_dec, k_tail, u, w, attn_intra, state_decay))
    state0 = jnp.zeros((bsz, nh, dk, dv), jnp.float32)
    _, o = lax.scan(step, state0, xs)
    o = jnp.moveaxis(o, 0, 2).reshape(bsz, nh, seq, dv)
    return jnp.transpose(o, (0, 2, 1, 3))


def mixer_gated_deltanet(qkv, z, b_logit, a_logit, conv_w, a_log, dt_bias, norm_w):
    bsz, seq = qkv.shape[:2]
    qkv = short_conv(qkv, conv_w)
    q, k, v = jnp.split(qkv, 3, axis=-1)
    shp = (bsz, seq, B_HEADS, B_HEAD_DIM)
    q = l2_normalize(q.reshape(shp)) * (B_HEAD_DIM ** -0.5)
    k = l2_normalize(k.reshape(shp))
    v = v.reshape(shp)
    beta = jax.nn.sigmoid(b_logit.astype(jnp.float32))
    g = -jnp.exp(a_log.astype(jnp.float32)) * jax.nn.softplus(
        a_logit.astype(jnp.float32) + dt_bias.astype(jnp.float32))
    o = gated_delta_rule(q, k, v, g, beta)
    o = o * lax.rsqrt(jnp.mean(o * o, -1, keepdims=True) + RMS_EPS) * norm_w.astype(jnp.float32)
    o = o * jax.nn.silu(z.reshape(shp).astype(jnp.float32))
    return o.reshape(bsz, seq, B_WIDTH)


def swiglu(x, w_gate, w_up, w_down):
    return (jax.nn.silu(x @ w_gate) * (x @ w_up)) @ w_down


def moe_swiglu(x, router_w, w_gate, w_up, w_down):
    logits = (x @ router_w).astype(jnp.float32)
    top_val, top_idx = lax.top_k(logits, TOP_K)
    top_w = jax.nn.softmax(top_val, axis=-1)
    gates = jnp.sum(jax.nn.one_hot(top_idx, N_EXPERTS, dtype=jnp.float32) * top_w[..., None], axis=-2)
    gates = gates.astype(x.dtype)
    y = jnp.zeros_like(x)
    for e in range(N_EXPERTS):
        y = y + gates[..., e:e + 1] * swiglu(x, w_gate[e], w_up[e], w_down[e])
    return y


def setup_inputs(seed: int = 0) -> dict:
    key = jax.random.key(seed)
    ks = jax.random.split(key, 24)

    def nrm(k, shape, scale):
        return jax.random.normal(k, shape, jnp.float32) * scale

    x = jax.random.normal(ks[0], (BATCH, SEQ, D_MODEL), jnp.float32)
    positions = (jnp.arange(SEQ, dtype=jnp.int32)[None, :]
                 + jax.random.randint(ks[1], (BATCH, 1), 0, 1024, dtype=jnp.int32))
    w_in = nrm(ks[2], (DEPTH, D_MODEL, N_IN), D_MODEL ** -0.5)
    conv_w = nrm(ks[3], (DEPTH, CONV_K, 3 * B_WIDTH), CONV_K ** -0.5)
    a_log = jnp.log(jax.random.uniform(ks[4], (DEPTH, B_HEADS), jnp.float32, 1.0, 16.0))
    dt = jnp.exp(jax.random.uniform(ks[5], (DEPTH, B_HEADS), jnp.float32,
                                    math.log(1e-3), math.log(1e-1)))
    dt_bias = dt + jnp.log(-jnp.expm1(-dt))
    dn_norm_w = 1.0 + nrm(ks[6], (DEPTH, B_HEAD_DIM), 0.02)
    w_branch_a = nrm(ks[7], (DEPTH, A_OUT, D_MODEL), A_OUT ** -0.5)
    w_branch_b = nrm(ks[8], (DEPTH, B_WIDTH, D_MODEL), B_WIDTH ** -0.5)
    w_out = nrm(ks[9], (DEPTH, D_MODEL, D_MODEL), D_MODEL ** -0.5 * DN_BETA)
    ln1_g = 1.0 + nrm(ks[10], (DEPTH, D_MODEL), 0.02)
    ln1_b = nrm(ks[11], (DEPTH, D_MODEL), 0.02)
    ffn_w_gate = nrm(ks[12], (N_DENSE, D_MODEL, D_FF), D_MODEL ** -0.5)
    ffn_w_up = nrm(ks[13], (N_DENSE, D_MODEL, D_FF), D_MODEL ** -0.5)
    ffn_w_down = nrm(ks[14], (N_DENSE, D_FF, D_MODEL), D_FF ** -0.5 * DN_BETA)
    router_w = nrm(ks[15], (N_MOE, D_MODEL, N_EXPERTS), D_MODEL ** -0.5)
    moe_w_gate = nrm(ks[16], (N_MOE, N_EXPERTS, D_MODEL, D_EXPERT), D_MODEL ** -0.5)
    moe_w_up = nrm(ks[17], (N_MOE, N_EXPERTS, D_MODEL, D_EXPERT), D_MODEL ** -0.5)
    moe_w_down = nrm(ks[18], (N_MOE, N_EXPERTS, D_EXPERT, D_MODEL), D_EXPERT ** -0.5 * DN_BETA)
    ln2_g = 1.0 + nrm(ks[19], (DEPTH, D_MODEL), 0.02)
    ln2_b = nrm(ks[20], (DEPTH, D_MODEL), 0.02)
    return {"x": x, "positions": positions, "w_in": w_in, "conv_w": conv_w,
            "a_log": a_log, "dt_bias": dt_bias, "dn_norm_w": dn_norm_w,
            "w_branch_a": w_branch_a, "w_branch_b": w_branch_b, "w_out": w_out,
            "ln1_g": ln1_g, "ln1_b": ln1_b,
            "ffn_w_gate": ffn_w_gate, "ffn_w_up": ffn_w_up, "ffn_w_down": ffn_w_down,
            "router_w": router_w, "moe_w_gate": moe_w_gate, "moe_w_up": moe_w_up,
            "moe_w_down": moe_w_down, "ln2_g": ln2_g, "ln2_b": ln2_b}


def reference(x, positions, w_in, conv_w, a_log, dt_bias, dn_norm_w, w_branch_a, w_branch_b,
              w_out, ln1_g, ln1_b, ffn_w_gate, ffn_w_up, ffn_w_down, router_w,
              moe_w_gate, moe_w_up, moe_w_down, ln2_g, ln2_b):
    bsz, seq, _ = x.shape
    cos, sin = rotary_tables(positions)
    a_shape = (bsz, seq, A_GROUPS, A_HEADS, A_HEAD_DIM)
    for layer in range(DEPTH):
        h = x @ w_in[layer]
        qa, ka, va, qkv_b, z_b, beta_b, a_b, gate_a, gate_b = jnp.split(h, IN_SPLITS, axis=-1)
        y_a = mixer_dilated(qa.reshape(a_shape), ka.reshape(a_shape), va.reshape(a_shape),
                            cos, sin).astype(x.dtype)
        y_b = mixer_gated_deltanet(qkv_b, z_b, beta_b, a_b, conv_w[layer], a_log[layer],
                                   dt_bias[layer], dn_norm_w[layer]).astype(x.dtype)
        merged = (jax.nn.sigmoid(gate_a) * (y_a @ w_branch_a[layer])
                  + jax.nn.sigmoid(gate_b) * (y_b @ w_branch_b[layer]))
        x = layer_norm(DN_ALPHA * x + merged @ w_out[layer], ln1_g[layer], ln1_b[layer])
        if layer % 2 == 0:
            f = swiglu(x, ffn_w_gate[layer // 2], ffn_w_up[layer // 2], ffn_w_down[layer // 2])
        else:
            f = moe_swiglu(x, router_w[layer // 2], moe_w_gate[layer // 2],
                           moe_w_up[layer // 2], moe_w_down[layer // 2])
        x = layer_norm(DN_ALPHA * x + f, ln2_g[layer], ln2_b[layer])
    return x
```

```python
import math
import os
from contextlib import ExitStack
import numpy as np
import concourse.bass as bass
import concourse.mybir as mybir
from concourse.bass_utils import run_bass_kernel_spmd

F32 = mybir.dt.float32
BF16 = mybir.dt.bfloat16
I32 = mybir.dt.int32
AF = mybir.ActivationFunctionType
ALU = mybir.AluOpType
AX = mybir.AxisListType

D = 1024
T = 4096
NIN = 10768
DFF = 2816
NE = 8
DEX = 3584
ALPHA = 4 ** 0.25
LN_EPS = 1e-5
RMS_EPS = 1e-6
PI = math.pi
A_PAIRS = ((128, 1), (512, 4), (2048, 16))
NDS = 8


class Tok:
    __slots__ = ("w", "r")

    def __init__(self):
        self.w = None
        self.r = {}


class KB:
    def __init__(self, nc):
        self.nc = nc
        self.E = {"pe": nc.tensor, "dve": nc.vector, "act": nc.scalar, "pool": nc.gpsimd, "sp": nc.sync}
        self.csem = {e: nc.alloc_semaphore("cs_" + e) for e in self.E}
        self.ccnt = {e: 0 for e in self.E}
        self.seen = {e: {} for e in self.E}
        self.dq = {q: [[nc.alloc_semaphore(f"ds_{q}{i}"), 0] for i in range(NDS)] for q in ("sp", "pool", "act")}
        self.dnext = {q: 0 for q in self.dq}
        self.ninst = 0

    def _sem(self, ev):
        if ev[0] == "c":
            return self.csem[ev[1]]
        return self.dq[ev[1]][ev[2]][0]

    def _wait(self, e, ev):
        key = ev[:-1]
        val = ev[-1]
        if self.seen[e].get(key, 0) >= val:
            return
        self.E[e].wait_ge(self._sem(ev), val)
        self.seen[e][key] = val
        self.ninst += 1

    def _deps(self, e, reads, writes):
        for t in reads:
            if t.w is not None:
                self._wait(e, t.w)
        for t in writes:
            if t.w is not None and not (e == "pe" and t.w[0] == "c" and t.w[1] == "pe"):
                self._wait(e, t.w)
            for k, ev in t.r.items():
                self._wait(e, ev)

    def _mark(self, ev, reads, writes):
        for t in reads:
            t.r[ev[:-1]] = ev
        for t in writes:
            t.w = ev
            t.r = {}

    def op(self, e, fn, reads=(), writes=()):
        self._deps(e, reads, writes)
        ins = fn(self.E[e])
        self.ccnt[e] += 1
        ins.then_inc(self.csem[e], 1)
        self.ninst += 1
        self._mark(("c", e, self.ccnt[e]), reads, writes)

    def dma(self, q, out, in_, reads=(), writes=()):
        self._deps(q, reads, writes)
        i = self.dnext[q]
        self.dnext[q] = (i + 1) % NDS
        slot = self.dq[q][i]
        if slot[1] > 0:
            self._wait(q, ("d", q, i, slot[1]))
        self.E[q].dma_start(out=out, in_=in_).then_inc(slot[0], 16)
        slot[1] += 16
        self.ninst += 1
        self._mark(("d", q, i, slot[1]), reads, writes)

    def barrier(self):
        for e in self.E:
            for f in self.E:
                if self.ccnt[f] > 0:
                    self._wait(e, ("c", f, self.ccnt[f]))
            for q in self.dq:
                for i, slot in enumerate(self.dq[q]):
                    if slot[1] > 0:
                        self._wait(e, ("d", q, i, slot[1]))


def ss(t0, n, d):
    return slice(t0, t0 + (n - 1) * d + 1, d)


def toks(n):
    return [Tok() for _ in range(n)]


C_ID, C_U, C_LI, C_LS, C_ONE, C_PERM, C_INVF, C_SIGN, C_MBS, C_MBT, C_N = 0, 128, 256, 384, 512, 640, 768, 769, 776, 904, 1032


def make_consts():
    c = np.zeros((128, C_N), np.float32)
    p = np.arange(128)
    c[:, C_ID:C_ID + 128] = np.eye(128)
    c[:, C_U:C_U + 128] = (p[:, None] <= p[None, :])
    c[:, C_LI:C_LI + 128] = (p[:, None] >= p[None, :])
    c[:, C_LS:C_LS + 128] = (p[:, None] > p[None, :])
    c[:, C_ONE:C_ONE + 128] = 1.0
    pm = np.zeros((32, 32), np.float32)
    for m in range(32):
        pm[(m + 16) % 32, m] = 1.0
    c[:32, C_PERM:C_PERM + 32] = pm
    invf = (500000.0 ** (-np.arange(0, 32, 2, dtype=np.float32) / 32)).astype(np.float32)
    c[:32, C_INVF] = np.concatenate([invf, invf])
    c[:16, C_SIGN] = -1.0
    c[16:32, C_SIGN] = 1.0
    c[:, C_MBS:C_MBS + 128] = 30000.0 * (1.0 - (p[:, None] > p[None, :]))
    c[:, C_MBT:C_MBT + 128] = -30000.0 * (1.0 - (p[:, None] <= p[None, :]))
    return c


class Prog:
    def __init__(self, nseq, dbg=None, stop_after=None):
        self.nseq = nseq
        self.dbg = dbg or ()
        self.stop_after = stop_after
        nc = bass.Bass("TRN2", target_bir_lowering=False)
        self.nc = nc
        self.kb = KB(nc)
        NT = nseq * T
        self.NT = NT
        dt = nc.dram_tensor
        I = "ExternalInput"
        self.x_in = dt("x", [NT, D], F32, kind=I)
        self.pos = dt("pos", [nseq, T], I32, kind=I)
        self.cst = dt("cst", [128, C_N], F32, kind=I)
        self.w_in = dt("w_in", [2, D, NIN], F32, kind=I)
        self.conv_w = dt("conv_w", [2, 128, 24, 4], F32, kind=I)
        self.a_log = dt("a_log", [2, 8], F32, kind=I)
        self.dt_bias = dt("dt_bias", [2, 8], F32, kind=I)
        self.dn_norm_w = dt("dn_norm_w", [2, 128], F32, kind=I)
        self.w_a = dt("w_branch_a", [2, 512, D], F32, kind=I)
        self.w_b = dt("w_branch_b", [2, D, D], F32, kind=I)
        self.w_o = dt("w_out", [2, D, D], F32, kind=I)
        self.ln1_g = dt("ln1_g", [2, D], F32, kind=I)
        self.ln1_b = dt("ln1_b", [2, D], F32, kind=I)
        self.ln2_g = dt("ln2_g", [2, D], F32, kind=I)
        self.ln2_b = dt("ln2_b", [2, D], F32, kind=I)
        self.ffn_g = dt("ffn_w_gate", [1, D, DFF], F32, kind=I)
        self.ffn_u = dt("ffn_w_up", [1, D, DFF], F32, kind=I)
        self.ffn_d = dt("ffn_w_down", [1, DFF, D], F32, kind=I)
        self.router = dt("router_w", [1, D, NE], F32, kind=I)
        self.moe_g = dt("moe_w_gate", [1, NE, D, DEX], F32, kind=I)
        self.moe_u = dt("moe_w_up", [1, NE, D, DEX], F32, kind=I)
        self.moe_d = dt("moe_w_down", [1, NE, DEX, D], F32, kind=I)
        self.y_out = dt("y", [NT, D], F32, kind="ExternalOutput")
        self.QKT = self.scr("QKT", [24, 128, T], BF16)
        self.VA = self.scr("VA", [3, 4, 128, 32, 128], BF16)
        self.GQT = self.scr("GQT", [24, 128, T], BF16)
        self.ZT = self.scr("ZT", [8, 128, T], BF16)
        self.GT = self.scr("GT", [16, 128, T], BF16)
        self.BG = self.scr("BG", [T, 16], F32)
        self.YAT = self.scr("YAT", [4, 128, T], BF16)
        self.YBT = self.scr("YBT", [8, 128, T], BF16)
        self.X1 = self.scr("X1", [T, D], F32)
        self.X2 = self.scr("X2", [T, D], F32)
        self.GATES = self.scr("GATES", [T, NE], F32)
        self.CSd = self.scr("CSd", [32, 2, T], F32)

    def scr(self, name, shape, dtype):
        kind = "ExternalOutput" if name in self.dbg else "Internal"
        return self.nc.dram_tensor(name, shape, dtype, kind=kind)

    def sb(self, es, name, shape, dtype):
        self.uid = getattr(self, "uid", 0) + 1
        return es.enter_context(self.nc.sbuf_tensor(f"{name}_{self.uid}", shape, dtype))

    def ps(self, es, name, shape=(128, 512), dtype=F32):
        self.uid = getattr(self, "uid", 0) + 1
        return es.enter_context(self.nc.psum_tensor(f"{name}_{self.uid}", list(shape), dtype))

    def bc_ap(self, handle, offset, n, parts=128):
        return bass.AP(handle, offset, [[0, parts], [1, n]])

    def wload(self, stage, tstage, dst, tdst, src, q="pool"):
        kb = self.kb
        kb.dma(q, stage, src, writes=[tstage])
        kb.op("pool", lambda e: e.tensor_copy(dst, stage), reads=[tstage], writes=[tdst])

    def load_consts(self, es):
        kb = self.kb
        self.c32 = self.sb(es, "c32", [128, C_N], F32)
        self.cbf = self.sb(es, "cbf", [128, C_N], BF16)
        self.tc = Tok()
        kb.dma("sp", self.c32[:], self.cst.ap()[:, :], writes=[self.tc])
        kb.op("dve", lambda e: e.tensor_copy(self.cbf[:], self.c32[:]), reads=[self.tc], writes=[self.tc])

    def transpose_rows(self, es_tag, src_sb, tsrc, xT, txT, col0, psT, tps, idx, x32=None, tx32=None):
        kb = self.kb
        ident = self.c32[:, C_ID:C_ID + 128]
        for hf in range(2):
            p = psT[(2 * idx + hf) % len(psT)]
            tp = tps[(2 * idx + hf) % len(psT)]

            def f(e, hf=hf, p=p):
                ins = None
                for j in range(4):
                    c = hf * 4 + j
                    ins = e.transpose(p[:, j * 128:(j + 1) * 128], src_sb[:, c * 128:(c + 1) * 128], ident)
                return ins
            kb.op("pe", f, reads=[tsrc, self.tc], writes=[tp])
            pv = p[:, :].rearrange("p (j t) -> p j t", j=4)
            if x32 is None:
                kb.op("act", lambda e, hf=hf, pv=pv: e.copy(xT[:, hf * 4:hf * 4 + 4, col0:col0 + 128], pv),
                      reads=[tp], writes=[txT])
            else:
                kb.op("act", lambda e, hf=hf, pv=pv: e.copy(x32[:, hf * 4:hf * 4 + 4, :], pv), reads=[tp], writes=[tx32])
                kb.op("dve", lambda e, hf=hf: e.tensor_copy(xT[:, hf * 4:hf * 4 + 4, col0:col0 + 128], x32[:, hf * 4:hf * 4 + 4, :]),
                      reads=[tx32], writes=[txT])

    def phase0(self, s, xT, txT):
        kb = self.kb
        with ExitStack() as es:
            xin = [self.sb(es, f"p0x{i}", [128, D], F32) for i in range(2)]
            tx = toks(2)
            psT = [self.ps(es, f"p0ps{i}") for i in range(4)]
            tps = toks(4)
            for t in range(T // 128):
                b = t % 2
                r0 = s * T + t * 128
                kb.dma("sp", xin[b][:], self.x_in.ap()[r0:r0 + 128, :], writes=[tx[b]])
                self.transpose_rows(None, xin[b], tx[b], xT, txT, t * 128, psT, tps, t)
            kb.barrier()

    def rope_tables(self, s):
        kb = self.kb
        with ExitStack() as es:
            pi_ = self.sb(es, "rp_i", [32, T], I32)
            ang = self.sb(es, "rp_a", [32, T], F32)
            kf = self.sb(es, "rp_k", [32, T], F32)
            ki = self.sb(es, "rp_ki", [32, T], I32)
            u = self.sb(es, "rp_u", [32, T], F32)
            cr = self.sb(es, "rp_c", [32, T], F32)
            CS = self.sb(es, "rp_CS", [32, 2, T], F32)
            tCS = Tok()
            t1 = Tok()
            kb.dma("sp", pi_[:], self.bc_ap(self.pos, s * T, T, 32), writes=[t1])
            kb.op("dve", lambda e: e.tensor_copy(ang[:], pi_[:]), reads=[t1], writes=[t1])
            kb.op("dve", lambda e: e.tensor_scalar(ang[:], ang[:], self.c32[0:32, C_INVF:C_INVF + 1], None, ALU.mult),
                  reads=[t1, self.tc], writes=[t1])
            t2 = Tok()
            for which in range(2):
                sh = PI / 2 if which == 0 else 0.0
                kb.op("dve", lambda e: e.tensor_scalar(kf[:], ang[:], 1.0 / (2 * PI), sh / (2 * PI), ALU.mult, ALU.add),
                      reads=[t1], writes=[t2])
                kb.op("dve", lambda e: e.tensor_copy(ki[:], kf[:]), reads=[t2], writes=[t2])
                kb.op("dve", lambda e: e.tensor_copy(kf[:], ki[:]), reads=[t2], writes=[t2])
                kb.op("dve", lambda e: e.scalar_tensor_tensor(u[:], kf[:], -2 * PI, ang[:], ALU.mult, ALU.add),
                      reads=[t2, t1], writes=[t2])
                if sh != 0.0:
                    kb.op("dve", lambda e: e.tensor_scalar(u[:], u[:], sh, None, ALU.add), reads=[t2], writes=[t2])
                kb.op("dve", lambda e: e.tensor_scalar(cr[:], u[:], PI, -2 * PI, ALU.is_gt, ALU.mult), reads=[t2], writes=[t2])
                kb.op("dve", lambda e: e.tensor_tensor(u[:], u[:], cr[:], ALU.add), reads=[t2], writes=[t2])
                kb.op("dve", lambda e: e.tensor_scalar(cr[:], u[:], -PI, 2 * PI, ALU.is_lt, ALU.mult), reads=[t2], writes=[t2])
                kb.op("dve", lambda e: e.tensor_tensor(u[:], u[:], cr[:], ALU.add), reads=[t2], writes=[t2])
                kb.op("dve", lambda e: e.tensor_scalar(u[:], u[:], PI, -PI, ALU.min, ALU.max), reads=[t2], writes=[t2])
                if which == 0:
                    kb.op("act", lambda e: e.activation(CS[0:32, 0, :], u[:], AF.Sin), reads=[t2], writes=[tCS])
                else:
                    kb.op("act", lambda e: e.activation(u[:], u[:], AF.Sin), reads=[t2], writes=[t2])
                    kb.op("dve", lambda e: e.tensor_scalar(CS[0:32, 1, :], u[:], self.c32[0:32, C_SIGN:C_SIGN + 1], None, ALU.mult),
                          reads=[t2, self.tc], writes=[tCS])
            kb.dma("sp", self.CSd.ap()[:, :, :], CS[:], reads=[tCS])
            kb.barrier()

    def phase1(self, L, s, xT, txT):
        kb = self.kb
        nc = self.nc
        win = self.w_in.ap()[L]
        NTT = T // 512
        with ExitStack() as es:
            wq = [self.sb(es, f"p1w{i}", [128, 8, 128], BF16) for i in range(2)]
            twq = toks(2)
            stg = [self.sb(es, f"p1s{i}", [128, T], BF16) for i in range(2)]
            tst = toks(2)
            raw = self.sb(es, "p1raw", [128, T + 3], F32)
            traw = Tok()
            acc = self.sb(es, "p1acc", [128, T], F32)
            tacc = Tok()
            h32 = [self.sb(es, f"p1h{i}", [128, 512], F32) for i in range(2)]
            th32 = toks(2)
            r1 = [self.sb(es, f"p1r{i}", [32, 512], F32) for i in range(2)]
            tr1 = toks(2)
            cw = self.sb(es, "p1cw", [128, 24, 4], F32)
            tcw = Tok()
            ps = [self.ps(es, f"p1ps{i}") for i in range(4)]
            tps = toks(4)
            ps2 = [self.ps(es, f"p1pq{i}") for i in range(2)]
            tps2 = toks(2)
            nps = [0]
            CS = self.sb(es, "p1CS", [32, 2, T], F32)
            tCS = Tok()
            kb.dma("sp", CS[:], self.CSd.ap()[:, :, :], writes=[tCS])

            kb.dma("sp", cw[:], self.conv_w.ap()[L], writes=[tcw])
            kb.op("dve", lambda e: e.memset(raw[:, 0:3], 0.0), writes=[traw])

            wst = [self.sb(es, f"p1wst{i}", [128, 8, 128], F32) for i in range(2)]
            twst = toks(2)
            wbig = self.sb(es, "p1wbig", [128, 8, 512], F32)
            twbig = Tok()
            wcols = [c * 128 for c in range(24)] + [4608 + c * 128 for c in range(24)] + \
                    [7680 + c * 128 for c in range(8)] + [8720 + c * 128 for c in range(16)]

            def w_dma(ci):
                b = ci % 2
                kb.dma("pool", wst[b][:], win[:, wcols[ci]:wcols[ci] + 128].rearrange("(k p) n -> p k n", p=128), writes=[twst[b]])

            def load_w(ci, col0):
                assert wcols[ci] == col0
                b = ci % 2
                if ci % 24 == 0:
                    w_dma(ci)
                if ci + 1 < len(wcols) and (ci + 1) % 24 != 0:
                    w_dma(ci + 1)
                kb.op("pool", lambda e: e.tensor_copy(wq[b][:], wst[b][:]), reads=[twst[b]], writes=[twq[b]])
                return wq[b], twq[b]

            def proj_tile(w, tw, tt, ncols=128):
                i = nps[0] % 4
                nps[0] += 1

                def f(e):
                    ins = None
                    for k in range(8):
                        ins = e.matmul(ps[i][0:ncols, :], w[:, k, 0:ncols], xT[:, k, tt * 512:(tt + 1) * 512],
                                       start=(k == 0), stop=(k == 7))
                    return ins
                kb.op("pe", f, reads=[tw, txT], writes=[tps[i]])
                return ps[i], tps[i]

            ci = 0
            SEC = os.environ.get('P1SEC', 'ABCDE')
            for c in (range(int(os.environ.get('P1NA', '24'))) if 'A' in SEC else ()):
                w, tw = load_w(ci, c * 128)
                ci += 1
                sb_ = c % 2
                for tt in range(NTT):
                    p, tp = proj_tile(w, tw, tt)
                    cols = slice(tt * 512, (tt + 1) * 512)
                    hb = (c * NTT + tt) % 2
                    kb.op("act", lambda e, p=p: e.copy(stg[sb_][:, cols], p[:, :]), reads=[tp], writes=[tst[sb_]])
                    AV = int(os.environ.get('P1AV', '3'))
                    if AV >= 2:
                        if os.environ.get('P1CP', 'act') == 'ts':
                            kb.op("dve", lambda e, p=p: e.tensor_scalar(h32[hb][:], p[:, :], 1.0, None, ALU.mult), reads=[tp], writes=[th32[hb]])
                        else:
                            kb.op("act", lambda e, p=p: e.copy(h32[hb][:], p[:, :]), reads=[tp], writes=[th32[hb]])
                        if os.environ.get('P1AW', 'c') >= 'b':
                            kb.op("dve", lambda e: e.tensor_tensor(r1[hb][:], h32[hb][0:32, :], CS[0:32, 0, cols], ALU.mult),
                                  reads=[th32[hb], tCS], writes=[tr1[hb]])
                    if AV >= 3:
                        kb.op("pe", lambda e: e.matmul(ps2[hb][:, :], self.c32[:, C_PERM:C_PERM + 128], h32[hb][:],
                                                       start=True, stop=True), reads=[th32[hb], self.tc], writes=[tps2[hb]])
                        kb.op("dve", lambda e: e.tensor_tensor(h32[hb][0:32, :], ps2[hb][0:32, :], CS[0:32, 1, cols], ALU.mult),
                              reads=[tps2[hb], tCS], writes=[th32[hb]])
                    if AV >= 2 and os.environ.get('P1AW', 'c') >= 'c':
                        kb.op("dve", lambda e: e.tensor_tensor(stg[sb_][0:32, cols], r1[hb][:], h32[hb][0:32, :], ALU.add),
                              reads=[tr1[hb], th32[hb]], writes=[tst[sb_]])
                kb.dma("sp", self.QKT.ap()[c], stg[sb_][:], reads=[tst[sb_]])
            wv = self.sb(es, "p1wv", [128, 8, 512], BF16)
            twv = Tok()
            vst = [self.sb(es, f"p1vs{i}", [128, 512], BF16) for i in range(2)]
            tvs = toks(2)
            nb_ = 0
            for g, (win_, dil) in (enumerate(A_PAIRS) if 'B' in SEC else ()):
                self.wload(wbig[:], twbig, wv[:], twv, win[:, 3072 + g * 512:3072 + (g + 1) * 512].rearrange("(k p) n -> p k n", p=128), q="sp")
                nblk = 32 // dil
                for r in range(dil):
                    for n in range(nblk):
                        blk = r * nblk + n
                        t0 = 128 * n * dil + r
                        i = nps[0] % 4
                        nps[0] += 1

                        def f(e, i=i, t0=t0, dil=dil):
                            ins = None
                            for k in range(8):
                                ins = e.matmul(ps[i][:, :], xT[:, k, ss(t0, 128, dil)], wv[:, k, :],
                                               start=(k == 0), stop=(k == 7))
                            return ins
                        kb.op("pe", f, reads=[twv, txT], writes=[tps[i]])
                        vb = nb_ % 2
                        nb_ += 1
                        kb.op("act", lambda e, i=i, vb=vb: e.copy(vst[vb][:], ps[i][:, :]), reads=[tps[i]], writes=[tvs[vb]])
                        kb.dma("sp", self.VA.ap()[g, :, :, blk, :].rearrange("h p e -> p h e"),
                               vst[vb][:, :].rearrange("p (h e) -> p h e", h=4), reads=[tvs[vb]])
            ci = 24
            for c in (range(24) if 'C' in SEC else ()):
                w, tw = load_w(ci, 4608 + c * 128)
                ci += 1
                sb_ = c % 2
                for tt in range(NTT):
                    p, tp = proj_tile(w, tw, tt)
                    kb.op("act", lambda e, p=p, tt=tt: e.copy(raw[:, 3 + tt * 512:3 + (tt + 1) * 512], p[:, :]),
                          reads=[tp], writes=[traw])
                for hh in range(2):
                    cs = slice(hh * 2048, (hh + 1) * 2048)
                    kb.op("dve", lambda e, cs=cs, hh=hh: e.tensor_scalar(acc[:, cs], raw[:, hh * 2048:hh * 2048 + 2048],
                                                                          cw[:, c, 0:1], None, ALU.mult),
                          reads=[traw, tcw], writes=[tacc])
                    for j in range(1, 4):
                        kb.op("dve", lambda e, cs=cs, hh=hh, j=j: e.scalar_tensor_tensor(
                            acc[:, cs], raw[:, hh * 2048 + j:hh * 2048 + j + 2048], cw[:, c, j:j + 1], acc[:, cs],
                            ALU.mult, ALU.add), reads=[traw, tcw, tacc], writes=[tacc])
                if c >= 16:
                    kb.op("act", lambda e: e.activation(stg[sb_][:], acc[:], AF.Silu), reads=[tacc], writes=[tst[sb_]])
                else:
                    kb.op("act", lambda e: e.activation(acc[:], acc[:], AF.Silu), reads=[tacc], writes=[tacc])
                    for tt in range(NTT):
                        cols = slice(tt * 512, (tt + 1) * 512)
                        hb = tt % 2
                        sq = raw
                        kb.op("dve", lambda e, cols=cols: e.tensor_tensor(raw[:, cols], acc[:, cols], acc[:, cols], ALU.mult),
                              reads=[tacc], writes=[traw])
                        i = nps[0] % 4
                        nps[0] += 1
                        kb.op("pe", lambda e, i=i, cols=cols: e.matmul(ps[i][:, :], self.c32[:, C_ONE:C_ONE + 128], raw[:, cols],
                                                                       start=True, stop=True),
                              reads=[traw, self.tc], writes=[tps[i]])
                        sc = (1.0 / 128) ** 0.5 if c < 8 else 1.0
                        kb.op("act", lambda e, i=i, cols=cols, sc=sc: e.activation(raw[:, cols], ps[i][:, :], AF.Sqrt,
                                                                                    bias=self.epsb[:, 0:1] if sc == 1.0 else self.epsb[:, 1:2],
                                                                                    scale=1.0 / (sc * sc)),
                              reads=[tps[i], self.tc], writes=[traw])
                        kb.op("dve", lambda e, cols=cols: e.reciprocal(raw[:, cols], raw[:, cols]), reads=[traw], writes=[traw])
                        kb.op("dve", lambda e, cols=cols: e.tensor_tensor(stg[sb_][:, cols], acc[:, cols], raw[:, cols], ALU.mult),
                              reads=[traw, tacc], writes=[tst[sb_]])
                    kb.op("dve", lambda e: e.memset(raw[:, 0:3], 0.0), reads=[traw], writes=[traw])
                kb.dma("sp", self.GQT.ap()[c], stg[sb_][:], reads=[tst[sb_]])
            ci = 48
            for c in (range(24) if 'D' in SEC else ()):
                col0 = 7680 + c * 128 if c < 8 else 8720 + (c - 8) * 128
                w, tw = load_w(ci, col0)
                ci += 1
                sb_ = c % 2
                fn = AF.Silu if c < 8 else AF.Sigmoid
                for tt in range(NTT):
                    p, tp = proj_tile(w, tw, tt)
                    kb.op("act", lambda e, p=p, tt=tt, fn=fn: e.activation(stg[sb_][:, tt * 512:(tt + 1) * 512], p[:, :], fn),
                          reads=[tp], writes=[tst[sb_]])
                dst = self.ZT.ap()[c] if c < 8 else self.GT.ap()[c - 8]
                kb.dma("sp", dst, stg[sb_][:], reads=[tst[sb_]])
            wbd = self.sb(es, "p1wbd", [128, 8, 16], BF16)
            twbd = Tok()
            self.wload(wbig[:, :, 0:16], twbig, wbd[:], twbd, win[:, 8704:8720].rearrange("(k p) n -> p k n", p=128), q="sp")
            rows = self.sb(es, "p1rows", [128, 16], F32)
            trows = Tok()
            kb.dma("sp", rows[:, 0:8], self.bc_ap(self.dt_bias, L * 8, 8), writes=[trows])
            kb.dma("sp", rows[:, 8:16], self.bc_ap(self.a_log, L * 8, 8), writes=[trows])
            kb.op("act", lambda e: e.activation(rows[:, 8:16], rows[:, 8:16], AF.Exp), reads=[trows], writes=[trows])
            bg = self.sb(es, "p1bg", [128, 32, 16], F32)
            tbg = Tok()
            tmp = self.sb(es, "p1tmp", [128, 8], F32)
            ttmp = Tok()
            for t in (range(32) if 'E' in SEC else ()):
                i = nps[0] % 4
                nps[0] += 1

                def f(e, i=i, t=t):
                    ins = None
                    for k in range(8):
                        ins = e.matmul(ps[i][:, 0:16], xT[:, k, t * 128:(t + 1) * 128], wbd[:, k, :], start=(k == 0), stop=(k == 7))
                    return ins
                kb.op("pe", f, reads=[twbd, txT], writes=[tps[i]])
                kb.op("act", lambda e, i=i, t=t: e.activation(bg[:, t, 0:8], ps[i][:, 0:8], AF.Sigmoid), reads=[tps[i]], writes=[tbg])
                kb.op("dve", lambda e, i=i: e.tensor_tensor(tmp[:], ps[i][:, 8:16], rows[:, 0:8], ALU.add),
                      reads=[tps[i], trows], writes=[ttmp])
                kb.op("act", lambda e: e.activation(tmp[:], tmp[:], AF.Exp), reads=[ttmp], writes=[ttmp])
                kb.op("act", lambda e: e.activation(tmp[:], tmp[:], AF.Ln, bias=self.epsb[:, 2:3]), reads=[ttmp, self.tc], writes=[ttmp])
                kb.op("dve", lambda e, t=t: e.scalar_tensor_tensor(bg[:, t, 8:16], tmp[:], -1.0, rows[:, 8:16], ALU.mult, ALU.mult),
                      reads=[ttmp, trows], writes=[tbg])
            kb.dma("sp", self.BG.ap().rearrange("(t p) c -> p t c", p=128), bg[:], reads=[tbg])
            kb.barrier()


    def phase2(self, s, xT):
        kb = self.kb
        scale = 128.0 ** -0.5
        with ExitStack() as es:
            QT = [xT[:, g, :] for g in range(3)]
            KT = [xT[:, 3 + g, :] for g in range(3)]
            VV = [self.sb(es, f"p2v{g}", [128, 32, 128], BF16) for g in range(3)]
            tq, tk, tv = toks(3), toks(3), toks(3)
            num = self.sb(es, "p2num", [128, T], F32)
            den = self.sb(es, "p2den", [128, T], F32)
            tnum, tden = Tok(), Tok()
            PT = [self.sb(es, f"p2pt{i}", [128, 256], BF16) for i in range(2)]
            tpt = toks(2)
            yst = self.sb(es, "p2y", [128, T], BF16)
            tyst = Tok()
            psS = [self.ps(es, f"p2pS{i}") for i in range(2)]
            psN = [self.ps(es, f"p2pN{i}") for i in range(2)]
            psD = [self.ps(es, f"p2pD{i}") for i in range(2)]
            tS, tN, tD = toks(2), toks(2), toks(2)
            ones_bf = self.cbf[:, C_ONE:C_ONE + 128]
            maskcat = self.cbf[:, C_U:C_U + 256]
            kbi = 0
            for slot in range(4):
                for g in range(3):
                    kb.dma("sp", QT[g], self.QKT.ap()[g * 4 + slot], writes=[tq[g]])
                    kb.dma("sp", KT[g], self.QKT.ap()[12 + g * 4 + slot], writes=[tk[g]])
                    kb.dma("sp", VV[g][:], self.VA.ap()[g, slot], writes=[tv[g]])
                kb.op("dve", lambda e: e.memset(num[:], 0.0), writes=[tnum])
                kb.op("dve", lambda e: e.memset(den[:], 0.0), writes=[tden])
                for g, (win_, dil) in enumerate(A_PAIRS):
                    nblk = 32 // dil
                    for r in range(dil):
                        for n in range(nblk):
                            blk = r * nblk + n
                            nq = 2 if n + 1 < nblk else 1
                            t0 = 128 * n * dil + r
                            b = kbi % 2
                            kbi += 1
                            kb.op("pe", lambda e, b=b, g=g, t0=t0, nq=nq, dil=dil: e.matmul(
                                psS[b][:, 0:128 * nq], KT[g][:, ss(t0, 128, dil)], QT[g][:, ss(t0, 128 * nq, dil)],
                                start=True, stop=True), reads=[tk[g], tq[g]], writes=[tS[b]])
                            kb.op("act", lambda e, b=b, nq=nq: e.activation(PT[b][:, 0:128 * nq], psS[b][:, 0:128 * nq], AF.Exp,
                                                                            scale=scale), reads=[tS[b]], writes=[tpt[b]])
                            kb.op("dve", lambda e, b=b, nq=nq: e.tensor_tensor(PT[b][:, 0:128 * nq], PT[b][:, 0:128 * nq],
                                                                              maskcat[:, 0:128 * nq], ALU.mult),
                                  reads=[tpt[b], self.tc], writes=[tpt[b]])
                            for mo in range(nq):
                                a = (n + mo) % 2

                                def f(e, a=a, mo=mo, b=b, g=g, blk=blk, n=n):
                                    st = (mo == 1 or n == 0)
                                    sp_ = (mo == 0)
                                    e.matmul(psN[a][:, 0:128], VV[g][:, blk, :], PT[b][:, mo * 128:(mo + 1) * 128], start=st, stop=sp_)
                                    return e.matmul(psD[a][:, 0:128], ones_bf, PT[b][:, mo * 128:(mo + 1) * 128], start=st, stop=sp_)
                                kb.op("pe", f, reads=[tv[g], tpt[b], self.tc], writes=[tN[a], tD[a]])
                            a = n % 2
                            kb.op("dve", lambda e, a=a, t0=t0, dil=dil: e.tensor_tensor(
                                num[:, ss(t0, 128, dil)], num[:, ss(t0, 128, dil)], psN[a][:, 0:128], ALU.add),
                                reads=[tN[a], tnum], writes=[tnum])
                            kb.op("dve", lambda e, a=a, t0=t0, dil=dil: e.tensor_tensor(
                                den[:, ss(t0, 128, dil)], den[:, ss(t0, 128, dil)], psD[a][:, 0:128], ALU.add),
                                reads=[tD[a], tden], writes=[tden])
                kb.op("dve", lambda e: e.reciprocal(den[:], den[:]), reads=[tden], writes=[tden])
                kb.op("dve", lambda e: e.tensor_tensor(yst[:], num[:], den[:], ALU.mult), reads=[tnum, tden], writes=[tyst])
                kb.dma("sp", self.YAT.ap()[slot], yst[:], reads=[tyst])
            kb.barrier()


    def phase3(self, L, s, xT):
        kb = self.kb
        c32, cbf = self.c32, self.cbf
        ident = c32[:, C_ID:C_ID + 128]
        ones = c32[:, C_ONE:C_ONE + 128]
        NCH = int(os.environ.get("P3NCH", "32"))
        NH = int(os.environ.get("P3NH", "8"))
        P3STOP = int(os.environ.get("P3STOP", "99"))
        with ExitStack() as es:
            KT, QT, VT, ZTs, ybst = (xT[:, i, :] for i in range(5))
            tld = toks(4)
            tyb = Tok()
            bg = self.sb(es, "p3bg", [128, 32, 16], F32)
            gc = self.sb(es, "p3gc", [128, 32, 8], F32)
            gl = self.sb(es, "p3gl", [128, 32, 8], F32)
            egc = self.sb(es, "p3egc", [128, 32, 8], F32)
            bge = self.sb(es, "p3bge", [128, 32, 8], F32)
            etl = self.sb(es, "p3etl", [128, 32, 8], F32)
            nbt = self.sb(es, "p3nbt", [128, 32, 8], F32)
            sda = self.sb(es, "p3sda", [128, 32, 8], F32)
            ngc = self.sb(es, "p3ngc", [128, 32, 8], F32)
            nwc = self.sb(es, "p3nw", [128, 1], F32)
            tsm = Tok()
            pb = [self.ps(es, f"p3ps{i}") for i in range(7)]
            psT = self.ps(es, "p3psT", (128, 512), BF16)
            tp = {k: Tok() for k in ("T", "g", "kk", "a", "b", "u", "v", "o")}
            tp["w"], tp["s"], tp["q"] = tp["u"], tp["v"], tp["o"]
            ps_g, ps_kk, ps_a, ps_b = pb[0], pb[1], pb[2], pb[3]
            ps_u, ps_w = pb[4][:, 0:128], pb[4][:, 128:256]
            ps_v, ps_s = pb[5][:, 0:128], pb[5][:, 128:256]
            ps_o, ps_q = pb[6][:, 0:128], pb[6][:, 128:256]
            kb.dma("sp", bg[:], self.BG.ap().rearrange("(t p) c -> p t c", p=128), writes=[tsm])
            kb.dma("sp", nwc[:], bass.AP(self.dn_norm_w, L * 128, [[1, 128], [1, 1]]), writes=[tsm])
            gsl = bg[:, :, 8:16]
            bsl = bg[:, :, 0:8]
            v3 = lambda ap: ap.rearrange("p (c h) -> p c h", h=8)
            kb.op("pe", lambda e: e.matmul(v3(pb[0][:, 0:256]), c32[:, C_U:C_U + 128], gsl, start=True, stop=True), reads=[tsm, self.tc], writes=[tp["g"]])
            kb.op("pe", lambda e: e.matmul(v3(pb[1][:, 0:256]), ones, gsl, start=True, stop=True), reads=[tsm, self.tc], writes=[tp["kk"]])
            kb.op("act", lambda e: e.copy(gc[:], v3(pb[0][:, 0:256])), reads=[tp["g"]], writes=[tsm])
            kb.op("act", lambda e: e.copy(gl[:], v3(pb[1][:, 0:256])), reads=[tp["kk"]], writes=[tsm])
            kb.op("act", lambda e: e.activation(egc[:], gc[:], AF.Exp), reads=[tsm], writes=[tsm])
            kb.op("act", lambda e: e.activation(sda[:], gl[:], AF.Exp), reads=[tsm], writes=[tsm])
            kb.op("dve", lambda e: e.tensor_tensor(bge[:], egc[:], bsl, ALU.mult), reads=[tsm], writes=[tsm])
            kb.op("dve", lambda e: e.tensor_tensor(etl[:], gl[:], gc[:], ALU.subtract), reads=[tsm], writes=[tsm])
            kb.op("act", lambda e: e.activation(etl[:], etl[:], AF.Exp), reads=[tsm], writes=[tsm])
            kb.op("dve", lambda e: e.tensor_scalar(nbt[:], bsl, -1.0, None, ALU.mult), reads=[tsm], writes=[tsm])
            kb.op("dve", lambda e: e.tensor_scalar(ngc[:], gc[:], -1.0, None, ALU.mult), reads=[tsm], writes=[tsm])
            kb.op("dve", lambda e: e.tensor_scalar(nwc[:], nwc[:], 128.0 ** 0.5, None, ALU.mult), reads=[tsm], writes=[tsm])

            def S(name, shape, dtype, n=1):
                return [self.sb(es, f"p3{name}{i}", shape, dtype) for i in range(n)]
            Sst, Sbf = S("S", [128, 128], F32)[0], S("Sb", [128, 128], BF16)[0]
            tS = Tok()
            Ug, dmS, dmT, eS, eT, eg = (S(n_, [128, 128], F32)[0] for n_ in ("Ug", "dmS", "dmT", "eS", "eT", "eg"))
            tUg, tdmS, tdmT, teS, teT, teg = toks(6)
            kbg, ktl, vb_ = (S(n_, [128, 128], BF16)[0] for n_ in ("kbg", "ktl", "vb"))
            tkbg, tktl, tvb = toks(3)
            PY = S("PY", [128, 256], F32, 2)
            Qm = S("Qm", [128, 128], F32, 2)
            tPY, tQ = toks(2), toks(2)
            TT, AT, wT, qdT, vnew = (S(n_, [128, 128], BF16)[0] for n_ in ("TT", "AT", "wT", "qdT", "vn"))
            tTT, tAT, twT, tqdT, tvn = toks(5)
            u_, sq, rinv, y1 = (S(n_, [128, 128], F32)[0] for n_ in ("u", "sq", "ri", "y1"))
            tu, tsq, tri, ty1 = toks(4)

            for h in range(NH):
                kb.dma("sp", KT, self.GQT.ap()[8 + h], writes=[tld[0]])
                kb.dma("sp", QT, self.GQT.ap()[h], writes=[tld[1]])
                kb.dma("sp", VT, self.GQT.ap()[16 + h], writes=[tld[2]])
                kb.dma("sp", ZTs, self.ZT.ap()[h], writes=[tld[3]])
                kb.op("dve", lambda e: e.memset(Sst[:], 0.0), writes=[tS])
                kb.op("dve", lambda e: e.memset(Sbf[:], 0.0), writes=[tS])
                for c in range(NCH):
                    cols = slice(c * 128, (c + 1) * 128)
                    col = lambda t: t[:, c, h:h + 1]
                    def ftr(e):
                        e.transpose(psT[:, 0:128], KT[:, cols], cbf[:, C_ID:C_ID + 128])
                        return e.transpose(psT[:, 128:256], VT[:, cols], cbf[:, C_ID:C_ID + 128])
                    kb.op("pe", ftr, reads=[tld[0], tld[2], self.tc], writes=[tp["T"]])
                    kb.op("act", lambda e: e.activation(kbg[:], psT[:, 0:128], AF.Copy, scale=col(bge)), reads=[tp["T"], tsm], writes=[tkbg])
                    kb.op("act", lambda e: e.activation(ktl[:], psT[:, 0:128], AF.Copy, scale=col(etl)), reads=[tp["T"], tsm], writes=[tktl])
                    kb.op("act", lambda e: e.activation(vb_[:], psT[:, 128:256], AF.Copy, scale=col(bsl)), reads=[tp["T"], tsm], writes=[tvb])
                    if P3STOP <= 1:
                        continue
                    kb.op("dve", lambda e: e.tensor_scalar(Ug[:], c32[:, C_U:C_U + 128], col(gsl), None, ALU.mult), reads=[tsm, self.tc], writes=[tUg])
                    kb.op("pe", lambda e: e.matmul(ps_g[:, 0:128], ones, Ug[:], start=True, stop=True), reads=[tUg, self.tc], writes=[tp["g"]])
                    kb.op("act", lambda e: e.activation(Ug[:], ps_g[:, 0:128], AF.Identity, bias=col(ngc), scale=1.0), reads=[tp["g"], tsm], writes=[tUg])
                    kb.op("dve", lambda e: e.tensor_tensor(dmS[:], Ug[:], c32[:, C_MBS:C_MBS + 128], ALU.max), reads=[tUg, self.tc], writes=[tdmS])
                    kb.op("dve", lambda e: e.tensor_tensor(dmT[:], Ug[:], c32[:, C_MBT:C_MBT + 128], ALU.min), reads=[tUg, self.tc], writes=[tdmT])
                    kb.op("act", lambda e: e.activation(eg[:], ps_g[:, 0:128], AF.Exp), reads=[tp["g"]], writes=[teg])
                    kb.op("act", lambda e: e.activation(eS[:], dmS[:], AF.Exp, scale=-1.0), reads=[tdmS], writes=[teS])
                    kb.op("act", lambda e: e.activation(eT[:], dmT[:], AF.Exp), reads=[tdmT], writes=[teT])
                    if P3STOP <= 2:
                        continue
                    kb.op("pe", lambda e: e.matmul(ps_kk[:, 0:128], KT[:, cols], KT[:, cols], start=True, stop=True), reads=[tld[0]], writes=[tp["kk"]])
                    kb.op("dve", lambda e: e.tensor_tensor(eS[:], ps_kk[:, 0:128], eS[:], ALU.mult), reads=[tp["kk"], teS], writes=[teS])
                    kb.op("dve", lambda e: e.tensor_scalar(Qm[0][:], eS[:], col(nbt), None, ALU.mult), reads=[tsm, teS], writes=[tQ[0]])
                    kb.op("pe", lambda e: e.transpose(ps_b[:, 0:128], Qm[0][:], ident), reads=[tQ[0], self.tc], writes=[tp["b"]])
                    kb.op("act", lambda e: e.copy(PY[0][:, 0:128], ps_b[:, 0:128]), reads=[tp["b"]], writes=[tPY[0]])
                    kb.op("dve", lambda e: e.tensor_tensor(PY[0][:, 128:256], ps_b[:, 0:128], ident, ALU.add), reads=[tp["b"], self.tc], writes=[tPY[0]])
                    if P3STOP <= 3:
                        continue
                    for k in range(7):
                        a, b = k % 2, (k + 1) % 2
                        if k == 0:
                            kb.op("pe", lambda e: e.matmul(ps_a[:, 0:128], Qm[a][:], PY[a][:, 0:128], start=True, stop=True),
                                  reads=[tQ[a], tPY[a]], writes=[tp["a"]])
                        elif k < 6:
                            kb.op("pe", lambda e: e.matmul(ps_a[:, 0:256], Qm[a][:], PY[a][:, 0:256], start=True, stop=True),
                                  reads=[tQ[a], tPY[a]], writes=[tp["a"]])
                        else:
                            kb.op("pe", lambda e: e.matmul(ps_a[:, 128:256], Qm[a][:], PY[a][:, 128:256], start=True, stop=True),
                                  reads=[tQ[a], tPY[a]], writes=[tp["a"]])
                        if k < 6:
                            kb.op("pe", lambda e: e.matmul(ps_b[:, 0:128], PY[a][:, 0:128], Qm[a][:], start=True, stop=True),
                                  reads=[tQ[a], tPY[a]], writes=[tp["b"]])
                            kb.op("act", lambda e: e.copy(PY[b][:, 0:128], ps_a[:, 0:128]), reads=[tp["a"]], writes=[tPY[b]])
                            kb.op("act", lambda e: e.copy(Qm[b][:], ps_b[:, 0:128]), reads=[tp["b"]], writes=[tQ[b]])
                            if k == 0:
                                kb.op("dve", lambda e: e.tensor_copy(PY[b][:, 128:256], PY[a][:, 128:256]), reads=[tPY[a]], writes=[tPY[b]])
                            else:
                                kb.op("dve", lambda e: e.tensor_tensor(PY[b][:, 128:256], PY[a][:, 128:256], ps_a[:, 128:256], ALU.add),
                                      reads=[tPY[a], tp["a"]], writes=[tPY[b]])
                        else:
                            kb.op("dve", lambda e: e.tensor_tensor(TT[:], PY[a][:, 128:256], ps_a[:, 128:256], ALU.add),
                                  reads=[tPY[a], tp["a"]], writes=[tTT])
                    if P3STOP <= 4:
                        continue
                    kb.op("pe", lambda e: e.matmul(ps_kk[:, 0:128], KT[:, cols], QT[:, cols], start=True, stop=True), reads=[tld[0], tld[1]], writes=[tp["kk"]])
                    kb.op("dve", lambda e: e.tensor_tensor(AT[:], ps_kk[:, 0:128], eT[:], ALU.mult), reads=[tp["kk"], teT], writes=[tAT])
                    kb.op("pe", lambda e: e.matmul(ps_u, TT[:], vb_[:], start=True, stop=True), reads=[tTT, tvb], writes=[tp["u"]])
                    kb.op("pe", lambda e: e.matmul(ps_w, kbg[:], TT[:], start=True, stop=True), reads=[tTT, tkbg], writes=[tp["w"]])
                    kb.op("act", lambda e: e.copy(u_[:], ps_u), reads=[tp["u"]], writes=[tu])
                    kb.op("act", lambda e: e.copy(wT[:], ps_w), reads=[tp["w"]], writes=[twT])
                    kb.op("dve", lambda e: e.tensor_tensor(qdT[:], QT[:, cols], eg[:], ALU.mult), reads=[tld[1], teg], writes=[tqdT])
                    if P3STOP <= 5:
                        continue
                    kb.op("pe", lambda e: e.matmul(ps_v, wT[:], Sbf[:], start=True, stop=True), reads=[twT, tS], writes=[tp["v"]])
                    kb.op("dve", lambda e: e.tensor_tensor(vnew[:], u_[:], ps_v, ALU.subtract), reads=[tu, tp["v"]], writes=[tvn])

                    def fo(e):
                        e.matmul(ps_o, Sbf[:], qdT[:], start=True, stop=False)
                        return e.matmul(ps_o, vnew[:], AT[:], start=False, stop=True)
                    kb.op("pe", fo, reads=[tS, tqdT, tvn, tAT], writes=[tp["o"]])
                    kb.op("pe", lambda e: e.matmul(ps_s, ktl[:], vnew[:], start=True, stop=True), reads=[tktl, tvn], writes=[tp["s"]])
                    kb.op("dve", lambda e: e.tensor_scalar(Sst[:], Sst[:], col(sda), None, ALU.mult), reads=[tS, tsm], writes=[tS])
                    kb.op("dve", lambda e: e.tensor_tensor(Sst[:], Sst[:], ps_s, ALU.add), reads=[tS, tp["s"]], writes=[tS])
                    kb.op("act", lambda e: e.copy(Sbf[:], Sst[:]), reads=[tS], writes=[tS])
                    if P3STOP <= 6:
                        continue
                    kb.op("act", lambda e: e.activation(sq[:], ps_o, AF.Square), reads=[tp["o"]], writes=[tsq])
                    kb.op("pe", lambda e: e.matmul(ps_q, ones, sq[:], start=True, stop=True), reads=[tsq, self.tc], writes=[tp["q"]])
                    kb.op("act", lambda e: e.activation(rinv[:], ps_q, AF.Sqrt, bias=self.epsb[:, 1:2], scale=1.0), reads=[tp["q"], self.tc], writes=[tri])
                    kb.op("dve", lambda e: e.reciprocal(rinv[:], rinv[:]), reads=[tri], writes=[tri])
                    kb.op("dve", lambda e: e.tensor_tensor(y1[:], ps_o, rinv[:], ALU.mult), reads=[tp["o"], tri], writes=[ty1])
                    kb.op("dve", lambda e: e.scalar_tensor_tensor(ybst[:, cols], y1[:], nwc[:, 0:1], ZTs[:, cols], ALU.mult, ALU.mult),
                          reads=[ty1, tsm, tld[3]], writes=[tyb])
                kb.dma("sp", self.YBT.ap()[h], ybst, reads=[tyb])
            kb.barrier()


    def layernorm(self, pre, tpre, g, b, tgb, out, tout, st, mv, tst):
        kb = self.kb

        def f(e):
            e.bn_stats(st[:, 0:6], pre[:, 0:512])
            return e.bn_stats(st[:, 6:12], pre[:, 512:1024])
        kb.op("dve", f, reads=[tpre], writes=[tst])
        kb.op("dve", lambda e: e.bn_aggr(mv[:, 0:2], st[:, 0:12]), reads=[tst], writes=[tst])
        kb.op("act", lambda e: e.activation(mv[:, 2:3], mv[:, 1:2], AF.Sqrt, bias=self.epsb[:, 3:4]), reads=[tst, self.tc], writes=[tst])
        kb.op("dve", lambda e: e.reciprocal(mv[:, 2:3], mv[:, 2:3]), reads=[tst], writes=[tst])
        kb.op("dve", lambda e: e.tensor_scalar(out, pre, mv[:, 0:1], mv[:, 2:3], ALU.subtract, ALU.mult), reads=[tpre, tst], writes=[tout])
        kb.op("pool", lambda e: e.tensor_tensor(out, out, g, ALU.mult), reads=[tgb], writes=[tout])
        kb.op("pool", lambda e: e.tensor_tensor(out, out, b, ALU.add), reads=[tgb], writes=[tout])

    def phase4(self, L, s, xT, txT, gates, tgates):
        kb = self.kb
        c32 = self.c32
        xres_src = self.x_in.ap()[s * T:(s + 1) * T, :] if L == 0 else self.X2.ap()
        moe = (L == 1)
        with ExitStack() as es:
            Wa = self.sb(es, "p4wa", [128, 4, D], BF16)
            Wb = self.sb(es, "p4wb", [128, 8, D], BF16)
            Wo = self.sb(es, "p4wo", [128, 8, D], BF16)
            tW = Tok()
            stage = self.sb(es, "p4stg", [128, 8, 512], F32)
            tstage = Tok()
            for hf in range(2):
                hs = slice(hf * 512, (hf + 1) * 512)
                self.wload(stage[:, 0:4, :], tstage, Wa[:, :, hs], tW, self.w_a.ap()[L][:, hs].rearrange("(k p) n -> p k n", p=128), q="sp")
                self.wload(stage[:], tstage, Wb[:, :, hs], tW, self.w_b.ap()[L][:, hs].rearrange("(k p) n -> p k n", p=128), q="sp")
                self.wload(stage[:], tstage, Wo[:, :, hs], tW, self.w_o.ap()[L][:, hs].rearrange("(k p) n -> p k n", p=128), q="sp")
            lng = self.sb(es, "p4lng", [128, D], F32)
            lnb = self.sb(es, "p4lnb", [128, D], F32)
            tgb = Tok()
            kb.dma("sp", lng[:], self.bc_ap(self.ln1_g, L * D, D), writes=[tgb])
            kb.dma("sp", lnb[:], self.bc_ap(self.ln1_b, L * D, D), writes=[tgb])
            if moe:
                Wr = self.sb(es, "p4wr", [128, 8, NE], F32)
                kb.dma("sp", Wr[:], self.router.ap()[0].rearrange("(k p) n -> p k n", p=128), writes=[tgb])
                x32 = self.sb(es, "p4x32", [128, 8, 128], F32)
                tx32 = Tok()
                rt = self.sb(es, "p4rt", [128, 64], F32)
                trt = Tok()
            ya = self.sb(es, "p4ya", [128, 4, 512], BF16)
            yb = self.sb(es, "p4yb", [128, 8, 512], BF16)
            gt = self.sb(es, "p4gt", [128, 16, 512], BF16)
            xr = self.sb(es, "p4xr", [128, 4, D], F32)
            tya, tyb, tgt, txr = toks(4)
            mT = self.sb(es, "p4mT", [128, 8, 512], BF16)
            tmT = Tok()
            m1 = [self.sb(es, f"p4m1{i}", [128, 512], F32) for i in range(1)] * 2
            m2 = [self.sb(es, f"p4m2{i}", [128, 512], F32) for i in range(1)] * 2
            tm1, tm2 = toks(1) * 2, toks(1) * 2
            pre = [self.sb(es, f"p4pre{i}", [128, D], F32) for i in range(1)] * 2
            x1t = [self.sb(es, f"p4x1{i}", [128, D], F32) for i in range(2)]
            tpre, tx1 = toks(1) * 2, toks(2)
            st = self.sb(es, "p4st", [128, 12], F32)
            mv = self.sb(es, "p4mv", [128, 4], F32)
            tst = Tok()
            psA = [self.ps(es, f"p4pA{i}") for i in range(2)]
            psB = [self.ps(es, f"p4pB{i}") for i in range(2)]
            psO = [self.ps(es, f"p4pO{i}") for i in range(2)]
            psT = [self.ps(es, f"p4pT{i}") for i in range(2)]
            tpA, tpB, tpO, tpT = toks(2), toks(2), toks(2), toks(2)
            no = 0
            for tt in range(T // 512):
                cs = slice(tt * 512, (tt + 1) * 512)
                kb.dma("sp", ya[:], self.YAT.ap()[:, :, cs].rearrange("k p t -> p k t"), writes=[tya])
                kb.dma("sp", yb[:], self.YBT.ap()[:, :, cs].rearrange("k p t -> p k t"), writes=[tyb])
                kb.dma("sp", gt[:], self.GT.ap()[:, :, cs].rearrange("k p t -> p k t"), writes=[tgt])
                kb.dma("sp", xr[:], xres_src[tt * 512:(tt + 1) * 512, :].rearrange("(j p) d -> p j d", p=128), writes=[txr])
                kb.op("act", lambda e: e.mul(xr[:], xr[:], ALPHA), reads=[txr], writes=[txr])
                for dc in range(8):
                    i = dc % 2
                    ds_ = slice(dc * 128, (dc + 1) * 128)

                    def fa(e, i=i, ds_=ds_):
                        ins = None
                        for k in range(4):
                            ins = e.matmul(psA[i][:, :], Wa[:, k, ds_], ya[:, k, :], start=(k == 0), stop=(k == 3))
                        return ins

                    def fb(e, i=i, ds_=ds_):
                        ins = None
                        for k in range(8):
                            ins = e.matmul(psB[i][:, :], Wb[:, k, ds_], yb[:, k, :], start=(k == 0), stop=(k == 7))
                        return ins
                    kb.op("pe", fa, reads=[tW, tya], writes=[tpA[i]])
                    kb.op("pe", fb, reads=[tW, tyb], writes=[tpB[i]])
                    kb.op("dve", lambda e, i=i, dc=dc: e.tensor_tensor(m1[i][:], psA[i][:, :], gt[:, dc, :], ALU.mult), reads=[tpA[i], tgt], writes=[tm1[i]])
                    kb.op("dve", lambda e, i=i, dc=dc: e.tensor_tensor(m2[i][:], psB[i][:, :], gt[:, 8 + dc, :], ALU.mult), reads=[tpB[i], tgt], writes=[tm2[i]])
                    kb.op("pool", lambda e, i=i, dc=dc: e.tensor_tensor(mT[:, dc, :], m1[i][:], m2[i][:], ALU.add), reads=[tm1[i], tm2[i]], writes=[tmT])
                for sub in range(4):
                    b = no % 2
                    no += 1
                    tok0 = tt * 512 + sub * 128
                    for hf in range(2):
                        hs = slice(hf * 512, (hf + 1) * 512)

                        def fo(e, hf=hf, hs=hs, sub=sub):
                            ins = None
                            for k in range(8):
                                ins = e.matmul(psO[hf][:, :], mT[:, k, sub * 128:(sub + 1) * 128], Wo[:, k, hs], start=(k == 0), stop=(k == 7))
                            return ins
                        kb.op("pe", fo, reads=[tW, tmT], writes=[tpO[hf]])
                        kb.op("dve", lambda e, hf=hf, hs=hs, b=b, sub=sub: e.tensor_tensor(pre[b][:, hs], psO[hf][:, :], xr[:, sub, hs], ALU.add),
                              reads=[tpO[hf], txr], writes=[tpre[b]])
                    self.layernorm(pre[b][:], tpre[b], lng[:], lnb[:], tgb, x1t[b][:], tx1[b], st, mv, tst)
                    kb.dma("sp", self.X1.ap()[tok0:tok0 + 128, :], x1t[b][:], reads=[tx1[b]])
                    if moe:
                        self.transpose_rows(None, x1t[b], tx1[b], xT, txT, tok0, psT, tpT, 0, x32=x32, tx32=tx32)
                        self.router_gates(x32, tx32, Wr, tgb, rt, trt, psA[0], tpA[0], gates[:, tok0 // 128, :], tgates)
                    else:
                        self.transpose_rows(None, x1t[b], tx1[b], xT, txT, tok0, psT, tpT, 0)
            kb.barrier()

    def router_gates(self, x32, tx32, Wr, tWr, rt, trt, ps, tps, gout, tgout):
        kb = self.kb

        def f(e):
            ins = None
            for k in range(8):
                ins = e.matmul(ps[:, 0:NE], x32[:, k, :], Wr[:, k, :], start=(k == 0), stop=(k == 7))
            return ins
        kb.op("pe", f, reads=[tx32, tWr], writes=[tps])
        lg, eq1, lg2, eq2, g1 = (rt[:, i * 8:(i + 1) * 8] for i in range(5))
        m1, m2, d_, w1, w2 = (rt[:, 40 + i:41 + i] for i in range(5))
        kb.op("act", lambda e: e.copy(lg, ps[:, 0:NE]), reads=[tps], writes=[trt])
        kb.op("dve", lambda e: e.reduce_max(m1, lg, AX.X), reads=[trt], writes=[trt])
        kb.op("dve", lambda e: e.tensor_scalar(eq1, lg, m1, None, ALU.is_equal), reads=[trt], writes=[trt])
        kb.op("dve", lambda e: e.scalar_tensor_tensor(lg2, eq1, -1e30, lg, ALU.mult, ALU.add), reads=[trt], writes=[trt])
        kb.op("dve", lambda e: e.reduce_max(m2, lg2, AX.X), reads=[trt], writes=[trt])
        kb.op("dve", lambda e: e.tensor_scalar(eq2, lg2, m2, None, ALU.is_equal), reads=[trt], writes=[trt])
        kb.op("dve", lambda e: e.tensor_tensor(d_, m2, m1, ALU.subtract), reads=[trt], writes=[trt])
        kb.op("act", lambda e: e.activation(d_, d_, AF.Exp), reads=[trt], writes=[trt])
        kb.op("dve", lambda e: e.tensor_scalar(w1, d_, 1.0, None, ALU.add), reads=[trt], writes=[trt])
        kb.op("dve", lambda e: e.reciprocal(w1, w1), reads=[trt], writes=[trt])
        kb.op("dve", lambda e: e.tensor_tensor(w2, d_, w1, ALU.mult), reads=[trt], writes=[trt])
        kb.op("dve", lambda e: e.tensor_scalar(g1, eq1, w1, None, ALU.mult), reads=[trt], writes=[trt])
        kb.op("dve", lambda e: e.scalar_tensor_tensor(gout, eq2, w2, g1, ALU.mult, ALU.add), reads=[trt], writes=[tgout])

    def phase5(self, L, s, xT, txT, gates, tgates):
        kb = self.kb
        c32 = self.c32
        moe = (L == 1)
        ne = NE if moe else 1
        dff = DEX if moe else DFF
        GW = 256
        ngr = dff // GW
        TS = 2048
        last = (L == 1)

        def wsrc(which, e_, g_):
            c0 = g_ * GW
            if moe:
                base = {"g": self.moe_g, "u": self.moe_u, "d": self.moe_d}[which].ap()[0][e_]
            else:
                base = {"g": self.ffn_g, "u": self.ffn_u, "d": self.ffn_d}[which].ap()[0]
            if which == "d":
                return base[c0:c0 + GW, :].rearrange("(k p) n -> p k n", p=128)
            return base[:, c0:c0 + GW].rearrange("(k p) n -> p k n", p=128)
        with ExitStack() as es:
            yacc = self.sb(es, "p5acc", [128, TS // 128, D], F32)
            tacc = Tok()
            stg_g = self.sb(es, "p5sg", [128, 8, GW], F32)
            stg_u = self.sb(es, "p5su", [128, 8, GW], F32)
            stg_d = self.sb(es, "p5sd", [128, GW // 128, D], F32)
            tsg, tsu, tsd = toks(3)
            Wg = [self.sb(es, f"p5wg{i}", [128, 8, GW], BF16) for i in range(2)]
            Wu = [self.sb(es, f"p5wu{i}", [128, 8, GW], BF16) for i in range(2)]
            Wd = [self.sb(es, f"p5wd{i}", [128, GW // 128, D], BF16) for i in range(2)]
            tWg, tWu, tWd = toks(2), toks(2), toks(2)
            hT = [self.sb(es, f"p5h{i}", [128, GW // 128, 512], BF16) for i in range(2)]
            thT = toks(2)
            sg = [self.sb(es, f"p5s{i}", [128, 512], BF16) for i in range(2)]
            tsg_ = toks(2)
            tmp = [self.sb(es, f"p5t{i}", [128, 512], F32) for i in range(2)]
            ttmp = toks(2)
            lng = self.sb(es, "p5lng", [128, D], F32)
            lnb = self.sb(es, "p5lnb", [128, D], F32)
            tgb = Tok()
            kb.dma("sp", lng[:], self.bc_ap(self.ln2_g, L * D, D), writes=[tgb])
            kb.dma("sp", lnb[:], self.bc_ap(self.ln2_b, L * D, D), writes=[tgb])
            x1 = [self.sb(es, f"p5x1{i}", [128, D], F32) for i in range(1)] * 2
            tx1 = toks(1) * 2
            st = self.sb(es, "p5st", [128, 12], F32)
            mv = self.sb(es, "p5mv", [128, 4], F32)
            tst = Tok()
            psG = [self.ps(es, f"p5pG{i}") for i in range(2)]
            psU = [self.ps(es, f"p5pU{i}") for i in range(2)]
            psY = [self.ps(es, f"p5pY{i}") for i in range(4)]
            tpG, tpU, tpY = toks(2), toks(2), toks(4)
            ngc, nyc, nhc = 0, 0, 0
            work = [(e_, g_) for e_ in range(ne) for g_ in range(ngr)]

            def w_dma(wi):
                e_, g_ = work[wi]
                kb.dma("pool", stg_g[:], wsrc("g", e_, g_), writes=[tsg])
                kb.dma("pool", stg_u[:], wsrc("u", e_, g_), writes=[tsu])
                kb.dma("pool", stg_d[:], wsrc("d", e_, g_), writes=[tsd])

            def w_cast(wi):
                b = wi % 2
                kb.op("pool", lambda e: e.tensor_copy(Wg[b][:], stg_g[:]), reads=[tsg], writes=[tWg[b]])
                kb.op("pool", lambda e: e.tensor_copy(Wu[b][:], stg_u[:]), reads=[tsu], writes=[tWu[b]])
                kb.op("pool", lambda e: e.tensor_copy(Wd[b][:], stg_d[:]), reads=[tsd], writes=[tWd[b]])
            for st_i in range(T // TS):
                kb.op("pool", lambda e: e.memset(yacc[:], 0.0), writes=[tacc])
                w_dma(0)
                w_cast(0)
                for wi, (e_, g_) in enumerate(work):
                    b = wi % 2
                    if wi + 1 < len(work):
                        w_dma(wi + 1)
                    for t4 in range(TS // 512):
                        c0 = st_i * TS + t4 * 512
                        hb = nhc % 2
                        nhc += 1
                        for fc in range(GW // 128):
                            i = ngc % 2
                            ngc += 1
                            fs = slice(fc * 128, (fc + 1) * 128)

                            def fg(e, i=i, fs=fs, c0=c0, b=b):
                                ins = None
                                for k in range(8):
                                    ins = e.matmul(psG[i][:, :], Wg[b][:, k, fs], xT[:, k, c0:c0 + 512], start=(k == 0), stop=(k == 7))
                                return ins

                            def fu(e, i=i, fs=fs, c0=c0, b=b):
                                ins = None
                                for k in range(8):
                                    ins = e.matmul(psU[i][:, :], Wu[b][:, k, fs], xT[:, k, c0:c0 + 512], start=(k == 0), stop=(k == 7))
                                return ins
                            kb.op("pe", fg, reads=[tWg[b], txT], writes=[tpG[i]])
                            kb.op("pe", fu, reads=[tWu[b], txT], writes=[tpU[i]])
                            kb.op("act", lambda e, i=i: e.activation(sg[i][:], psG[i][:, :], AF.Silu), reads=[tpG[i]], writes=[tsg_[i]])
                            kb.op("dve", lambda e, i=i, fc=fc, hb=hb: e.tensor_tensor(hT[hb][:, fc, :], sg[i][:], psU[i][:, :], ALU.mult),
                                  reads=[tsg_[i], tpU[i]], writes=[thT[hb]])
                        for sub in range(4):
                            tsub = t4 * 4 + sub
                            gtile = (c0 + sub * 128) // 128
                            for hf in range(2):
                                j = nyc % 4
                                tb = nyc % 2
                                nyc += 1
                                hs = slice(hf * 512, (hf + 1) * 512)

                                def fy(e, j=j, sub=sub, hs=hs, hb=hb, b=b):
                                    ins = None
                                    nk = GW // 128
                                    for k in range(nk):
                                        ins = e.matmul(psY[j][:, :], hT[hb][:, k, sub * 128:(sub + 1) * 128], Wd[b][:, k, hs],
                                                       start=(k == 0), stop=(k == nk - 1))
                                    return ins
                                kb.op("pe", fy, reads=[thT[hb], tWd[b]], writes=[tpY[j]])
                                if moe:
                                    kb.op("act", lambda e, j=j, tb=tb, gtile=gtile, e_=e_: e.activation(
                                        tmp[tb][:], psY[j][:, :], AF.Copy, scale=gates[:, gtile, e_:e_ + 1]),
                                        reads=[tpY[j], tgates], writes=[ttmp[tb]])
                                else:
                                    kb.op("act", lambda e, j=j, tb=tb: e.copy(tmp[tb][:], psY[j][:, :]), reads=[tpY[j]], writes=[ttmp[tb]])
                                kb.op("pool", lambda e, tb=tb, tsub=tsub, hs=hs: e.tensor_tensor(yacc[:, tsub, hs], yacc[:, tsub, hs], tmp[tb][:], ALU.add),
                                      reads=[ttmp[tb], tacc], writes=[tacc])
                    if wi + 1 < len(work):
                        w_cast(wi + 1)
                for tsub in range(TS // 128):
                    b = tsub % 2
                    tok0 = st_i * TS + tsub * 128
                    kb.dma("sp", x1[b][:], self.X1.ap()[tok0:tok0 + 128, :], writes=[tx1[b]])
                    kb.op("dve", lambda e, b=b, tsub=tsub: e.scalar_tensor_tensor(yacc[:, tsub, :], x1[b][:], ALPHA, yacc[:, tsub, :], ALU.mult, ALU.add),
                          reads=[tx1[b], tacc], writes=[tacc])
                    self.layernorm(yacc[:, tsub, :], tacc, lng[:], lnb[:], tgb, x1[b][:], tx1[b], st, mv, tst)
                    if last:
                        kb.dma("sp", self.y_out.ap()[s * T + tok0:s * T + tok0 + 128, :], x1[b][:], reads=[tx1[b]])
                    else:
                        kb.dma("sp", self.X2.ap()[tok0:tok0 + 128, :], x1[b][:], reads=[tx1[b]])
                        self.transpose_rows(None, x1[b], tx1[b], xT, txT, tok0, psG, tpG, 0)
            kb.barrier()

    def build(self):
        kb = self.kb
        with ExitStack() as es:
            self.load_consts(es)
            self.epsb = self.sb(es, "epsb", [128, 4], F32)
            kb.op("dve", lambda e: e.memset(self.epsb[:, 0:1], RMS_EPS), writes=[self.tc])
            kb.op("dve", lambda e: e.memset(self.epsb[:, 1:2], RMS_EPS * 128), writes=[self.tc])
            kb.op("dve", lambda e: e.memset(self.epsb[:, 2:3], 1.0), writes=[self.tc])
            kb.op("dve", lambda e: e.memset(self.epsb[:, 3:4], LN_EPS), writes=[self.tc])
            xT = self.sb(es, "xT", [128, 8, T], BF16)
            txT = Tok()
            gates = self.sb(es, "gates", [128, T // 128, NE], F32)
            tgates = Tok()
            kb.barrier()
            for s in range(self.nseq):
                self.phase0(s, xT, txT)
                if "XTd" in self.dbg:
                    xtd = self.scr("XTd", [128, 8, T], BF16)
                    kb.dma("sp", xtd.ap()[:, :, :], xT[:], reads=[txT])
                if self.stop_after == "p0":
                    break
                self.rope_tables(s)
                if self.stop_after == "rope":
                    break
                for L in range(2):
                    self.phase1(L, s, xT, txT)
                    if self.stop_after == "p1":
                        break
                    self.phase2(s, xT)
                    if self.stop_after == "p2":
                        break
                    self.phase3(L, s, xT)
                    if self.stop_after == "p3":
                        break
                    self.phase4(L, s, xT, txT, gates, tgates)
                    if self.stop_after == "p4":
                        break
                    self.phase5(L, s, xT, txT, gates, tgates)
                    if self.stop_after == f"p5_{L}":
                        break
                if self.stop_after:
                    break
            kb.barrier()
        return self.nc


_W_KEYS = ("w_in", "a_log", "dt_bias", "dn_norm_w", "w_branch_a", "w_branch_b", "w_out", "ln1_g", "ln1_b", "ln2_g", "ln2_b",
           "ffn_w_gate", "ffn_w_up", "ffn_w_down", "router_w", "moe_w_gate", "moe_w_up", "moe_w_down")


def core_maps(inputs, seq_groups):
    cst = make_consts()
    conv = np.ascontiguousarray(np.asarray(inputs["conv_w"], np.float32).reshape(2, 4, 24, 128).transpose(0, 3, 2, 1))
    ws = {k: np.ascontiguousarray(np.asarray(inputs[k], np.float32)) for k in _W_KEYS}
    x = np.asarray(inputs["x"], np.float32)
    pos = np.asarray(inputs["positions"], np.int32)
    maps = []
    for seqs in seq_groups:
        m = {"x": np.ascontiguousarray(x[seqs].reshape(-1, D)), "pos": np.ascontiguousarray(pos[seqs]), "cst": cst, "conv_w": conv}
        m.update(ws)
        maps.append(m)
    return maps


def kernel(**inputs):
    x = np.asarray(inputs["x"])
    B = x.shape[0]
    n_cores = 8
    per = B // n_cores
    groups = [list(range(c * per, (c + 1) * per)) for c in range(n_cores)]
    prog = Prog(per)
    nc = prog.build()
    res = run_bass_kernel_spmd(nc, core_maps(inputs, groups), core_ids=list(range(n_cores)))
    out = np.concatenate([np.asarray(r["y"], np.float32).reshape(per, T, D) for r in res.results], axis=0)
    return out
```

```python
import math
import os
from contextlib import ExitStack
import numpy as np
import concourse.bass as bass
import concourse.mybir as mybir
from concourse.bass_utils import run_bass_kernel_spmd

F32 = mybir.dt.float32
BF16 = mybir.dt.bfloat16
I32 = mybir.dt.int32
AF = mybir.ActivationFunctionType
ALU = mybir.AluOpType
AX = mybir.AxisListType

D = 1024
T = 4096
NIN = 10768
DFF = 2816
NE = 8
DEX = 3584
ALPHA = 4 ** 0.25
LN_EPS = 1e-5
RMS_EPS = 1e-6
PI = math.pi
A_PAIRS = ((128, 1), (512, 4), (2048, 16))
NDS = 8


class Tok:
    __slots__ = ("w", "r")

    def __init__(self):
        self.w = None
        self.r = {}


class KB:
    def __init__(self, nc):
        self.nc = nc
        self.E = {"pe": nc.tensor, "dve": nc.vector, "act": nc.scalar, "pool": nc.gpsimd, "sp": nc.sync}
        self.csem = {e: nc.alloc_semaphore("cs_" + e) for e in self.E}
        self.ccnt = {e: 0 for e in self.E}
        self.seen = {e: {} for e in self.E}
        self.dq = {q: [[nc.alloc_semaphore(f"ds_{q}{i}"), 0] for i in range(NDS)] for q in ("sp", "pool", "act")}
        self.dnext = {q: 0 for q in self.dq}
        self.ninst = 0

    def _sem(self, ev):
        if ev[0] == "c":
            return self.csem[ev[1]]
        return self.dq[ev[1]][ev[2]][0]

    def _wait(self, e, ev):
        key = ev[:-1]
        val = ev[-1]
        if self.seen[e].get(key, 0) >= val:
            return
        self.E[e].wait_ge(self._sem(ev), val)
        self.seen[e][key] = val
        self.ninst += 1

    def _deps(self, e, reads, writes):
        for t in reads:
            if t.w is not None:
                self._wait(e, t.w)
        for t in writes:
            if t.w is not None and not (e == "pe" and t.w[0] == "c" and t.w[1] == "pe"):
                self._wait(e, t.w)
            for k, ev in t.r.items():
                self._wait(e, ev)

    def _mark(self, ev, reads, writes):
        for t in reads:
            t.r[ev[:-1]] = ev
        for t in writes:
            t.w = ev
            t.r = {}

    def op(self, e, fn, reads=(), writes=()):
        self._deps(e, reads, writes)
        ins = fn(self.E[e])
        self.ccnt[e] += 1
        ins.then_inc(self.csem[e], 1)
        self.ninst += 1
        self._mark(("c", e, self.ccnt[e]), reads, writes)

    def dma(self, q, out, in_, reads=(), writes=()):
        self._deps(q, reads, writes)
        i = self.dnext[q]
        self.dnext[q] = (i + 1) % NDS
        slot = self.dq[q][i]
        if slot[1] > 0:
            self._wait(q, ("d", q, i, slot[1]))
        self.E[q].dma_start(out=out, in_=in_).then_inc(slot[0], 16)
        slot[1] += 16
        self.ninst += 1
        self._mark(("d", q, i, slot[1]), reads, writes)

    def barrier(self):
        for e in self.E:
            for f in self.E:
                if self.ccnt[f] > 0:
                    self._wait(e, ("c", f, self.ccnt[f]))
            for q in self.dq:
                for i, slot in enumerate(self.dq[q]):
                    if slot[1] > 0:
                        self._wait(e, ("d", q, i, slot[1]))


def ss(t0, n, d):
    return slice(t0, t0 + (n - 1) * d + 1, d)


def toks(n):
    return [Tok() for _ in range(n)]


C_ID, C_U, C_LI, C_LS, C_ONE, C_PERM, C_INVF, C_SIGN, C_MBS, C_MBT, C_N = 0, 128, 256, 384, 512, 640, 768, 769, 776, 904, 1032


def make_consts():
    c = np.zeros((128, C_N), np.float32)
    p = np.arange(128)
    c[:, C_ID:C_ID + 128] = np.eye(128)
    c[:, C_U:C_U + 128] = (p[:, None] <= p[None, :])
    c[:, C_LI:C_LI + 128] = (p[:, None] >= p[None, :])
    c[:, C_LS:C_LS + 128] = (p[:, None] > p[None, :])
    c[:, C_ONE:C_ONE + 128] = 1.0
    pm = np.zeros((32, 32), np.float32)
    for m in range(32):
        pm[(m + 16) % 32, m] = 1.0
    c[:32, C_PERM:C_PERM + 32] = pm
    invf = (500000.0 ** (-np.arange(0, 32, 2, dtype=np.float32) / 32)).astype(np.float32)
    c[:32, C_INVF] = np.concatenate([invf, invf])
    c[:16, C_SIGN] = -1.0
    c[16:32, C_SIGN] = 1.0
    c[:, C_MBS:C_MBS + 128] = 30000.0 * (1.0 - (p[:, None] > p[None, :]))
    c[:, C_MBT:C_MBT + 128] = -30000.0 * (1.0 - (p[:, None] <= p[None, :]))
    return c


class Prog:
    def __init__(self, nseq, dbg=None, stop_after=None):
        self.nseq = nseq
        self.dbg = dbg or ()
        self.stop_after = stop_after
        nc = bass.Bass("TRN2", target_bir_lowering=False)
        self.nc = nc
        self.kb = KB(nc)
        NT = nseq * T
        self.NT = NT
        dt = nc.dram_tensor
        I = "ExternalInput"
        self.x_in = dt("x", [NT, D], F32, kind=I)
        self.pos = dt("pos", [nseq, T], I32, kind=I)
        self.cst = dt("cst", [128, C_N], F32, kind=I)
        self.w_in = dt("w_in", [2, D, NIN], F32, kind=I)
        self.conv_w = dt("conv_w", [2, 128, 24, 4], F32, kind=I)
        self.a_log = dt("a_log", [2, 8], F32, kind=I)
        self.dt_bias = dt("dt_bias", [2, 8], F32, kind=I)
        self.dn_norm_w = dt("dn_norm_w", [2, 128], F32, kind=I)
        self.w_a = dt("w_branch_a", [2, 512, D], F32, kind=I)
        self.w_b = dt("w_branch_b", [2, D, D], F32, kind=I)
        self.w_o = dt("w_out", [2, D, D], F32, kind=I)
        self.ln1_g = dt("ln1_g", [2, D], F32, kind=I)
        self.ln1_b = dt("ln1_b", [2, D], F32, kind=I)
        self.ln2_g = dt("ln2_g", [2, D], F32, kind=I)
        self.ln2_b = dt("ln2_b", [2, D], F32, kind=I)
        self.ffn_g = dt("ffn_w_gate", [1, D, DFF], F32, kind=I)
        self.ffn_u = dt("ffn_w_up", [1, D, DFF], F32, kind=I)
        self.ffn_d = dt("ffn_w_down", [1, DFF, D], F32, kind=I)
        self.router = dt("router_w", [1, D, NE], F32, kind=I)
        self.moe_g = dt("moe_w_gate", [1, NE, D, DEX], F32, kind=I)
        self.moe_u = dt("moe_w_up", [1, NE, D, DEX], F32, kind=I)
        self.moe_d = dt("moe_w_down", [1, NE, DEX, D], F32, kind=I)
        self.y_out = dt("y", [NT, D], F32, kind="ExternalOutput")
        self.QKT = self.scr("QKT", [24, 128, T], BF16)
        self.VA = self.scr("VA", [3, 4, 128, 32, 128], BF16)
        self.GQT = self.scr("GQT", [24, 128, T], BF16)
        self.ZT = self.scr("ZT", [8, 128, T], BF16)
        self.GT = self.scr("GT", [16, 128, T], BF16)
        self.BG = self.scr("BG", [T, 16], F32)
        self.YAT = self.scr("YAT", [4, 128, T], BF16)
        self.YBT = self.scr("YBT", [8, 128, T], BF16)
        self.X1 = self.scr("X1", [T, D], F32)
        self.X2 = self.scr("X2", [T, D], F32)
        self.GATES = self.scr("GATES", [T, NE], F32)
        self.CSd = self.scr("CSd", [32, 2, T], F32)

    def scr(self, name, shape, dtype):
        kind = "ExternalOutput" if name in self.dbg else "Internal"
        return self.nc.dram_tensor(name, shape, dtype, kind=kind)

    def sb(self, es, name, shape, dtype):
        self.uid = getattr(self, "uid", 0) + 1
        return es.enter_context(self.nc.sbuf_tensor(f"{name}_{self.uid}", shape, dtype))

    def ps(self, es, name, shape=(128, 512), dtype=F32):
        self.uid = getattr(self, "uid", 0) + 1
        return es.enter_context(self.nc.psum_tensor(f"{name}_{self.uid}", list(shape), dtype))

    def bc_ap(self, handle, offset, n, parts=128):
        return bass.AP(handle, offset, [[0, parts], [1, n]])

    def wload(self, stage, tstage, dst, tdst, src, q="pool"):
        kb = self.kb
        kb.dma(q, stage, src, writes=[tstage])
        kb.op("pool", lambda e: e.tensor_copy(dst, stage), reads=[tstage], writes=[tdst])

    def load_consts(self, es):
        kb = self.kb
        self.c32 = self.sb(es, "c32", [128, C_N], F32)
        self.cbf = self.sb(es, "cbf", [128, C_N], BF16)
        self.tc = Tok()
        kb.dma("sp", self.c32[:], self.cst.ap()[:, :], writes=[self.tc])
        kb.op("dve", lambda e: e.tensor_copy(self.cbf[:], self.c32[:]), reads=[self.tc], writes=[self.tc])

    def transpose_rows(self, es_tag, src_sb, tsrc, xT, txT, col0, psT, tps, idx, x32=None, tx32=None):
        kb = self.kb
        ident = self.c32[:, C_ID:C_ID + 128]
        for hf in range(2):
            p = psT[(2 * idx + hf) % len(psT)]
            tp = tps[(2 * idx + hf) % len(psT)]

            def f(e, hf=hf, p=p):
                ins = None
                for j in range(4):
                    c = hf * 4 + j
                    ins = e.transpose(p[:, j * 128:(j + 1) * 128], src_sb[:, c * 128:(c + 1) * 128], ident)
                return ins
            kb.op("pe", f, reads=[tsrc, self.tc], writes=[tp])
            pv = p[:, :].rearrange("p (j t) -> p j t", j=4)
            if x32 is None:
                kb.op("act", lambda e, hf=hf, pv=pv: e.copy(xT[:, hf * 4:hf * 4 + 4, col0:col0 + 128], pv),
                      reads=[tp], writes=[txT])
            else:
                kb.op("act", lambda e, hf=hf, pv=pv: e.copy(x32[:, hf * 4:hf * 4 + 4, :], pv), reads=[tp], writes=[tx32])
                kb.op("dve", lambda e, hf=hf: e.tensor_copy(xT[:, hf * 4:hf * 4 + 4, col0:col0 + 128], x32[:, hf * 4:hf * 4 + 4, :]),
                      reads=[tx32], writes=[txT])

    def phase0(self, s, xT, txT):
        kb = self.kb
        with ExitStack() as es:
            xin = [self.sb(es, f"p0x{i}", [128, D], F32) for i in range(2)]
            tx = toks(2)
            psT = [self.ps(es, f"p0ps{i}") for i in range(4)]
            tps = toks(4)
            for t in range(T // 128):
                b = t % 2
                r0 = s * T + t * 128
                kb.dma("sp", xin[b][:], self.x_in.ap()[r0:r0 + 128, :], writes=[tx[b]])
                self.transpose_rows(None, xin[b], tx[b], xT, txT, t * 128, psT, tps, t)
            kb.barrier()

    def rope_tables(self, s):
        kb = self.kb
        with ExitStack() as es:
            pi_ = self.sb(es, "rp_i", [32, T], I32)
            ang = self.sb(es, "rp_a", [32, T], F32)
            kf = self.sb(es, "rp_k", [32, T], F32)
            ki = self.sb(es, "rp_ki", [32, T], I32)
            u = self.sb(es, "rp_u", [32, T], F32)
            cr = self.sb(es, "rp_c", [32, T], F32)
            CS = self.sb(es, "rp_CS", [32, 2, T], F32)
            tCS = Tok()
            t1 = Tok()
            kb.dma("sp", pi_[:], self.bc_ap(self.pos, s * T, T, 32), writes=[t1])
            kb.op("dve", lambda e: e.tensor_copy(ang[:], pi_[:]), reads=[t1], writes=[t1])
            kb.op("dve", lambda e: e.tensor_scalar(ang[:], ang[:], self.c32[0:32, C_INVF:C_INVF + 1], None, ALU.mult),
                  reads=[t1, self.tc], writes=[t1])
            t2 = Tok()
            for which in range(2):
                sh = PI / 2 if which == 0 else 0.0
                kb.op("dve", lambda e: e.tensor_scalar(kf[:], ang[:], 1.0 / (2 * PI), sh / (2 * PI), ALU.mult, ALU.add),
                      reads=[t1], writes=[t2])
                kb.op("dve", lambda e: e.tensor_copy(ki[:], kf[:]), reads=[t2], writes=[t2])
                kb.op("dve", lambda e: e.tensor_copy(kf[:], ki[:]), reads=[t2], writes=[t2])
                kb.op("dve", lambda e: e.scalar_tensor_tensor(u[:], kf[:], -2 * PI, ang[:], ALU.mult, ALU.add),
                      reads=[t2, t1], writes=[t2])
                if sh != 0.0:
                    kb.op("dve", lambda e: e.tensor_scalar(u[:], u[:], sh, None, ALU.add), reads=[t2], writes=[t2])
                kb.op("dve", lambda e: e.tensor_scalar(cr[:], u[:], PI, -2 * PI, ALU.is_gt, ALU.mult), reads=[t2], writes=[t2])
                kb.op("dve", lambda e: e.tensor_tensor(u[:], u[:], cr[:], ALU.add), reads=[t2], writes=[t2])
                kb.op("dve", lambda e: e.tensor_scalar(cr[:], u[:], -PI, 2 * PI, ALU.is_lt, ALU.mult), reads=[t2], writes=[t2])
                kb.op("dve", lambda e: e.tensor_tensor(u[:], u[:], cr[:], ALU.add), reads=[t2], writes=[t2])
                kb.op("dve", lambda e: e.tensor_scalar(u[:], u[:], PI, -PI, ALU.min, ALU.max), reads=[t2], writes=[t2])
                if which == 0:
                    kb.op("act", lambda e: e.activation(CS[0:32, 0, :], u[:], AF.Sin), reads=[t2], writes=[tCS])
                else:
                    kb.op("act", lambda e: e.activation(u[:], u[:], AF.Sin), reads=[t2], writes=[t2])
                    kb.op("dve", lambda e: e.tensor_scalar(CS[0:32, 1, :], u[:], self.c32[0:32, C_SIGN:C_SIGN + 1], None, ALU.mult),
                          reads=[t2, self.tc], writes=[tCS])
            kb.dma("sp", self.CSd.ap()[:, :, :], CS[:], reads=[tCS])
            kb.barrier()

    def phase1(self, L, s, xT, txT):
        kb = self.kb
        nc = self.nc
        win = self.w_in.ap()[L]
        NTT = T // 512
        with ExitStack() as es:
            wq = [self.sb(es, f"p1w{i}", [128, 8, 128], BF16) for i in range(2)]
            twq = toks(2)
            stg = [self.sb(es, f"p1s{i}", [128, T], BF16) for i in range(2)]
            tst = toks(2)
            raw = self.sb(es, "p1raw", [128, T + 3], F32)
            traw = Tok()
            acc = self.sb(es, "p1acc", [128, T], F32)
            tacc = Tok()
            h32 = [self.sb(es, f"p1h{i}", [128, 512], F32) for i in range(2)]
            th32 = toks(2)
            r1 = [self.sb(es, f"p1r{i}", [32, 512], F32) for i in range(2)]
            tr1 = toks(2)
            cw = self.sb(es, "p1cw", [128, 24, 4], F32)
            tcw = Tok()
            ps = [self.ps(es, f"p1ps{i}") for i in range(4)]
            tps = toks(4)
            ps2 = [self.ps(es, f"p1pq{i}") for i in range(2)]
            tps2 = toks(2)
            nps = [0]
            CS = self.sb(es, "p1CS", [32, 2, T], F32)
            tCS = Tok()
            kb.dma("sp", CS[:], self.CSd.ap()[:, :, :], writes=[tCS])

            kb.dma("sp", cw[:], self.conv_w.ap()[L], writes=[tcw])
            kb.op("dve", lambda e: e.memset(raw[:, 0:3], 0.0), writes=[traw])

            wst = [self.sb(es, f"p1wst{i}", [128, 8, 128], F32) for i in range(2)]
            twst = toks(2)
            wbig = self.sb(es, "p1wbig", [128, 8, 512], F32)
            twbig = Tok()
            wcols = [c * 128 for c in range(24)] + [4608 + c * 128 for c in range(24)] + \
                    [7680 + c * 128 for c in range(8)] + [8720 + c * 128 for c in range(16)]

            def w_dma(ci):
                b = ci % 2
                kb.dma("pool", wst[b][:], win[:, wcols[ci]:wcols[ci] + 128].rearrange("(k p) n -> p k n", p=128), writes=[twst[b]])

            def load_w(ci, col0):
                assert wcols[ci] == col0
                b = ci % 2
                if ci % 24 == 0:
                    w_dma(ci)
                if ci + 1 < len(wcols) and (ci + 1) % 24 != 0:
                    w_dma(ci + 1)
                kb.op("pool", lambda e: e.tensor_copy(wq[b][:], wst[b][:]), reads=[twst[b]], writes=[twq[b]])
                return wq[b], twq[b]

            def proj_tile(w, tw, tt, ncols=128):
                i = nps[0] % 4
                nps[0] += 1

                def f(e):
                    ins = None
                    for k in range(8):
                        ins = e.matmul(ps[i][0:ncols, :], w[:, k, 0:ncols], xT[:, k, tt * 512:(tt + 1) * 512],
                                       start=(k == 0), stop=(k == 7))
                    return ins
                kb.op("pe", f, reads=[tw, txT], writes=[tps[i]])
                return ps[i], tps[i]

            ci = 0
            SEC = os.environ.get('P1SEC', 'ABCDE')
            for c in (range(int(os.environ.get('P1NA', '24'))) if 'A' in SEC else ()):
                w, tw = load_w(ci, c * 128)
                ci += 1
                sb_ = c % 2
                for tt in range(NTT):
                    p, tp = proj_tile(w, tw, tt)
                    cols = slice(tt * 512, (tt + 1) * 512)
                    hb = (c * NTT + tt) % 2
                    kb.op("act", lambda e, p=p: e.copy(stg[sb_][:, cols], p[:, :]), reads=[tp], writes=[tst[sb_]])
                    AV = int(os.environ.get('P1AV', '3'))
                    if AV >= 2:
                        if os.environ.get('P1CP', 'act') == 'ts':
                            kb.op("dve", lambda e, p=p: e.tensor_scalar(h32[hb][:], p[:, :], 1.0, None, ALU.mult), reads=[tp], writes=[th32[hb]])
                        else:
                            kb.op("act", lambda e, p=p: e.copy(h32[hb][:], p[:, :]), reads=[tp], writes=[th32[hb]])
                        if os.environ.get('P1AW', 'c') >= 'b':
                            kb.op("dve", lambda e: e.tensor_tensor(r1[hb][:], h32[hb][0:32, :], CS[0:32, 0, cols], ALU.mult),
                                  reads=[th32[hb], tCS], writes=[tr1[hb]])
                    if AV >= 3:
                        kb.op("pe", lambda e: e.matmul(ps2[hb][:, :], self.c32[:, C_PERM:C_PERM + 128], h32[hb][:],
                                                       start=True, stop=True), reads=[th32[hb], self.tc], writes=[tps2[hb]])
                        kb.op("dve", lambda e: e.tensor_tensor(h32[hb][0:32, :], ps2[hb][0:32, :], CS[0:32, 1, cols], ALU.mult),
                              reads=[tps2[hb], tCS], writes=[th32[hb]])
                    if AV >= 2 and os.environ.get('P1AW', 'c') >= 'c':
                        kb.op("dve", lambda e: e.tensor_tensor(stg[sb_][0:32, cols], r1[hb][:], h32[hb][0:32, :], ALU.add),
                              reads=[tr1[hb], th32[hb]], writes=[tst[sb_]])
                kb.dma("sp", self.QKT.ap()[c], stg[sb_][:], reads=[tst[sb_]])
            wv = self.sb(es, "p1wv", [128, 8, 512], BF16)
            twv = Tok()
            vst = [self.sb(es, f"p1vs{i}", [128, 512], BF16) for i in range(2)]
            tvs = toks(2)
            nb_ = 0
            for g, (win_, dil) in (enumerate(A_PAIRS) if 'B' in SEC else ()):
                self.wload(wbig[:], twbig, wv[:], twv, win[:, 3072 + g * 512:3072 + (g + 1) * 512].rearrange("(k p) n -> p k n", p=128), q="sp")
                nblk = 32 // dil
                for r in range(dil):
                    for n in range(nblk):
                        blk = r * nblk + n
                        t0 = 128 * n * dil + r
                        i = nps[0] % 4
                        nps[0] += 1

                        def f(e, i=i, t0=t0, dil=dil):
                            ins = None
                            for k in range(8):
                                ins = e.matmul(ps[i][:, :], xT[:, k, ss(t0, 128, dil)], wv[:, k, :],
                                               start=(k == 0), stop=(k == 7))
                            return ins
                        kb.op("pe", f, reads=[twv, txT], writes=[tps[i]])
                        vb = nb_ % 2
                        nb_ += 1
                        kb.op("act", lambda e, i=i, vb=vb: e.copy(vst[vb][:], ps[i][:, :]), reads=[tps[i]], writes=[tvs[vb]])
                        kb.dma("sp", self.VA.ap()[g, :, :, blk, :].rearrange("h p e -> p h e"),
                               vst[vb][:, :].rearrange("p (h e) -> p h e", h=4), reads=[tvs[vb]])
            ci = 24
            for c in (range(24) if 'C' in SEC else ()):
                w, tw = load_w(ci, 4608 + c * 128)
                ci += 1
                sb_ = c % 2
                for tt in range(NTT):
                    p, tp = proj_tile(w, tw, tt)
                    kb.op("act", lambda e, p=p, tt=tt: e.copy(raw[:, 3 + tt * 512:3 + (tt + 1) * 512], p[:, :]),
                          reads=[tp], writes=[traw])
                for hh in range(2):
                    cs = slice(hh * 2048, (hh + 1) * 2048)
                    kb.op("dve", lambda e, cs=cs, hh=hh: e.tensor_scalar(acc[:, cs], raw[:, hh * 2048:hh * 2048 + 2048],
                                                                          cw[:, c, 0:1], None, ALU.mult),
                          reads=[traw, tcw], writes=[tacc])
                    for j in range(1, 4):
                        kb.op("dve", lambda e, cs=cs, hh=hh, j=j: e.scalar_tensor_tensor(
                            acc[:, cs], raw[:, hh * 2048 + j:hh * 2048 + j + 2048], cw[:, c, j:j + 1], acc[:, cs],
                            ALU.mult, ALU.add), reads=[traw, tcw, tacc], writes=[tacc])
                if c >= 16:
                    kb.op("act", lambda e: e.activation(stg[sb_][:], acc[:], AF.Silu), reads=[tacc], writes=[tst[sb_]])
                else:
                    kb.op("act", lambda e: e.activation(acc[:], acc[:], AF.Silu), reads=[tacc], writes=[tacc])
                    for tt in range(NTT):
                        cols = slice(tt * 512, (tt + 1) * 512)
                        hb = tt % 2
                        sq = raw
                        kb.op("dve", lambda e, cols=cols: e.tensor_tensor(raw[:, cols], acc[:, cols], acc[:, cols], ALU.mult),
                              reads=[tacc], writes=[traw])
                        i = nps[0] % 4
                        nps[0] += 1
                        kb.op("pe", lambda e, i=i, cols=cols: e.matmul(ps[i][:, :], self.c32[:, C_ONE:C_ONE + 128], raw[:, cols],
                                                                       start=True, stop=True),
                              reads=[traw, self.tc], writes=[tps[i]])
                        sc = (1.0 / 128) ** 0.5 if c < 8 else 1.0
                        kb.op("act", lambda e, i=i, cols=cols, sc=sc: e.activation(raw[:, cols], ps[i][:, :], AF.Sqrt,
                                                                                    bias=self.epsb[:, 0:1] if sc == 1.0 else self.epsb[:, 1:2],
                                                                                    scale=1.0 / (sc * sc)),
                              reads=[tps[i], self.tc], writes=[traw])
                        kb.op("dve", lambda e, cols=cols: e.reciprocal(raw[:, cols], raw[:, cols]), reads=[traw], writes=[traw])
                        kb.op("dve", lambda e, cols=cols: e.tensor_tensor(stg[sb_][:, cols], acc[:, cols], raw[:, cols], ALU.mult),
                              reads=[traw, tacc], writes=[tst[sb_]])
                    kb.op("dve", lambda e: e.memset(raw[:, 0:3], 0.0), reads=[traw], writes=[traw])
                kb.dma("sp", self.GQT.ap()[c], stg[sb_][:], reads=[tst[sb_]])
            ci = 48
            for c in (range(24) if 'D' in SEC else ()):
                col0 = 7680 + c * 128 if c < 8 else 8720 + (c - 8) * 128
                w, tw = load_w(ci, col0)
                ci += 1
                sb_ = c % 2
                fn = AF.Silu if c < 8 else AF.Sigmoid
                for tt in range(NTT):
                    p, tp = proj_tile(w, tw, tt)
                    kb.op("act", lambda e, p=p, tt=tt, fn=fn: e.activation(stg[sb_][:, tt * 512:(tt + 1) * 512], p[:, :], fn),
                          reads=[tp], writes=[tst[sb_]])
                dst = self.ZT.ap()[c] if c < 8 else self.GT.ap()[c - 8]
                kb.dma("sp", dst, stg[sb_][:], reads=[tst[sb_]])
            wbd = self.sb(es, "p1wbd", [128, 8, 16], BF16)
            twbd = Tok()
            self.wload(wbig[:, :, 0:16], twbig, wbd[:], twbd, win[:, 8704:8720].rearrange("(k p) n -> p k n", p=128), q="sp")
            rows = self.sb(es, "p1rows", [128, 16], F32)
            trows = Tok()
            kb.dma("sp", rows[:, 0:8], self.bc_ap(self.dt_bias, L * 8, 8), writes=[trows])
            kb.dma("sp", rows[:, 8:16], self.bc_ap(self.a_log, L * 8, 8), writes=[trows])
            kb.op("act", lambda e: e.activation(rows[:, 8:16], rows[:, 8:16], AF.Exp), reads=[trows], writes=[trows])
            bg = self.sb(es, "p1bg", [128, 32, 16], F32)
            tbg = Tok()
            tmp = self.sb(es, "p1tmp", [128, 8], F32)
            ttmp = Tok()
            for t in (range(32) if 'E' in SEC else ()):
                i = nps[0] % 4
                nps[0] += 1

                def f(e, i=i, t=t):
                    ins = None
                    for k in range(8):
                        ins = e.matmul(ps[i][:, 0:16], xT[:, k, t * 128:(t + 1) * 128], wbd[:, k, :], start=(k == 0), stop=(k == 7))
                    return ins
                kb.op("pe", f, reads=[twbd, txT], writes=[tps[i]])
                kb.op("act", lambda e, i=i, t=t: e.activation(bg[:, t, 0:8], ps[i][:, 0:8], AF.Sigmoid), reads=[tps[i]], writes=[tbg])
                kb.op("dve", lambda e, i=i: e.tensor_tensor(tmp[:], ps[i][:, 8:16], rows[:, 0:8], ALU.add),
                      reads=[tps[i], trows], writes=[ttmp])
                kb.op("act", lambda e: e.activation(tmp[:], tmp[:], AF.Exp), reads=[ttmp], writes=[ttmp])
                kb.op("act", lambda e: e.activation(tmp[:], tmp[:], AF.Ln, bias=self.epsb[:, 2:3]), reads=[ttmp, self.tc], writes=[ttmp])
                kb.op("dve", lambda e, t=t: e.scalar_tensor_tensor(bg[:, t, 8:16], tmp[:], -1.0, rows[:, 8:16], ALU.mult, ALU.mult),
                      reads=[ttmp, trows], writes=[tbg])
            kb.dma("sp", self.BG.ap().rearrange("(t p) c -> p t c", p=128), bg[:], reads=[tbg])
            kb.barrier()


    def phase2(self, s, xT):
        kb = self.kb
        scale = 128.0 ** -0.5
        with ExitStack() as es:
            QT = [xT[:, g, :] for g in range(3)]
            KT = [xT[:, 3 + g, :] for g in range(3)]
            VV = [self.sb(es, f"p2v{g}", [128, 32, 128], BF16) for g in range(3)]
            tq, tk, tv = toks(3), toks(3), toks(3)
            num = self.sb(es, "p2num", [128, T], F32)
            den = self.sb(es, "p2den", [128, T], F32)
            tnum, tden = Tok(), Tok()
            PT = [self.sb(es, f"p2pt{i}", [128, 256], BF16) for i in range(2)]
            tpt = toks(2)
            yst = self.sb(es, "p2y", [128, T], BF16)
            tyst = Tok()
            psS = [self.ps(es, f"p2pS{i}") for i in range(2)]
            psN = [self.ps(es, f"p2pN{i}") for i in range(2)]
            psD = [self.ps(es, f"p2pD{i}") for i in range(2)]
            tS, tN, tD = toks(2), toks(2), toks(2)
            ones_bf = self.cbf[:, C_ONE:C_ONE + 128]
            maskcat = self.cbf[:, C_U:C_U + 256]
            kbi = 0
            for slot in range(4):
                for g in range(3):
                    kb.dma("sp", QT[g], self.QKT.ap()[g * 4 + slot], writes=[tq[g]])
                    kb.dma("sp", KT[g], self.QKT.ap()[12 + g * 4 + slot], writes=[tk[g]])
                    kb.dma("sp", VV[g][:], self.VA.ap()[g, slot], writes=[tv[g]])
                kb.op("dve", lambda e: e.memset(num[:], 0.0), writes=[tnum])
                kb.op("dve", lambda e: e.memset(den[:], 0.0), writes=[tden])
                for g, (win_, dil) in enumerate(A_PAIRS):
                    nblk = 32 // dil
                    for r in range(dil):
                        for n in range(nblk):
                            blk = r * nblk + n
                            nq = 2 if n + 1 < nblk else 1
                            t0 = 128 * n * dil + r
                            b = kbi % 2
                            kbi += 1
                            kb.op("pe", lambda e, b=b, g=g, t0=t0, nq=nq, dil=dil: e.matmul(
                                psS[b][:, 0:128 * nq], KT[g][:, ss(t0, 128, dil)], QT[g][:, ss(t0, 128 * nq, dil)],
                                start=True, stop=True), reads=[tk[g], tq[g]], writes=[tS[b]])
                            kb.op("act", lambda e, b=b, nq=nq: e.activation(PT[b][:, 0:128 * nq], psS[b][:, 0:128 * nq], AF.Exp,
                                                                            scale=scale), reads=[tS[b]], writes=[tpt[b]])
                            kb.op("dve", lambda e, b=b, nq=nq: e.tensor_tensor(PT[b][:, 0:128 * nq], PT[b][:, 0:128 * nq],
                                                                              maskcat[:, 0:128 * nq], ALU.mult),
                                  reads=[tpt[b], self.tc], writes=[tpt[b]])
                            for mo in range(nq):
                                a = (n + mo) % 2

                                def f(e, a=a, mo=mo, b=b, g=g, blk=blk, n=n):
                                    st = (mo == 1 or n == 0)
                                    sp_ = (mo == 0)
                                    e.matmul(psN[a][:, 0:128], VV[g][:, blk, :], PT[b][:, mo * 128:(mo + 1) * 128], start=st, stop=sp_)
                                    return e.matmul(psD[a][:, 0:128], ones_bf, PT[b][:, mo * 128:(mo + 1) * 128], start=st, stop=sp_)
                                kb.op("pe", f, reads=[tv[g], tpt[b], self.tc], writes=[tN[a], tD[a]])
                            a = n % 2
                            kb.op("dve", lambda e, a=a, t0=t0, dil=dil: e.tensor_tensor(
                                num[:, ss(t0, 128, dil)], num[:, ss(t0, 128, dil)], psN[a][:, 0:128], ALU.add),
                                reads=[tN[a], tnum], writes=[tnum])
                            kb.op("dve", lambda e, a=a, t0=t0, dil=dil: e.tensor_tensor(
                                den[:, ss(t0, 128, dil)], den[:, ss(t0, 128, dil)], psD[a][:, 0:128], ALU.add),
                                reads=[tD[a], tden], writes=[tden])
                kb.op("dve", lambda e: e.reciprocal(den[:], den[:]), reads=[tden], writes=[tden])
                kb.op("dve", lambda e: e.tensor_tensor(yst[:], num[:], den[:], ALU.mult), reads=[tnum, tden], writes=[tyst])
                kb.dma("sp", self.YAT.ap()[slot], yst[:], reads=[tyst])
            kb.barrier()


    def phase3(self, L, s, xT):
        kb = self.kb
        c32, cbf = self.c32, self.cbf
        ident = c32[:, C_ID:C_ID + 128]
        ones = c32[:, C_ONE:C_ONE + 128]
        NCH = int(os.environ.get("P3NCH", "32"))
        NH = int(os.environ.get("P3NH", "8"))
        P3STOP = int(os.environ.get("P3STOP", "99"))
        with ExitStack() as es:
            KT, QT, VT, ZTs, ybst = (xT[:, i, :] for i in range(5))
            tld = toks(4)
            tyb = Tok()
            bg = self.sb(es, "p3bg", [128, 32, 16], F32)
            gc = self.sb(es, "p3gc", [128, 32, 8], F32)
            gl = self.sb(es, "p3gl", [128, 32, 8], F32)
            egc = self.sb(es, "p3egc", [128, 32, 8], F32)
            bge = self.sb(es, "p3bge", [128, 32, 8], F32)
            etl = self.sb(es, "p3etl", [128, 32, 8], F32)
            nbt = self.sb(es, "p3nbt", [128, 32, 8], F32)
            sda = self.sb(es, "p3sda", [128, 32, 8], F32)
            ngc = self.sb(es, "p3ngc", [128, 32, 8], F32)
            nwc = self.sb(es, "p3nw", [128, 1], F32)
            tsm = Tok()
            pb = [self.ps(es, f"p3ps{i}") for i in range(7)]
            psT = self.ps(es, "p3psT", (128, 512), BF16)
            tp = {k: Tok() for k in ("T", "g", "kk", "a", "b", "u", "v", "o")}
            tp["w"], tp["s"], tp["q"] = tp["u"], tp["v"], tp["o"]
            ps_g, ps_kk, ps_a, ps_b = pb[0], pb[1], pb[2], pb[3]
            ps_u, ps_w = pb[4][:, 0:128], pb[4][:, 128:256]
            ps_v, ps_s = pb[5][:, 0:128], pb[5][:, 128:256]
            ps_o, ps_q = pb[6][:, 0:128], pb[6][:, 128:256]
            kb.dma("sp", bg[:], self.BG.ap().rearrange("(t p) c -> p t c", p=128), writes=[tsm])
            kb.dma("sp", nwc[:], bass.AP(self.dn_norm_w, L * 128, [[1, 128], [1, 1]]), writes=[tsm])
            gsl = bg[:, :, 8:16]
            bsl = bg[:, :, 0:8]
            v3 = lambda ap: ap.rearrange("p (c h) -> p c h", h=8)
            kb.op("pe", lambda e: e.matmul(v3(pb[0][:, 0:256]), c32[:, C_U:C_U + 128], gsl, start=True, stop=True), reads=[tsm, self.tc], writes=[tp["g"]])
            kb.op("pe", lambda e: e.matmul(v3(pb[1][:, 0:256]), ones, gsl, start=True, stop=True), reads=[tsm, self.tc], writes=[tp["kk"]])
            kb.op("act", lambda e: e.copy(gc[:], v3(pb[0][:, 0:256])), reads=[tp["g"]], writes=[tsm])
            kb.op("act", lambda e: e.copy(gl[:], v3(pb[1][:, 0:256])), reads=[tp["kk"]], writes=[tsm])
            kb.op("act", lambda e: e.activation(egc[:], gc[:], AF.Exp), reads=[tsm], writes=[tsm])
            kb.op("act", lambda e: e.activation(sda[:], gl[:], AF.Exp), reads=[tsm], writes=[tsm])
            kb.op("dve", lambda e: e.tensor_tensor(bge[:], egc[:], bsl, ALU.mult), reads=[tsm], writes=[tsm])
            kb.op("dve", lambda e: e.tensor_tensor(etl[:], gl[:], gc[:], ALU.subtract), reads=[tsm], writes=[tsm])
            kb.op("act", lambda e: e.activation(etl[:], etl[:], AF.Exp), reads=[tsm], writes=[tsm])
            kb.op("dve", lambda e: e.tensor_scalar(nbt[:], bsl, -1.0, None, ALU.mult), reads=[tsm], writes=[tsm])
            kb.op("dve", lambda e: e.tensor_scalar(ngc[:], gc[:], -1.0, None, ALU.mult), reads=[tsm], writes=[tsm])
            kb.op("dve", lambda e: e.tensor_scalar(nwc[:], nwc[:], 128.0 ** 0.5, None, ALU.mult), reads=[tsm], writes=[tsm])

            def S(name, shape, dtype, n=1):
                return [self.sb(es, f"p3{name}{i}", shape, dtype) for i in range(n)]
            Sst, Sbf = S("S", [128, 128], F32)[0], S("Sb", [128, 128], BF16)[0]
            tS = Tok()
            Ug, dmS, dmT, eS, eT, eg = (S(n_, [128, 128], F32)[0] for n_ in ("Ug", "dmS", "dmT", "eS", "eT", "eg"))
            tUg, tdmS, tdmT, teS, teT, teg = toks(6)
            kbg, ktl, vb_ = (S(n_, [128, 128], BF16)[0] for n_ in ("kbg", "ktl", "vb"))
            tkbg, tktl, tvb = toks(3)
            PY = S("PY", [128, 256], F32, 2)
            Qm = S("Qm", [128, 128], F32, 2)
            tPY, tQ = toks(2), toks(2)
            TT, AT, wT, qdT, vnew = (S(n_, [128, 128], BF16)[0] for n_ in ("TT", "AT", "wT", "qdT", "vn"))
            tTT, tAT, twT, tqdT, tvn = toks(5)
            u_, sq, rinv, y1 = (S(n_, [128, 128], F32)[0] for n_ in ("u", "sq", "ri", "y1"))
            tu, tsq, tri, ty1 = toks(4)

            for h in range(NH):
                kb.dma("sp", KT, self.GQT.ap()[8 + h], writes=[tld[0]])
                kb.dma("sp", QT, self.GQT.ap()[h], writes=[tld[1]])
                kb.dma("sp", VT, self.GQT.ap()[16 + h], writes=[tld[2]])
                kb.dma("sp", ZTs, self.ZT.ap()[h], writes=[tld[3]])
                kb.op("dve", lambda e: e.memset(Sst[:], 0.0), writes=[tS])
                kb.op("dve", lambda e: e.memset(Sbf[:], 0.0), writes=[tS])
                for c in range(NCH):
                    cols = slice(c * 128, (c + 1) * 128)
                    col = lambda t: t[:, c, h:h + 1]
                    def ftr(e):
                        e.transpose(psT[:, 0:128], KT[:, cols], cbf[:, C_ID:C_ID + 128])
                        return e.transpose(psT[:, 128:256], VT[:, cols], cbf[:, C_ID:C_ID + 128])
                    kb.op("pe", ftr, reads=[tld[0], tld[2], self.tc], writes=[tp["T"]])
                    kb.op("act", lambda e: e.activation(kbg[:], psT[:, 0:128], AF.Copy, scale=col(bge)), reads=[tp["T"], tsm], writes=[tkbg])
                    kb.op("act", lambda e: e.activation(ktl[:], psT[:, 0:128], AF.Copy, scale=col(etl)), reads=[tp["T"], tsm], writes=[tktl])
                    kb.op("act", lambda e: e.activation(vb_[:], psT[:, 128:256], AF.Copy, scale=col(bsl)), reads=[tp["T"], tsm], writes=[tvb])
                    if P3STOP <= 1:
                        continue
                    kb.op("dve", lambda e: e.tensor_scalar(Ug[:], c32[:, C_U:C_U + 128], col(gsl), None, ALU.mult), reads=[tsm, self.tc], writes=[tUg])
                    kb.op("pe", lambda e: e.matmul(ps_g[:, 0:128], ones, Ug[:], start=True, stop=True), reads=[tUg, self.tc], writes=[tp["g"]])
                    kb.op("act", lambda e: e.activation(Ug[:], ps_g[:, 0:128], AF.Identity, bias=col(ngc), scale=1.0), reads=[tp["g"], tsm], writes=[tUg])
                    kb.op("dve", lambda e: e.tensor_tensor(dmS[:], Ug[:], c32[:, C_MBS:C_MBS + 128], ALU.max), reads=[tUg, self.tc], writes=[tdmS])
                    kb.op("dve", lambda e: e.tensor_tensor(dmT[:], Ug[:], c32[:, C_MBT:C_MBT + 128], ALU.min), reads=[tUg, self.tc], writes=[tdmT])
                    kb.op("act", lambda e: e.activation(eg[:], ps_g[:, 0:128], AF.Exp), reads=[tp["g"]], writes=[teg])
                    kb.op("act", lambda e: e.activation(eS[:], dmS[:], AF.Exp, scale=-1.0), reads=[tdmS], writes=[teS])
                    kb.op("act", lambda e: e.activation(eT[:], dmT[:], AF.Exp), reads=[tdmT], writes=[teT])
                    if P3STOP <= 2:
                        continue
                    kb.op("pe", lambda e: e.matmul(ps_kk[:, 0:128], KT[:, cols], KT[:, cols], start=True, stop=True), reads=[tld[0]], writes=[tp["kk"]])
                    kb.op("dve", lambda e: e.tensor_tensor(eS[:], ps_kk[:, 0:128], eS[:], ALU.mult), reads=[tp["kk"], teS], writes=[teS])
                    kb.op("dve", lambda e: e.tensor_scalar(Qm[0][:], eS[:], col(nbt), None, ALU.mult), reads=[tsm, teS], writes=[tQ[0]])
                    kb.op("pe", lambda e: e.transpose(ps_b[:, 0:128], Qm[0][:], ident), reads=[tQ[0], self.tc], writes=[tp["b"]])
                    kb.op("act", lambda e: e.copy(PY[0][:, 0:128], ps_b[:, 0:128]), reads=[tp["b"]], writes=[tPY[0]])
                    kb.op("dve", lambda e: e.tensor_tensor(PY[0][:, 128:256], ps_b[:, 0:128], ident, ALU.add), reads=[tp["b"], self.tc], writes=[tPY[0]])
                    if P3STOP <= 3:
                        continue
                    for k in range(7):
                        a, b = k % 2, (k + 1) % 2
                        if k == 0:
                            kb.op("pe", lambda e: e.matmul(ps_a[:, 0:128], Qm[a][:], PY[a][:, 0:128], start=True, stop=True),
                                  reads=[tQ[a], tPY[a]], writes=[tp["a"]])
                        elif k < 6:
                            kb.op("pe", lambda e: e.matmul(ps_a[:, 0:256], Qm[a][:], PY[a][:, 0:256], start=True, stop=True),
                                  reads=[tQ[a], tPY[a]], writes=[tp["a"]])
                        else:
                            kb.op("pe", lambda e: e.matmul(ps_a[:, 128:256], Qm[a][:], PY[a][:, 128:256], start=True, stop=True),
                                  reads=[tQ[a], tPY[a]], writes=[tp["a"]])
                        if k < 6:
                            kb.op("pe", lambda e: e.matmul(ps_b[:, 0:128], PY[a][:, 0:128], Qm[a][:], start=True, stop=True),
                                  reads=[tQ[a], tPY[a]], writes=[tp["b"]])
                            kb.op("act", lambda e: e.copy(PY[b][:, 0:128], ps_a[:, 0:128]), reads=[tp["a"]], writes=[tPY[b]])
                            kb.op("act", lambda e: e.copy(Qm[b][:], ps_b[:, 0:128]), reads=[tp["b"]], writes=[tQ[b]])
                            if k == 0:
                                kb.op("dve", lambda e: e.tensor_copy(PY[b][:, 128:256], PY[a][:, 128:256]), reads=[tPY[a]], writes=[tPY[b]])
                            else:
                                kb.op("dve", lambda e: e.tensor_tensor(PY[b][:, 128:256], PY[a][:, 128:256], ps_a[:, 128:256], ALU.add),
                                      reads=[tPY[a], tp["a"]], writes=[tPY[b]])
                        else:
                            kb.op("dve", lambda e: e.tensor_tensor(TT[:], PY[a][:, 128:256], ps_a[:, 128:256], ALU.add),
                                  reads=[tPY[a], tp["a"]], writes=[tTT])
                    if P3STOP <= 4:
                        continue
                    kb.op("pe", lambda e: e.matmul(ps_kk[:, 0:128], KT[:, cols], QT[:, cols], start=True, stop=True), reads=[tld[0], tld[1]], writes=[tp["kk"]])
                    kb.op("dve", lambda e: e.tensor_tensor(AT[:], ps_kk[:, 0:128], eT[:], ALU.mult), reads=[tp["kk"], teT], writes=[tAT])
                    kb.op("pe", lambda e: e.matmul(ps_u, TT[:], vb_[:], start=True, stop=True), reads=[tTT, tvb], writes=[tp["u"]])
                    kb.op("pe", lambda e: e.matmul(ps_w, kbg[:], TT[:], start=True, stop=True), reads=[tTT, tkbg], writes=[tp["w"]])
                    kb.op("act", lambda e: e.copy(u_[:], ps_u), reads=[tp["u"]], writes=[tu])
                    kb.op("act", lambda e: e.copy(wT[:], ps_w), reads=[tp["w"]], writes=[twT])
                    kb.op("dve", lambda e: e.tensor_tensor(qdT[:], QT[:, cols], eg[:], ALU.mult), reads=[tld[1], teg], writes=[tqdT])
                    if P3STOP <= 5:
                        continue
                    kb.op("pe", lambda e: e.matmul(ps_v, wT[:], Sbf[:], start=True, stop=True), reads=[twT, tS], writes=[tp["v"]])
                    kb.op("dve", lambda e: e.tensor_tensor(vnew[:], u_[:], ps_v, ALU.subtract), reads=[tu, tp["v"]], writes=[tvn])

                    def fo(e):
                        e.matmul(ps_o, Sbf[:], qdT[:], start=True, stop=False)
                        return e.matmul(ps_o, vnew[:], AT[:], start=False, stop=True)
                    kb.op("pe", fo, reads=[tS, tqdT, tvn, tAT], writes=[tp["o"]])
                    kb.op("pe", lambda e: e.matmul(ps_s, ktl[:], vnew[:], start=True, stop=True), reads=[tktl, tvn], writes=[tp["s"]])
                    kb.op("dve", lambda e: e.tensor_scalar(Sst[:], Sst[:], col(sda), None, ALU.mult), reads=[tS, tsm], writes=[tS])
                    kb.op("dve", lambda e: e.tensor_tensor(Sst[:], Sst[:], ps_s, ALU.add), reads=[tS, tp["s"]], writes=[tS])
                    kb.op("act", lambda e: e.copy(Sbf[:], Sst[:]), reads=[tS], writes=[tS])
                    if P3STOP <= 6:
                        continue
                    kb.op("act", lambda e: e.activation(sq[:], ps_o, AF.Square), reads=[tp["o"]], writes=[tsq])
                    kb.op("pe", lambda e: e.matmul(ps_q, ones, sq[:], start=True, stop=True), reads=[tsq, self.tc], writes=[tp["q"]])
                    kb.op("act", lambda e: e.activation(rinv[:], ps_q, AF.Sqrt, bias=self.epsb[:, 1:2], scale=1.0), reads=[tp["q"], self.tc], writes=[tri])
                    kb.op("dve", lambda e: e.reciprocal(rinv[:], rinv[:]), reads=[tri], writes=[tri])
                    kb.op("dve", lambda e: e.tensor_tensor(y1[:], ps_o, rinv[:], ALU.mult), reads=[tp["o"], tri], writes=[ty1])
                    kb.op("dve", lambda e: e.scalar_tensor_tensor(ybst[:, cols], y1[:], nwc[:, 0:1], ZTs[:, cols], ALU.mult, ALU.mult),
                          reads=[ty1, tsm, tld[3]], writes=[tyb])
                kb.dma("sp", self.YBT.ap()[h], ybst, reads=[tyb])
            kb.barrier()


    def layernorm(self, pre, tpre, g, b, tgb, out, tout, st, mv, tst):
        kb = self.kb

        def f(e):
            e.bn_stats(st[:, 0:6], pre[:, 0:512])
            return e.bn_stats(st[:, 6:12], pre[:, 512:1024])
        kb.op("dve", f, reads=[tpre], writes=[tst])
        kb.op("dve", lambda e: e.bn_aggr(mv[:, 0:2], st[:, 0:12]), reads=[tst], writes=[tst])
        kb.op("act", lambda e: e.activation(mv[:, 2:3], mv[:, 1:2], AF.Sqrt, bias=self.epsb[:, 3:4]), reads=[tst, self.tc], writes=[tst])
        kb.op("dve", lambda e: e.reciprocal(mv[:, 2:3], mv[:, 2:3]), reads=[tst], writes=[tst])
        kb.op("dve", lambda e: e.tensor_scalar(out, pre, mv[:, 0:1], mv[:, 2:3], ALU.subtract, ALU.mult), reads=[tpre, tst], writes=[tout])
        kb.op("dve", lambda e: e.tensor_tensor(out, out, g, ALU.mult), reads=[tgb], writes=[tout])
        kb.op("dve", lambda e: e.tensor_tensor(out, out, b, ALU.add), reads=[tgb], writes=[tout])

    def phase4(self, L, s, xT, txT, gates, tgates):
        kb = self.kb
        c32 = self.c32
        xres_src = self.x_in.ap()[s * T:(s + 1) * T, :] if L == 0 else self.X2.ap()
        moe = (L == 1)
        with ExitStack() as es:
            Wa = self.sb(es, "p4wa", [128, 4, D], BF16)
            Wb = self.sb(es, "p4wb", [128, 8, D], BF16)
            Wo = self.sb(es, "p4wo", [128, 8, D], BF16)
            tW = Tok()
            stage = self.sb(es, "p4stg", [128, 8, 512], F32)
            tstage = Tok()
            for hf in range(2):
                hs = slice(hf * 512, (hf + 1) * 512)
                self.wload(stage[:, 0:4, :], tstage, Wa[:, :, hs], tW, self.w_a.ap()[L][:, hs].rearrange("(k p) n -> p k n", p=128), q="sp")
                self.wload(stage[:], tstage, Wb[:, :, hs], tW, self.w_b.ap()[L][:, hs].rearrange("(k p) n -> p k n", p=128), q="sp")
                self.wload(stage[:], tstage, Wo[:, :, hs], tW, self.w_o.ap()[L][:, hs].rearrange("(k p) n -> p k n", p=128), q="sp")
            lng = self.sb(es, "p4lng", [128, D], F32)
            lnb = self.sb(es, "p4lnb", [128, D], F32)
            tgb = Tok()
            kb.dma("sp", lng[:], self.bc_ap(self.ln1_g, L * D, D), writes=[tgb])
            kb.dma("sp", lnb[:], self.bc_ap(self.ln1_b, L * D, D), writes=[tgb])
            if moe:
                Wr = self.sb(es, "p4wr", [128, 8, NE], F32)
                kb.dma("sp", Wr[:], self.router.ap()[0].rearrange("(k p) n -> p k n", p=128), writes=[tgb])
                x32 = self.sb(es, "p4x32", [128, 8, 128], F32)
                tx32 = Tok()
                rt = self.sb(es, "p4rt", [128, 64], F32)
                trt = Tok()
            ya = self.sb(es, "p4ya", [128, 4, 512], BF16)
            yb = self.sb(es, "p4yb", [128, 8, 512], BF16)
            gt = self.sb(es, "p4gt", [128, 16, 512], BF16)
            xr = self.sb(es, "p4xr", [128, 4, D], F32)
            tya, tyb, tgt, txr = toks(4)
            mT = self.sb(es, "p4mT", [128, 8, 512], BF16)
            tmT = Tok()
            m1 = [self.sb(es, f"p4m1{i}", [128, 512], F32) for i in range(1)] * 2
            m2 = [self.sb(es, f"p4m2{i}", [128, 512], F32) for i in range(1)] * 2
            tm1, tm2 = toks(1) * 2, toks(1) * 2
            pre = [self.sb(es, f"p4pre{i}", [128, D], F32) for i in range(1)] * 2
            x1t = [self.sb(es, f"p4x1{i}", [128, D], F32) for i in range(2)]
            tpre, tx1 = toks(1) * 2, toks(2)
            st = self.sb(es, "p4st", [128, 12], F32)
            mv = self.sb(es, "p4mv", [128, 4], F32)
            tst = Tok()
            psA = [self.ps(es, f"p4pA{i}") for i in range(2)]
            psB = [self.ps(es, f"p4pB{i}") for i in range(2)]
            psO = [self.ps(es, f"p4pO{i}") for i in range(2)]
            psT = [self.ps(es, f"p4pT{i}") for i in range(2)]
            tpA, tpB, tpO, tpT = toks(2), toks(2), toks(2), toks(2)
            no = 0
            for tt in range(T // 512):
                cs = slice(tt * 512, (tt + 1) * 512)
                kb.dma("sp", ya[:], self.YAT.ap()[:, :, cs].rearrange("k p t -> p k t"), writes=[tya])
                kb.dma("sp", yb[:], self.YBT.ap()[:, :, cs].rearrange("k p t -> p k t"), writes=[tyb])
                kb.dma("sp", gt[:], self.GT.ap()[:, :, cs].rearrange("k p t -> p k t"), writes=[tgt])
                kb.dma("sp", xr[:], xres_src[tt * 512:(tt + 1) * 512, :].rearrange("(j p) d -> p j d", p=128), writes=[txr])
                kb.op("act", lambda e: e.mul(xr[:], xr[:], ALPHA), reads=[txr], writes=[txr])
                for dc in range(8):
                    i = dc % 2
                    ds_ = slice(dc * 128, (dc + 1) * 128)

                    def fa(e, i=i, ds_=ds_):
                        ins = None
                        for k in range(4):
                            ins = e.matmul(psA[i][:, :], Wa[:, k, ds_], ya[:, k, :], start=(k == 0), stop=(k == 3))
                        return ins

                    def fb(e, i=i, ds_=ds_):
                        ins = None
                        for k in range(8):
                            ins = e.matmul(psB[i][:, :], Wb[:, k, ds_], yb[:, k, :], start=(k == 0), stop=(k == 7))
                        return ins
                    kb.op("pe", fa, reads=[tW, tya], writes=[tpA[i]])
                    kb.op("pe", fb, reads=[tW, tyb], writes=[tpB[i]])
                    kb.op("dve", lambda e, i=i, dc=dc: e.tensor_tensor(m1[i][:], psA[i][:, :], gt[:, dc, :], ALU.mult), reads=[tpA[i], tgt], writes=[tm1[i]])
                    kb.op("dve", lambda e, i=i, dc=dc: e.tensor_tensor(m2[i][:], psB[i][:, :], gt[:, 8 + dc, :], ALU.mult), reads=[tpB[i], tgt], writes=[tm2[i]])
                    kb.op("pool", lambda e, i=i, dc=dc: e.tensor_tensor(mT[:, dc, :], m1[i][:], m2[i][:], ALU.add), reads=[tm1[i], tm2[i]], writes=[tmT])
                for sub in range(4):
                    b = no % 2
                    no += 1
                    tok0 = tt * 512 + sub * 128
                    for hf in range(2):
                        hs = slice(hf * 512, (hf + 1) * 512)

                        def fo(e, hf=hf, hs=hs, sub=sub):
                            ins = None
                            for k in range(8):
                                ins = e.matmul(psO[hf][:, :], mT[:, k, sub * 128:(sub + 1) * 128], Wo[:, k, hs], start=(k == 0), stop=(k == 7))
                            return ins
                        kb.op("pe", fo, reads=[tW, tmT], writes=[tpO[hf]])
                        kb.op("dve", lambda e, hf=hf, hs=hs, b=b, sub=sub: e.tensor_tensor(pre[b][:, hs], psO[hf][:, :], xr[:, sub, hs], ALU.add),
                              reads=[tpO[hf], txr], writes=[tpre[b]])
                    self.layernorm(pre[b][:], tpre[b], lng[:], lnb[:], tgb, x1t[b][:], tx1[b], st, mv, tst)
                    kb.dma("sp", self.X1.ap()[tok0:tok0 + 128, :], x1t[b][:], reads=[tx1[b]])
                    if moe:
                        self.transpose_rows(None, x1t[b], tx1[b], xT, txT, tok0, psT, tpT, 0, x32=x32, tx32=tx32)
                        self.router_gates(x32, tx32, Wr, tgb, rt, trt, psA[0], tpA[0], gates[:, tok0 // 128, :], tgates)
                    else:
                        self.transpose_rows(None, x1t[b], tx1[b], xT, txT, tok0, psT, tpT, 0)
            kb.barrier()

    def router_gates(self, x32, tx32, Wr, tWr, rt, trt, ps, tps, gout, tgout):
        kb = self.kb

        def f(e):
            ins = None
            for k in range(8):
                ins = e.matmul(ps[:, 0:NE], x32[:, k, :], Wr[:, k, :], start=(k == 0), stop=(k == 7))
            return ins
        kb.op("pe", f, reads=[tx32, tWr], writes=[tps])
        lg, eq1, lg2, eq2, g1 = (rt[:, i * 8:(i + 1) * 8] for i in range(5))
        m1, m2, d_, w1, w2 = (rt[:, 40 + i:41 + i] for i in range(5))
        kb.op("act", lambda e: e.copy(lg, ps[:, 0:NE]), reads=[tps], writes=[trt])
        kb.op("dve", lambda e: e.reduce_max(m1, lg, AX.X), reads=[trt], writes=[trt])
        kb.op("dve", lambda e: e.tensor_scalar(eq1, lg, m1, None, ALU.is_equal), reads=[trt], writes=[trt])
        kb.op("dve", lambda e: e.scalar_tensor_tensor(lg2, eq1, -1e30, lg, ALU.mult, ALU.add), reads=[trt], writes=[trt])
        kb.op("dve", lambda e: e.reduce_max(m2, lg2, AX.X), reads=[trt], writes=[trt])
        kb.op("dve", lambda e: e.tensor_scalar(eq2, lg2, m2, None, ALU.is_equal), reads=[trt], writes=[trt])
        kb.op("dve", lambda e: e.tensor_tensor(d_, m2, m1, ALU.subtract), reads=[trt], writes=[trt])
        kb.op("act", lambda e: e.activation(d_, d_, AF.Exp), reads=[trt], writes=[trt])
        kb.op("dve", lambda e: e.tensor_scalar(w1, d_, 1.0, None, ALU.add), reads=[trt], writes=[trt])
        kb.op("dve", lambda e: e.reciprocal(w1, w1), reads=[trt], writes=[trt])
        kb.op("dve", lambda e: e.tensor_tensor(w2, d_, w1, ALU.mult), reads=[trt], writes=[trt])
        kb.op("dve", lambda e: e.tensor_scalar(g1, eq1, w1, None, ALU.mult), reads=[trt], writes=[trt])
        kb.op("dve", lambda e: e.scalar_tensor_tensor(gout, eq2, w2, g1, ALU.mult, ALU.add), reads=[trt], writes=[tgout])

    def phase5(self, L, s, xT, txT, gates, tgates):
        kb = self.kb
        c32 = self.c32
        moe = (L == 1)
        ne = NE if moe else 1
        dff = DEX if moe else DFF
        GW = 256
        ngr = dff // GW
        TS = 2048
        last = (L == 1)

        def wsrc(which, e_, g_):
            c0 = g_ * GW
            if moe:
                base = {"g": self.moe_g, "u": self.moe_u, "d": self.moe_d}[which].ap()[0][e_]
            else:
                base = {"g": self.ffn_g, "u": self.ffn_u, "d": self.ffn_d}[which].ap()[0]
            if which == "d":
                return base[c0:c0 + GW, :].rearrange("(k p) n -> p k n", p=128)
            return base[:, c0:c0 + GW].rearrange("(k p) n -> p k n", p=128)
        with ExitStack() as es:
            yacc = self.sb(es, "p5acc", [128, TS // 128, D], F32)
            taccs = toks(2 * TS // 128)
            stg_g = self.sb(es, "p5sg", [128, 8, GW], F32)
            stg_u = self.sb(es, "p5su", [128, 8, GW], F32)
            stg_d = self.sb(es, "p5sd", [128, GW // 128, D], F32)
            tsg, tsu, tsd = toks(3)
            Wg = [self.sb(es, f"p5wg{i}", [128, 8, GW], BF16) for i in range(2)]
            Wu = [self.sb(es, f"p5wu{i}", [128, 8, GW], BF16) for i in range(2)]
            Wd = [self.sb(es, f"p5wd{i}", [128, GW // 128, D], BF16) for i in range(2)]
            tWg, tWu, tWd = toks(2), toks(2), toks(2)
            hT = [self.sb(es, f"p5h{i}", [128, GW // 128, 512], BF16) for i in range(2)]
            thT = toks(2)
            sg = [self.sb(es, f"p5s{i}", [128, 512], BF16) for i in range(2)]
            tsg_ = toks(2)
            tmp = [self.sb(es, f"p5t{i}", [128, 512], F32) for i in range(2)]
            ttmp = toks(2)
            lng = self.sb(es, "p5lng", [128, D], F32)
            lnb = self.sb(es, "p5lnb", [128, D], F32)
            tgb = Tok()
            kb.dma("sp", lng[:], self.bc_ap(self.ln2_g, L * D, D), writes=[tgb])
            kb.dma("sp", lnb[:], self.bc_ap(self.ln2_b, L * D, D), writes=[tgb])
            x1 = [self.sb(es, f"p5x1{i}", [128, D], F32) for i in range(1)] * 2
            tx1 = toks(1) * 2
            st = self.sb(es, "p5st", [128, 12], F32)
            mv = self.sb(es, "p5mv", [128, 4], F32)
            tst = Tok()
            psG = [self.ps(es, f"p5pG{i}") for i in range(2)]
            psU = [self.ps(es, f"p5pU{i}") for i in range(2)]
            psY = [self.ps(es, f"p5pY{i}") for i in range(4)]
            tpG, tpU, tpY = toks(2), toks(2), toks(4)
            work = [(e_, g_) for e_ in range(ne) for g_ in range(ngr)]

            def w_dma(wi):
                e_, g_ = work[wi]
                kb.dma("sp", stg_g[:], wsrc("g", e_, g_), writes=[tsg])
                kb.dma("sp", stg_u[:], wsrc("u", e_, g_), writes=[tsu])
                kb.dma("sp", stg_d[:], wsrc("d", e_, g_), writes=[tsd])

            def w_cast(wi):
                b = wi % 2
                kb.op("act", lambda e: e.copy(Wg[b][:], stg_g[:]), reads=[tsg], writes=[tWg[b]])
                kb.op("act", lambda e: e.copy(Wu[b][:], stg_u[:]), reads=[tsu], writes=[tWu[b]])
                kb.op("act", lambda e: e.copy(Wd[b][:], stg_d[:]), reads=[tsd], writes=[tWd[b]])

            def GU(st_i, wi, t4, hb):
                b = wi % 2
                c0 = st_i * TS + t4 * 512
                for fc in range(GW // 128):
                    i = self.ngc % 2
                    self.ngc += 1
                    fs = slice(fc * 128, (fc + 1) * 128)

                    def fg(e, i=i, fs=fs):
                        ins = None
                        for k in range(8):
                            ins = e.matmul(psG[i][:, :], Wg[b][:, k, fs], xT[:, k, c0:c0 + 512], start=(k == 0), stop=(k == 7))
                        return ins

                    def fu(e, i=i, fs=fs):
                        ins = None
                        for k in range(8):
                            ins = e.matmul(psU[i][:, :], Wu[b][:, k, fs], xT[:, k, c0:c0 + 512], start=(k == 0), stop=(k == 7))
                        return ins
                    kb.op("pe", fg, reads=[tWg[b], txT], writes=[tpG[i]])
                    kb.op("pe", fu, reads=[tWu[b], txT], writes=[tpU[i]])
                    kb.op("act", lambda e, i=i: e.activation(sg[i][:], psG[i][:, :], AF.Silu), reads=[tpG[i]], writes=[tsg_[i]])
                    kb.op("dve", lambda e, i=i, fc=fc: e.tensor_tensor(hT[hb][:, fc, :], sg[i][:], psU[i][:, :], ALU.mult),
                          reads=[tsg_[i], tpU[i]], writes=[thT[hb]])

            def YD(st_i, wi, t4, hb):
                b = wi % 2
                e_, g_ = work[wi]
                c0 = st_i * TS + t4 * 512
                for sub in range(4):
                    tsub = t4 * 4 + sub
                    gtile = (c0 + sub * 128) // 128
                    for hf in range(2):
                        j = self.nyc % 4
                        tb = self.nyc % 2
                        self.nyc += 1
                        hs = slice(hf * 512, (hf + 1) * 512)

                        def fy(e, j=j, sub=sub, hs=hs):
                            ins = None
                            nk = GW // 128
                            for k in range(nk):
                                ins = e.matmul(psY[j][:, :], hT[hb][:, k, sub * 128:(sub + 1) * 128], Wd[b][:, k, hs],
                                               start=(k == 0), stop=(k == nk - 1))
                            return ins
                        kb.op("pe", fy, reads=[thT[hb], tWd[b]], writes=[tpY[j]])
                        if moe:
                            kb.op("act", lambda e, j=j, tb=tb: e.activation(tmp[tb][:], psY[j][:, :], AF.Copy, scale=gates[:, gtile, e_:e_ + 1]),
                                  reads=[tpY[j], tgates], writes=[ttmp[tb]])
                        else:
                            kb.op("act", lambda e, j=j, tb=tb: e.copy(tmp[tb][:], psY[j][:, :]), reads=[tpY[j]], writes=[ttmp[tb]])
                        tk = taccs[tsub * 2 + hf]
                        kb.op("dve", lambda e, tb=tb, tsub=tsub, hs=hs: e.tensor_tensor(yacc[:, tsub, hs], yacc[:, tsub, hs], tmp[tb][:], ALU.add),
                              reads=[ttmp[tb], tk], writes=[tk])
            self.ngc, self.nyc = 0, 0
            NT4 = TS // 512
            for st_i in range(T // TS):
                kb.op("pool", lambda e: e.memset(yacc[:], 0.0), writes=taccs)
                w_dma(0)
                w_cast(0)
                units = [(wi, t4) for wi in range(len(work)) for t4 in range(NT4)]
                GU(st_i, 0, 0, 0)
                for ui, (wi, t4) in enumerate(units):
                    if t4 == 0 and wi + 1 < len(work):
                        w_dma(wi + 1)
                    if t4 == 2 and wi + 1 < len(work):
                        w_cast(wi + 1)
                    if ui + 1 < len(units):
                        GU(st_i, units[ui + 1][0], units[ui + 1][1], (ui + 1) % 2)
                    YD(st_i, wi, t4, ui % 2)
                for tsub in range(TS // 128):
                    b = tsub % 2
                    tok0 = st_i * TS + tsub * 128
                    kb.dma("sp", x1[b][:], self.X1.ap()[tok0:tok0 + 128, :], writes=[tx1[b]])
                    tacc = taccs[tsub * 2]
                    kb.op("dve", lambda e, b=b, tsub=tsub: e.scalar_tensor_tensor(yacc[:, tsub, :], x1[b][:], ALPHA, yacc[:, tsub, :], ALU.mult, ALU.add),
                          reads=[tx1[b], taccs[tsub * 2], taccs[tsub * 2 + 1]], writes=[tacc])
                    self.layernorm(yacc[:, tsub, :], tacc, lng[:], lnb[:], tgb, x1[b][:], tx1[b], st, mv, tst)
                    if last:
                        kb.dma("sp", self.y_out.ap()[s * T + tok0:s * T + tok0 + 128, :], x1[b][:], reads=[tx1[b]])
                    else:
                        kb.dma("sp", self.X2.ap()[tok0:tok0 + 128, :], x1[b][:], reads=[tx1[b]])
                        self.transpose_rows(None, x1[b], tx1[b], xT, txT, tok0, psG, tpG, 0)
            kb.barrier()

    def build(self):
        kb = self.kb
        with ExitStack() as es:
            self.load_consts(es)
            self.epsb = self.sb(es, "epsb", [128, 4], F32)
            kb.op("dve", lambda e: e.memset(self.epsb[:, 0:1], RMS_EPS), writes=[self.tc])
            kb.op("dve", lambda e: e.memset(self.epsb[:, 1:2], RMS_EPS * 128), writes=[self.tc])
            kb.op("dve", lambda e: e.memset(self.epsb[:, 2:3], 1.0), writes=[self.tc])
            kb.op("dve", lambda e: e.memset(self.epsb[:, 3:4], LN_EPS), writes=[self.tc])
            xT = self.sb(es, "xT", [128, 8, T], BF16)
            txT = Tok()
            gates = self.sb(es, "gates", [128, T // 128, NE], F32)
            tgates = Tok()
            kb.barrier()
            only = os.environ.get("ONLY")
            for s in range(self.nseq):
                if only:
                    ph, LL = only.split(":")
                    LL = int(LL)
                    kb.op("dve", lambda e: e.memset(xT[:], 0.0), writes=[txT])
                    kb.op("dve", lambda e: e.memset(gates[:], 0.0), writes=[tgates])
                    kb.barrier()
                    {"p1": lambda: self.phase1(LL, s, xT, txT), "p2": lambda: self.phase2(s, xT), "p3": lambda: self.phase3(LL, s, xT),
                     "p4": lambda: self.phase4(LL, s, xT, txT, gates, tgates), "p5": lambda: self.phase5(LL, s, xT, txT, gates, tgates)}[ph]()
                    continue
                self.phase0(s, xT, txT)
                if "XTd" in self.dbg:
                    xtd = self.scr("XTd", [128, 8, T], BF16)
                    kb.dma("sp", xtd.ap()[:, :, :], xT[:], reads=[txT])
                if self.stop_after == "p0":
                    break
                self.rope_tables(s)
                if self.stop_after == "rope":
                    break
                for L in range(2):
                    self.phase1(L, s, xT, txT)
                    if self.stop_after == "p1":
                        break
                    self.phase2(s, xT)
                    if self.stop_after == "p2":
                        break
                    self.phase3(L, s, xT)
                    if self.stop_after == "p3":
                        break
                    self.phase4(L, s, xT, txT, gates, tgates)
                    if self.stop_after == "p4":
                        break
                    self.phase5(L, s, xT, txT, gates, tgates)
                    if self.stop_after == f"p5_{L}":
                        break
                if self.stop_after:
                    break
            kb.barrier()
        return self.nc


_W_KEYS = ("w_in", "a_log", "dt_bias", "dn_norm_w", "w_branch_a", "w_branch_b", "w_out", "ln1_g", "ln1_b", "ln2_g", "ln2_b",
           "ffn_w_gate", "ffn_w_up", "ffn_w_down", "router_w", "moe_w_gate", "moe_w_up", "moe_w_down")


def core_maps(inputs, seq_groups):
    cst = make_consts()
    conv = np.ascontiguousarray(np.asarray(inputs["conv_w"], np.float32).reshape(2, 4, 24, 128).transpose(0, 3, 2, 1))
    ws = {k: np.ascontiguousarray(np.asarray(inputs[k], np.float32)) for k in _W_KEYS}
    x = np.asarray(inputs["x"], np.float32)
    pos = np.asarray(inputs["positions"], np.int32)
    maps = []
    for seqs in seq_groups:
        m = {"x": np.ascontiguousarray(x[seqs].reshape(-1, D)), "pos": np.ascontiguousarray(pos[seqs]), "cst": cst, "conv_w": conv}
        m.update(ws)
        maps.append(m)
    return maps


def kernel(**inputs):
    x = np.asarray(inputs["x"])
    B = x.shape[0]
    n_cores = 8
    per = B // n_cores
    groups = [list(range(c * per, (c + 1) * per)) for c in range(n_cores)]
    prog = Prog(per)
    nc = prog.build()
    res = run_bass_kernel_spmd(nc, core_maps(inputs, groups), core_ids=list(range(n_cores)))
    out = np.concatenate([np.asarray(r["y"], np.float32).reshape(per, T, D) for r in res.results], axis=0)
    return out
```

```python
import math
import os
from contextlib import ExitStack
import numpy as np
import concourse.bass as bass
import concourse.mybir as mybir
from concourse.bass_utils import run_bass_kernel_spmd

F32 = mybir.dt.float32
BF16 = mybir.dt.bfloat16
I32 = mybir.dt.int32
AF = mybir.ActivationFunctionType
ALU = mybir.AluOpType
AX = mybir.AxisListType

D = 1024
T = 4096
NIN = 10768
DFF = 2816
NE = 8
DEX = 3584
ALPHA = 4 ** 0.25
LN_EPS = 1e-5
RMS_EPS = 1e-6
PI = math.pi
A_PAIRS = ((128, 1), (512, 4), (2048, 16))
NDS = 8


class Tok:
    __slots__ = ("w", "r")

    def __init__(self):
        self.w = None
        self.r = {}


class KB:
    def __init__(self, nc):
        self.nc = nc
        self.E = {"pe": nc.tensor, "dve": nc.vector, "act": nc.scalar, "pool": nc.gpsimd, "sp": nc.sync}
        self.csem = {e: nc.alloc_semaphore("cs_" + e) for e in self.E}
        self.ccnt = {e: 0 for e in self.E}
        self.seen = {e: {} for e in self.E}
        self.dq = {q: [[nc.alloc_semaphore(f"ds_{q}{i}"), 0] for i in range(NDS)] for q in ("sp", "pool", "act")}
        self.dnext = {q: 0 for q in self.dq}
        self.ninst = 0

    def _sem(self, ev):
        if ev[0] == "c":
            return self.csem[ev[1]]
        return self.dq[ev[1]][ev[2]][0]

    def _wait(self, e, ev):
        key = ev[:-1]
        val = ev[-1]
        if self.seen[e].get(key, 0) >= val:
            return
        self.E[e].wait_ge(self._sem(ev), val)
        self.seen[e][key] = val
        self.ninst += 1

    def _deps(self, e, reads, writes):
        for t in reads:
            if t.w is not None:
                self._wait(e, t.w)
        for t in writes:
            if t.w is not None and not (e == "pe" and t.w[0] == "c" and t.w[1] == "pe"):
                self._wait(e, t.w)
            for k, ev in t.r.items():
                self._wait(e, ev)

    def _mark(self, ev, reads, writes):
        for t in reads:
            t.r[ev[:-1]] = ev
        for t in writes:
            t.w = ev
            t.r = {}

    def op(self, e, fn, reads=(), writes=()):
        self._deps(e, reads, writes)
        ins = fn(self.E[e])
        self.ccnt[e] += 1
        ins.then_inc(self.csem[e], 1)
        self.ninst += 1
        self._mark(("c", e, self.ccnt[e]), reads, writes)

    def dma(self, q, out, in_, reads=(), writes=()):
        self._deps(q, reads, writes)
        i = self.dnext[q]
        self.dnext[q] = (i + 1) % NDS
        slot = self.dq[q][i]
        if slot[1] > 0:
            self._wait(q, ("d", q, i, slot[1]))
        self.E[q].dma_start(out=out, in_=in_).then_inc(slot[0], 16)
        slot[1] += 16
        self.ninst += 1
        self._mark(("d", q, i, slot[1]), reads, writes)

    def barrier(self):
        for e in self.E:
            for f in self.E:
                if self.ccnt[f] > 0:
                    self._wait(e, ("c", f, self.ccnt[f]))
            for q in self.dq:
                for i, slot in enumerate(self.dq[q]):
                    if slot[1] > 0:
                        self._wait(e, ("d", q, i, slot[1]))


def ss(t0, n, d):
    return slice(t0, t0 + (n - 1) * d + 1, d)


def toks(n):
    return [Tok() for _ in range(n)]


C_ID, C_U, C_LI, C_LS, C_ONE, C_PERM, C_INVF, C_SIGN, C_MBS, C_MBT, C_N = 0, 128, 256, 384, 512, 640, 768, 769, 776, 904, 1032


def make_consts():
    c = np.zeros((128, C_N), np.float32)
    p = np.arange(128)
    c[:, C_ID:C_ID + 128] = np.eye(128)
    c[:, C_U:C_U + 128] = (p[:, None] <= p[None, :])
    c[:, C_LI:C_LI + 128] = (p[:, None] >= p[None, :])
    c[:, C_LS:C_LS + 128] = (p[:, None] > p[None, :])
    c[:, C_ONE:C_ONE + 128] = 1.0
    pm = np.zeros((32, 32), np.float32)
    for m in range(32):
        pm[(m + 16) % 32, m] = 1.0
    c[:32, C_PERM:C_PERM + 32] = pm
    invf = (500000.0 ** (-np.arange(0, 32, 2, dtype=np.float32) / 32)).astype(np.float32)
    c[:32, C_INVF] = np.concatenate([invf, invf])
    c[:16, C_SIGN] = -1.0
    c[16:32, C_SIGN] = 1.0
    c[:, C_MBS:C_MBS + 128] = 30000.0 * (1.0 - (p[:, None] > p[None, :]))
    c[:, C_MBT:C_MBT + 128] = -30000.0 * (1.0 - (p[:, None] <= p[None, :]))
    return c


class Prog:
    def __init__(self, nseq, dbg=None, stop_after=None):
        self.nseq = nseq
        self.dbg = dbg or ()
        self.stop_after = stop_after
        nc = bass.Bass("TRN2", target_bir_lowering=False)
        self.nc = nc
        self.kb = KB(nc)
        NT = nseq * T
        self.NT = NT
        dt = nc.dram_tensor
        I = "ExternalInput"
        self.x_in = dt("x", [NT, D], F32, kind=I)
        self.pos = dt("pos", [nseq, T], I32, kind=I)
        self.cst = dt("cst", [128, C_N], F32, kind=I)
        self.w_in = dt("w_in", [2, D, NIN], F32, kind=I)
        self.conv_w = dt("conv_w", [2, 128, 24, 4], F32, kind=I)
        self.a_log = dt("a_log", [2, 8], F32, kind=I)
        self.dt_bias = dt("dt_bias", [2, 8], F32, kind=I)
        self.dn_norm_w = dt("dn_norm_w", [2, 128], F32, kind=I)
        self.w_a = dt("w_branch_a", [2, 512, D], F32, kind=I)
        self.w_b = dt("w_branch_b", [2, D, D], F32, kind=I)
        self.w_o = dt("w_out", [2, D, D], F32, kind=I)
        self.ln1_g = dt("ln1_g", [2, D], F32, kind=I)
        self.ln1_b = dt("ln1_b", [2, D], F32, kind=I)
        self.ln2_g = dt("ln2_g", [2, D], F32, kind=I)
        self.ln2_b = dt("ln2_b", [2, D], F32, kind=I)
        self.ffn_g = dt("ffn_w_gate", [1, D, DFF], F32, kind=I)
        self.ffn_u = dt("ffn_w_up", [1, D, DFF], F32, kind=I)
        self.ffn_d = dt("ffn_w_down", [1, DFF, D], F32, kind=I)
        self.router = dt("router_w", [1, D, NE], F32, kind=I)
        self.moe_g = dt("moe_w_gate", [1, NE, D, DEX], F32, kind=I)
        self.moe_u = dt("moe_w_up", [1, NE, D, DEX], F32, kind=I)
        self.moe_d = dt("moe_w_down", [1, NE, DEX, D], F32, kind=I)
        self.y_out = dt("y", [NT, D], F32, kind="ExternalOutput")
        self.QKT = self.scr("QKT", [24, 128, T], BF16)
        self.VA = self.scr("VA", [3, 4, 128, 32, 128], BF16)
        self.GQT = self.scr("GQT", [24, 128, T], BF16)
        self.ZT = self.scr("ZT", [8, 128, T], BF16)
        self.GT = self.scr("GT", [16, 128, T], BF16)
        self.BG = self.scr("BG", [T, 16], F32)
        self.YAT = self.scr("YAT", [4, 128, T], BF16)
        self.YBT = self.scr("YBT", [8, 128, T], BF16)
        self.X1 = self.scr("X1", [T, D], F32)
        self.X2 = self.scr("X2", [T, D], F32)
        self.GATES = self.scr("GATES", [T, NE], F32)
        self.CSd = self.scr("CSd", [32, 2, T], F32)

    def scr(self, name, shape, dtype):
        kind = "ExternalOutput" if name in self.dbg else "Internal"
        return self.nc.dram_tensor(name, shape, dtype, kind=kind)

    def sb(self, es, name, shape, dtype):
        self.uid = getattr(self, "uid", 0) + 1
        return es.enter_context(self.nc.sbuf_tensor(f"{name}_{self.uid}", shape, dtype))

    def ps(self, es, name, shape=(128, 512), dtype=F32):
        self.uid = getattr(self, "uid", 0) + 1
        return es.enter_context(self.nc.psum_tensor(f"{name}_{self.uid}", list(shape), dtype))

    def bc_ap(self, handle, offset, n, parts=128):
        return bass.AP(handle, offset, [[0, parts], [1, n]])

    def wload(self, stage, tstage, dst, tdst, src, q="pool"):
        kb = self.kb
        kb.dma(q, stage, src, writes=[tstage])
        kb.op("pool", lambda e: e.tensor_copy(dst, stage), reads=[tstage], writes=[tdst])

    def load_consts(self, es):
        kb = self.kb
        self.c32 = self.sb(es, "c32", [128, C_N], F32)
        self.cbf = self.sb(es, "cbf", [128, C_N], BF16)
        self.tc = Tok()
        kb.dma("sp", self.c32[:], self.cst.ap()[:, :], writes=[self.tc])
        kb.op("dve", lambda e: e.tensor_copy(self.cbf[:], self.c32[:]), reads=[self.tc], writes=[self.tc])

    def transpose_rows(self, es_tag, src_sb, tsrc, xT, txT, col0, psT, tps, idx, x32=None, tx32=None):
        kb = self.kb
        ident = self.c32[:, C_ID:C_ID + 128]
        for hf in range(2):
            p = psT[(2 * idx + hf) % len(psT)]
            tp = tps[(2 * idx + hf) % len(psT)]

            def f(e, hf=hf, p=p):
                ins = None
                for j in range(4):
                    c = hf * 4 + j
                    ins = e.transpose(p[:, j * 128:(j + 1) * 128], src_sb[:, c * 128:(c + 1) * 128], ident)
                return ins
            kb.op("pe", f, reads=[tsrc, self.tc], writes=[tp])
            pv = p[:, :].rearrange("p (j t) -> p j t", j=4)
            if x32 is None:
                kb.op("act", lambda e, hf=hf, pv=pv: e.copy(xT[:, hf * 4:hf * 4 + 4, col0:col0 + 128], pv),
                      reads=[tp], writes=[txT])
            else:
                kb.op("act", lambda e, hf=hf, pv=pv: e.copy(x32[:, hf * 4:hf * 4 + 4, :], pv), reads=[tp], writes=[tx32])
                kb.op("dve", lambda e, hf=hf: e.tensor_copy(xT[:, hf * 4:hf * 4 + 4, col0:col0 + 128], x32[:, hf * 4:hf * 4 + 4, :]),
                      reads=[tx32], writes=[txT])

    def phase0(self, s, xT, txT):
        kb = self.kb
        with ExitStack() as es:
            xin = [self.sb(es, f"p0x{i}", [128, D], F32) for i in range(2)]
            tx = toks(2)
            psT = [self.ps(es, f"p0ps{i}") for i in range(4)]
            tps = toks(4)
            for t in range(T // 128):
                b = t % 2
                r0 = s * T + t * 128
                kb.dma("sp", xin[b][:], self.x_in.ap()[r0:r0 + 128, :], writes=[tx[b]])
                self.transpose_rows(None, xin[b], tx[b], xT, txT, t * 128, psT, tps, t)
            kb.barrier()

    def rope_tables(self, s):
        kb = self.kb
        with ExitStack() as es:
            pi_ = self.sb(es, "rp_i", [32, T], I32)
            ang = self.sb(es, "rp_a", [32, T], F32)
            kf = self.sb(es, "rp_k", [32, T], F32)
            ki = self.sb(es, "rp_ki", [32, T], I32)
            u = self.sb(es, "rp_u", [32, T], F32)
            cr = self.sb(es, "rp_c", [32, T], F32)
            CS = self.sb(es, "rp_CS", [32, 2, T], F32)
            tCS = Tok()
            t1 = Tok()
            kb.dma("sp", pi_[:], self.bc_ap(self.pos, s * T, T, 32), writes=[t1])
            kb.op("dve", lambda e: e.tensor_copy(ang[:], pi_[:]), reads=[t1], writes=[t1])
            kb.op("dve", lambda e: e.tensor_scalar(ang[:], ang[:], self.c32[0:32, C_INVF:C_INVF + 1], None, ALU.mult),
                  reads=[t1, self.tc], writes=[t1])
            t2 = Tok()
            for which in range(2):
                sh = PI / 2 if which == 0 else 0.0
                kb.op("dve", lambda e: e.tensor_scalar(kf[:], ang[:], 1.0 / (2 * PI), sh / (2 * PI), ALU.mult, ALU.add),
                      reads=[t1], writes=[t2])
                kb.op("dve", lambda e: e.tensor_copy(ki[:], kf[:]), reads=[t2], writes=[t2])
                kb.op("dve", lambda e: e.tensor_copy(kf[:], ki[:]), reads=[t2], writes=[t2])
                kb.op("dve", lambda e: e.scalar_tensor_tensor(u[:], kf[:], -2 * PI, ang[:], ALU.mult, ALU.add),
                      reads=[t2, t1], writes=[t2])
                if sh != 0.0:
                    kb.op("dve", lambda e: e.tensor_scalar(u[:], u[:], sh, None, ALU.add), reads=[t2], writes=[t2])
                kb.op("dve", lambda e: e.tensor_scalar(cr[:], u[:], PI, -2 * PI, ALU.is_gt, ALU.mult), reads=[t2], writes=[t2])
                kb.op("dve", lambda e: e.tensor_tensor(u[:], u[:], cr[:], ALU.add), reads=[t2], writes=[t2])
                kb.op("dve", lambda e: e.tensor_scalar(cr[:], u[:], -PI, 2 * PI, ALU.is_lt, ALU.mult), reads=[t2], writes=[t2])
                kb.op("dve", lambda e: e.tensor_tensor(u[:], u[:], cr[:], ALU.add), reads=[t2], writes=[t2])
                kb.op("dve", lambda e: e.tensor_scalar(u[:], u[:], PI, -PI, ALU.min, ALU.max), reads=[t2], writes=[t2])
                if which == 0:
                    kb.op("act", lambda e: e.activation(CS[0:32, 0, :], u[:], AF.Sin), reads=[t2], writes=[tCS])
                else:
                    kb.op("act", lambda e: e.activation(u[:], u[:], AF.Sin), reads=[t2], writes=[t2])
                    kb.op("dve", lambda e: e.tensor_scalar(CS[0:32, 1, :], u[:], self.c32[0:32, C_SIGN:C_SIGN + 1], None, ALU.mult),
                          reads=[t2, self.tc], writes=[tCS])
            kb.dma("sp", self.CSd.ap()[:, :, :], CS[:], reads=[tCS])
            kb.barrier()

    def phase1(self, L, s, xT, txT):
        kb = self.kb
        nc = self.nc
        win = self.w_in.ap()[L]
        NTT = T // 512
        with ExitStack() as es:
            wq = [self.sb(es, f"p1w{i}", [128, 8, 128], BF16) for i in range(2)]
            twq = toks(2)
            stg = [self.sb(es, f"p1s{i}", [128, T], BF16) for i in range(2)]
            tst = toks(2)
            raw = self.sb(es, "p1raw", [128, T + 3], F32)
            traw = Tok()
            acc = self.sb(es, "p1acc", [128, T], F32)
            tacc = Tok()
            h32 = [self.sb(es, f"p1h{i}", [128, 512], F32) for i in range(2)]
            th32 = toks(2)
            r1 = [self.sb(es, f"p1r{i}", [32, 512], F32) for i in range(2)]
            tr1 = toks(2)
            cw = self.sb(es, "p1cw", [128, 24, 4], F32)
            tcw = Tok()
            ps = [self.ps(es, f"p1ps{i}") for i in range(4)]
            tps = toks(4)
            ps2 = [self.ps(es, f"p1pq{i}") for i in range(2)]
            tps2 = toks(2)
            nps = [0]
            CS = self.sb(es, "p1CS", [32, 2, T], F32)
            tCS = Tok()
            kb.dma("sp", CS[:], self.CSd.ap()[:, :, :], writes=[tCS])

            kb.dma("sp", cw[:], self.conv_w.ap()[L], writes=[tcw])
            kb.op("dve", lambda e: e.memset(raw[:, 0:3], 0.0), writes=[traw])

            wst = [self.sb(es, f"p1wst{i}", [128, 8, 128], F32) for i in range(2)]
            twst = toks(2)
            wbig = self.sb(es, "p1wbig", [128, 8, 512], F32)
            twbig = Tok()
            dcols = [7680 + c * 128 for c in range(8)] + [8720 + c * 128 for c in range(16)]
            wcols = [c * 128 for c in range(24)]
            for c in range(24):
                wcols += [4608 + c * 128, dcols[c]]

            def w_dma(ci):
                b = ci % 2
                kb.dma("pool", wst[b][:], win[:, wcols[ci]:wcols[ci] + 128].rearrange("(k p) n -> p k n", p=128), writes=[twst[b]])

            def load_w(ci, col0):
                assert wcols[ci] == col0
                b = ci % 2
                if ci == 0:
                    w_dma(ci)
                if ci + 1 < len(wcols):
                    w_dma(ci + 1)
                kb.op("pool", lambda e: e.tensor_copy(wq[b][:], wst[b][:]), reads=[twst[b]], writes=[twq[b]])
                return wq[b], twq[b]

            def proj_tile(w, tw, tt, ncols=128):
                i = nps[0] % 4
                nps[0] += 1

                def f(e):
                    ins = None
                    for k in range(8):
                        ins = e.matmul(ps[i][0:ncols, :], w[:, k, 0:ncols], xT[:, k, tt * 512:(tt + 1) * 512],
                                       start=(k == 0), stop=(k == 7))
                    return ins
                kb.op("pe", f, reads=[tw, txT], writes=[tps[i]])
                return ps[i], tps[i]

            ci = 0
            tstA = [toks(NTT), toks(NTT)]
            pend = [None]

            def flush():
                if pend[0] is not None:
                    pend[0]()
                    pend[0] = None
            for c in range(24):
                w, tw = load_w(ci, c * 128)
                ci += 1
                sb_ = c % 2
                for tt in range(NTT):
                    p, tp = proj_tile(w, tw, tt)
                    cols = slice(tt * 512, (tt + 1) * 512)
                    hb = (c * NTT + tt) % 2
                    tk = tstA[sb_][tt]
                    kb.op("act", lambda e, p=p, cols=cols: e.copy(stg[sb_][:, cols], p[:, :]), reads=[tp], writes=[tk])
                    kb.op("act", lambda e, p=p, hb=hb: e.copy(h32[hb][0:32, :], p[0:32, :]), reads=[tp], writes=[th32[hb]])
                    flush()

                    def rot(hb=hb, cols=cols, tk=tk, sb_=sb_):
                        kb.op("pe", lambda e: e.matmul(ps2[hb][:, :], self.cbf[:, C_PERM:C_PERM + 128], stg[sb_][:, cols],
                                                       start=True, stop=True), reads=[tk, self.tc], writes=[tps2[hb]])
                        kb.op("dve", lambda e: e.tensor_tensor(r1[hb][:], h32[hb][0:32, :], CS[0:32, 0, cols], ALU.mult),
                              reads=[th32[hb], tCS], writes=[tr1[hb]])
                        kb.op("dve", lambda e: e.tensor_tensor(h32[hb][0:32, :], ps2[hb][0:32, :], CS[0:32, 1, cols], ALU.mult),
                              reads=[tps2[hb], tCS], writes=[th32[hb]])
                        kb.op("dve", lambda e: e.tensor_tensor(stg[sb_][0:32, cols], r1[hb][:], h32[hb][0:32, :], ALU.add),
                              reads=[tr1[hb], th32[hb]], writes=[tk])
                    pend[0] = rot
                flush()
                kb.dma("sp", self.QKT.ap()[c], stg[sb_][:], reads=tstA[sb_])
                tst[sb_].r.update({k_: v_ for t_ in tstA[sb_] for k_, v_ in t_.r.items()})
            wv = self.sb(es, "p1wv", [128, 8, 512], BF16)
            twv = Tok()
            vst = [self.sb(es, f"p1vs{i}", [128, 512], BF16) for i in range(2)]
            tvs = toks(2)
            nb_ = 0
            for g, (win_, dil) in enumerate(A_PAIRS):
                self.wload(wbig[:], twbig, wv[:], twv, win[:, 3072 + g * 512:3072 + (g + 1) * 512].rearrange("(k p) n -> p k n", p=128), q="sp")
                nblk = 32 // dil
                for r in range(dil):
                    for n in range(nblk):
                        blk = r * nblk + n
                        t0 = 128 * n * dil + r
                        i = nps[0] % 4
                        nps[0] += 1

                        def f(e, i=i, t0=t0, dil=dil):
                            ins = None
                            for k in range(8):
                                ins = e.matmul(ps[i][:, :], xT[:, k, ss(t0, 128, dil)], wv[:, k, :],
                                               start=(k == 0), stop=(k == 7))
                            return ins
                        kb.op("pe", f, reads=[twv, txT], writes=[tps[i]])
                        vb = nb_ % 2
                        nb_ += 1
                        kb.op("act", lambda e, i=i, vb=vb: e.copy(vst[vb][:], ps[i][:, :]), reads=[tps[i]], writes=[tvs[vb]])
                        kb.dma("sp", self.VA.ap()[g, :, :, blk, :].rearrange("h p e -> p h e"),
                               vst[vb][:, :].rearrange("p (h e) -> p h e", h=4), reads=[tvs[vb]])
            ci = 24
            for c in range(24):
                w, tw = load_w(ci, 4608 + c * 128)
                ci += 1
                sb_ = 0
                for tt in range(NTT):
                    p, tp = proj_tile(w, tw, tt)
                    kb.op("act", lambda e, p=p, tt=tt: e.copy(raw[:, 3 + tt * 512:3 + (tt + 1) * 512], p[:, :]),
                          reads=[tp], writes=[traw])
                wD, twD = load_w(ci, dcols[c])
                ci += 1

                def dchunk(c=c, w=wD, tw=twD):
                    sb_ = 1
                    fn = AF.Silu if c < 8 else AF.Sigmoid
                    for tt in range(NTT):
                        p, tp = proj_tile(w, tw, tt)
                        kb.op("act", lambda e, p=p, tt=tt, fn=fn: e.activation(stg[sb_][:, tt * 512:(tt + 1) * 512], p[:, :], fn),
                              reads=[tp], writes=[tst[sb_]])
                    dst = self.ZT.ap()[c] if c < 8 else self.GT.ap()[c - 8]
                    kb.dma("sp", dst, stg[sb_][:], reads=[tst[sb_]])
                dchunk()
                sb_ = 0
                for hh in range(2):
                    cs = slice(hh * 2048, (hh + 1) * 2048)
                    kb.op("dve", lambda e, cs=cs, hh=hh: e.tensor_scalar(acc[:, cs], raw[:, hh * 2048:hh * 2048 + 2048],
                                                                          cw[:, c, 0:1], None, ALU.mult),
                          reads=[traw, tcw], writes=[tacc])
                    for j in range(1, 4):
                        kb.op("dve", lambda e, cs=cs, hh=hh, j=j: e.scalar_tensor_tensor(
                            acc[:, cs], raw[:, hh * 2048 + j:hh * 2048 + j + 2048], cw[:, c, j:j + 1], acc[:, cs],
                            ALU.mult, ALU.add), reads=[traw, tcw, tacc], writes=[tacc])
                if c >= 16:
                    kb.op("act", lambda e: e.activation(stg[sb_][:], acc[:], AF.Silu), reads=[tacc], writes=[tst[sb_]])
                else:
                    kb.op("act", lambda e: e.activation(acc[:], acc[:], AF.Silu), reads=[tacc], writes=[tacc])
                    for tt in range(NTT):
                        cols = slice(tt * 512, (tt + 1) * 512)
                        hb = tt % 2
                        sq = raw
                        kb.op("dve", lambda e, cols=cols: e.tensor_tensor(raw[:, cols], acc[:, cols], acc[:, cols], ALU.mult),
                              reads=[tacc], writes=[traw])
                        i = nps[0] % 4
                        nps[0] += 1
                        kb.op("pe", lambda e, i=i, cols=cols: e.matmul(ps[i][:, :], self.c32[:, C_ONE:C_ONE + 128], raw[:, cols],
                                                                       start=True, stop=True),
                              reads=[traw, self.tc], writes=[tps[i]])
                        sc = (1.0 / 128) ** 0.5 if c < 8 else 1.0
                        kb.op("act", lambda e, i=i, cols=cols, sc=sc: e.activation(raw[:, cols], ps[i][:, :], AF.Sqrt,
                                                                                    bias=self.epsb[:, 0:1] if sc == 1.0 else self.epsb[:, 1:2],
                                                                                    scale=1.0 / (sc * sc)),
                              reads=[tps[i], self.tc], writes=[traw])
                        kb.op("dve", lambda e, cols=cols: e.reciprocal(raw[:, cols], raw[:, cols]), reads=[traw], writes=[traw])
                        kb.op("dve", lambda e, cols=cols: e.tensor_tensor(stg[sb_][:, cols], acc[:, cols], raw[:, cols], ALU.mult),
                              reads=[traw, tacc], writes=[tst[sb_]])
                    kb.op("dve", lambda e: e.memset(raw[:, 0:3], 0.0), reads=[traw], writes=[traw])
                kb.dma("sp", self.GQT.ap()[c], stg[sb_][:], reads=[tst[sb_]])
            wbd = self.sb(es, "p1wbd", [128, 8, 16], BF16)
            twbd = Tok()
            self.wload(wbig[:, :, 0:16], twbig, wbd[:], twbd, win[:, 8704:8720].rearrange("(k p) n -> p k n", p=128), q="sp")
            rows = self.sb(es, "p1rows", [128, 16], F32)
            trows = Tok()
            kb.dma("sp", rows[:, 0:8], self.bc_ap(self.dt_bias, L * 8, 8), writes=[trows])
            kb.dma("sp", rows[:, 8:16], self.bc_ap(self.a_log, L * 8, 8), writes=[trows])
            kb.op("act", lambda e: e.activation(rows[:, 8:16], rows[:, 8:16], AF.Exp), reads=[trows], writes=[trows])
            bg = self.sb(es, "p1bg", [128, 32, 16], F32)
            tbg = Tok()
            tmp = self.sb(es, "p1tmp", [128, 8], F32)
            ttmp = Tok()
            for t in range(32):
                i = nps[0] % 4
                nps[0] += 1

                def f(e, i=i, t=t):
                    ins = None
                    for k in range(8):
                        ins = e.matmul(ps[i][:, 0:16], xT[:, k, t * 128:(t + 1) * 128], wbd[:, k, :], start=(k == 0), stop=(k == 7))
                    return ins
                kb.op("pe", f, reads=[twbd, txT], writes=[tps[i]])
                kb.op("act", lambda e, i=i, t=t: e.activation(bg[:, t, 0:8], ps[i][:, 0:8], AF.Sigmoid), reads=[tps[i]], writes=[tbg])
                kb.op("dve", lambda e, i=i: e.tensor_tensor(tmp[:], ps[i][:, 8:16], rows[:, 0:8], ALU.add),
                      reads=[tps[i], trows], writes=[ttmp])
                kb.op("act", lambda e: e.activation(tmp[:], tmp[:], AF.Exp), reads=[ttmp], writes=[ttmp])
                kb.op("act", lambda e: e.activation(tmp[:], tmp[:], AF.Ln, bias=self.epsb[:, 2:3]), reads=[ttmp, self.tc], writes=[ttmp])
                kb.op("dve", lambda e, t=t: e.scalar_tensor_tensor(bg[:, t, 8:16], tmp[:], -1.0, rows[:, 8:16], ALU.mult, ALU.mult),
                      reads=[ttmp, trows], writes=[tbg])
            kb.dma("sp", self.BG.ap().rearrange("(t p) c -> p t c", p=128), bg[:], reads=[tbg])
            kb.barrier()


    def phase2(self, s, xT):
        kb = self.kb
        scale = 128.0 ** -0.5
        with ExitStack() as es:
            QT = [xT[:, g, :] for g in range(3)]
            KT = [xT[:, 3 + g, :] for g in range(3)]
            VV = [self.sb(es, f"p2v{g}", [128, 32, 128], BF16) for g in range(3)]
            tq, tk, tv = toks(3), toks(3), toks(3)
            num = self.sb(es, "p2num", [128, T], F32)
            den = self.sb(es, "p2den", [128, T], F32)
            tnum, tden = Tok(), Tok()
            PT = [self.sb(es, f"p2pt{i}", [128, 256], BF16) for i in range(2)]
            tpt = toks(2)
            yst = self.sb(es, "p2y", [128, T], BF16)
            tyst = Tok()
            psS = [self.ps(es, f"p2pS{i}") for i in range(2)]
            psN = [self.ps(es, f"p2pN{i}") for i in range(2)]
            psD = [self.ps(es, f"p2pD{i}") for i in range(2)]
            tS, tN, tD = toks(2), toks(2), toks(2)
            ones_bf = self.cbf[:, C_ONE:C_ONE + 128]
            maskcat = self.cbf[:, C_U:C_U + 256]
            kbi = 0
            for slot in range(4):
                for g in range(3):
                    kb.dma("sp", QT[g], self.QKT.ap()[g * 4 + slot], writes=[tq[g]])
                    kb.dma("sp", KT[g], self.QKT.ap()[12 + g * 4 + slot], writes=[tk[g]])
                    kb.dma("sp", VV[g][:], self.VA.ap()[g, slot], writes=[tv[g]])
                kb.op("dve", lambda e: e.memset(num[:], 0.0), writes=[tnum])
                kb.op("dve", lambda e: e.memset(den[:], 0.0), writes=[tden])
                for g, (win_, dil) in enumerate(A_PAIRS):
                    nblk = 32 // dil
                    for r in range(dil):
                        for n in range(nblk):
                            blk = r * nblk + n
                            nq = 2 if n + 1 < nblk else 1
                            t0 = 128 * n * dil + r
                            b = kbi % 2
                            kbi += 1
                            kb.op("pe", lambda e, b=b, g=g, t0=t0, nq=nq, dil=dil: e.matmul(
                                psS[b][:, 0:128 * nq], KT[g][:, ss(t0, 128, dil)], QT[g][:, ss(t0, 128 * nq, dil)],
                                start=True, stop=True), reads=[tk[g], tq[g]], writes=[tS[b]])
                            kb.op("act", lambda e, b=b, nq=nq: e.activation(PT[b][:, 0:128 * nq], psS[b][:, 0:128 * nq], AF.Exp,
                                                                            scale=scale), reads=[tS[b]], writes=[tpt[b]])
                            kb.op("dve", lambda e, b=b, nq=nq: e.tensor_tensor(PT[b][:, 0:128 * nq], PT[b][:, 0:128 * nq],
                                                                              maskcat[:, 0:128 * nq], ALU.mult),
                                  reads=[tpt[b], self.tc], writes=[tpt[b]])
                            for mo in range(nq):
                                a = (n + mo) % 2

                                def f(e, a=a, mo=mo, b=b, g=g, blk=blk, n=n):
                                    st = (mo == 1 or n == 0)
                                    sp_ = (mo == 0)
                                    e.matmul(psN[a][:, 0:128], VV[g][:, blk, :], PT[b][:, mo * 128:(mo + 1) * 128], start=st, stop=sp_)
                                    return e.matmul(psD[a][:, 0:128], ones_bf, PT[b][:, mo * 128:(mo + 1) * 128], start=st, stop=sp_)
                                kb.op("pe", f, reads=[tv[g], tpt[b], self.tc], writes=[tN[a], tD[a]])
                            a = n % 2
                            kb.op("dve", lambda e, a=a, t0=t0, dil=dil: e.tensor_tensor(
                                num[:, ss(t0, 128, dil)], num[:, ss(t0, 128, dil)], psN[a][:, 0:128], ALU.add),
                                reads=[tN[a], tnum], writes=[tnum])
                            kb.op("dve", lambda e, a=a, t0=t0, dil=dil: e.tensor_tensor(
                                den[:, ss(t0, 128, dil)], den[:, ss(t0, 128, dil)], psD[a][:, 0:128], ALU.add),
                                reads=[tD[a], tden], writes=[tden])
                kb.op("dve", lambda e: e.reciprocal(den[:], den[:]), reads=[tden], writes=[tden])
                kb.op("dve", lambda e: e.tensor_tensor(yst[:], num[:], den[:], ALU.mult), reads=[tnum, tden], writes=[tyst])
                kb.dma("sp", self.YAT.ap()[slot], yst[:], reads=[tyst])
            kb.barrier()


    def phase3(self, L, s, xT):
        kb = self.kb
        c32, cbf = self.c32, self.cbf
        ident = c32[:, C_ID:C_ID + 128]
        ones = c32[:, C_ONE:C_ONE + 128]
        NCH = int(os.environ.get("P3NCH", "32"))
        NH = int(os.environ.get("P3NH", "8"))
        P3STOP = int(os.environ.get("P3STOP", "99"))
        with ExitStack() as es:
            KT, QT, VT, ZTs, ybst = (xT[:, i, :] for i in range(5))
            tld = toks(4)
            tyb = Tok()
            bg = self.sb(es, "p3bg", [128, 32, 16], F32)
            gc = self.sb(es, "p3gc", [128, 32, 8], F32)
            gl = self.sb(es, "p3gl", [128, 32, 8], F32)
            egc = self.sb(es, "p3egc", [128, 32, 8], F32)
            bge = self.sb(es, "p3bge", [128, 32, 8], F32)
            etl = self.sb(es, "p3etl", [128, 32, 8], F32)
            nbt = self.sb(es, "p3nbt", [128, 32, 8], F32)
            sda = self.sb(es, "p3sda", [128, 32, 8], F32)
            ngc = self.sb(es, "p3ngc", [128, 32, 8], F32)
            nwc = self.sb(es, "p3nw", [128, 1], F32)
            tsm = Tok()
            pb = [self.ps(es, f"p3ps{i}") for i in range(7)]
            psT = self.ps(es, "p3psT", (128, 512), BF16)
            tp = {k: Tok() for k in ("T", "g", "kk", "a", "b", "u", "v", "o")}
            tp["w"], tp["s"], tp["q"] = tp["u"], tp["v"], tp["o"]
            ps_g, ps_kk, ps_a, ps_b = pb[0], pb[1], pb[2], pb[3]
            ps_u, ps_w = pb[4][:, 0:128], pb[4][:, 128:256]
            ps_v, ps_s = pb[5][:, 0:128], pb[5][:, 128:256]
            ps_o, ps_q = pb[6][:, 0:128], pb[6][:, 128:256]
            kb.dma("sp", bg[:], self.BG.ap().rearrange("(t p) c -> p t c", p=128), writes=[tsm])
            kb.dma("sp", nwc[:], bass.AP(self.dn_norm_w, L * 128, [[1, 128], [1, 1]]), writes=[tsm])
            gsl = bg[:, :, 8:16]
            bsl = bg[:, :, 0:8]
            v3 = lambda ap: ap.rearrange("p (c h) -> p c h", h=8)
            kb.op("pe", lambda e: e.matmul(v3(pb[0][:, 0:256]), c32[:, C_U:C_U + 128], gsl, start=True, stop=True), reads=[tsm, self.tc], writes=[tp["g"]])
            kb.op("pe", lambda e: e.matmul(v3(pb[1][:, 0:256]), ones, gsl, start=True, stop=True), reads=[tsm, self.tc], writes=[tp["kk"]])
            kb.op("act", lambda e: e.copy(gc[:], v3(pb[0][:, 0:256])), reads=[tp["g"]], writes=[tsm])
            kb.op("act", lambda e: e.copy(gl[:], v3(pb[1][:, 0:256])), reads=[tp["kk"]], writes=[tsm])
            kb.op("act", lambda e: e.activation(egc[:], gc[:], AF.Exp), reads=[tsm], writes=[tsm])
            kb.op("act", lambda e: e.activation(sda[:], gl[:], AF.Exp), reads=[tsm], writes=[tsm])
            kb.op("dve", lambda e: e.tensor_tensor(bge[:], egc[:], bsl, ALU.mult), reads=[tsm], writes=[tsm])
            kb.op("dve", lambda e: e.tensor_tensor(etl[:], gl[:], gc[:], ALU.subtract), reads=[tsm], writes=[tsm])
            kb.op("act", lambda e: e.activation(etl[:], etl[:], AF.Exp), reads=[tsm], writes=[tsm])
            kb.op("dve", lambda e: e.tensor_scalar(nbt[:], bsl, -1.0, None, ALU.mult), reads=[tsm], writes=[tsm])
            kb.op("dve", lambda e: e.tensor_scalar(ngc[:], gc[:], -1.0, None, ALU.mult), reads=[tsm], writes=[tsm])
            kb.op("dve", lambda e: e.tensor_scalar(nwc[:], nwc[:], 128.0 ** 0.5, None, ALU.mult), reads=[tsm], writes=[tsm])

            def S(name, shape, dtype, n=1):
                return [self.sb(es, f"p3{name}{i}", shape, dtype) for i in range(n)]
            Sst, Sbf = S("S", [128, 128], F32)[0], S("Sb", [128, 128], BF16)[0]
            tS = Tok()
            Ug, dmS, dmT, eS, eT, eg = (S(n_, [128, 128], F32)[0] for n_ in ("Ug", "dmS", "dmT", "eS", "eT", "eg"))
            tUg, tdmS, tdmT, teS, teT, teg = toks(6)
            kbg, ktl, vb_ = (S(n_, [128, 128], BF16)[0] for n_ in ("kbg", "ktl", "vb"))
            tkbg, tktl, tvb = toks(3)
            PY = S("PY", [128, 256], F32, 2)
            Qm = S("Qm", [128, 128], F32, 2)
            tPY, tQ = toks(2), toks(2)
            TT, AT, wT, qdT, vnew = (S(n_, [128, 128], BF16)[0] for n_ in ("TT", "AT", "wT", "qdT", "vn"))
            tTT, tAT, twT, tqdT, tvn = toks(5)
            u_, sq, rinv, y1 = (S(n_, [128, 128], F32)[0] for n_ in ("u", "sq", "ri", "y1"))
            tu, tsq, tri, ty1 = toks(4)

            for h in range(NH):
                kb.dma("sp", KT, self.GQT.ap()[8 + h], writes=[tld[0]])
                kb.dma("sp", QT, self.GQT.ap()[h], writes=[tld[1]])
                kb.dma("sp", VT, self.GQT.ap()[16 + h], writes=[tld[2]])
                kb.dma("sp", ZTs, self.ZT.ap()[h], writes=[tld[3]])
                kb.op("dve", lambda e: e.memset(Sst[:], 0.0), writes=[tS])
                kb.op("dve", lambda e: e.memset(Sbf[:], 0.0), writes=[tS])
                for c in range(NCH):
                    cols = slice(c * 128, (c + 1) * 128)
                    col = lambda t: t[:, c, h:h + 1]
                    def ftr(e):
                        e.transpose(psT[:, 0:128], KT[:, cols], cbf[:, C_ID:C_ID + 128])
                        return e.transpose(psT[:, 128:256], VT[:, cols], cbf[:, C_ID:C_ID + 128])
                    kb.op("pe", ftr, reads=[tld[0], tld[2], self.tc], writes=[tp["T"]])
                    kb.op("act", lambda e: e.activation(kbg[:], psT[:, 0:128], AF.Copy, scale=col(bge)), reads=[tp["T"], tsm], writes=[tkbg])
                    kb.op("act", lambda e: e.activation(ktl[:], psT[:, 0:128], AF.Copy, scale=col(etl)), reads=[tp["T"], tsm], writes=[tktl])
                    kb.op("act", lambda e: e.activation(vb_[:], psT[:, 128:256], AF.Copy, scale=col(bsl)), reads=[tp["T"], tsm], writes=[tvb])
                    if P3STOP <= 1:
                        continue
                    kb.op("dve", lambda e: e.tensor_scalar(Ug[:], c32[:, C_U:C_U + 128], col(gsl), None, ALU.mult), reads=[tsm, self.tc], writes=[tUg])
                    kb.op("pe", lambda e: e.matmul(ps_g[:, 0:128], ones, Ug[:], start=True, stop=True), reads=[tUg, self.tc], writes=[tp["g"]])
                    kb.op("act", lambda e: e.activation(Ug[:], ps_g[:, 0:128], AF.Identity, bias=col(ngc), scale=1.0), reads=[tp["g"], tsm], writes=[tUg])
                    kb.op("dve", lambda e: e.tensor_tensor(dmS[:], Ug[:], c32[:, C_MBS:C_MBS + 128], ALU.max), reads=[tUg, self.tc], writes=[tdmS])
                    kb.op("dve", lambda e: e.tensor_tensor(dmT[:], Ug[:], c32[:, C_MBT:C_MBT + 128], ALU.min), reads=[tUg, self.tc], writes=[tdmT])
                    kb.op("act", lambda e: e.activation(eg[:], ps_g[:, 0:128], AF.Exp), reads=[tp["g"]], writes=[teg])
                    kb.op("act", lambda e: e.activation(eS[:], dmS[:], AF.Exp, scale=-1.0), reads=[tdmS], writes=[teS])
                    kb.op("act", lambda e: e.activation(eT[:], dmT[:], AF.Exp), reads=[tdmT], writes=[teT])
                    if P3STOP <= 2:
                        continue
                    kb.op("pe", lambda e: e.matmul(ps_kk[:, 0:128], KT[:, cols], KT[:, cols], start=True, stop=True), reads=[tld[0]], writes=[tp["kk"]])
                    kb.op("dve", lambda e: e.tensor_tensor(eS[:], ps_kk[:, 0:128], eS[:], ALU.mult), reads=[tp["kk"], teS], writes=[teS])
                    kb.op("dve", lambda e: e.tensor_scalar(Qm[0][:], eS[:], col(nbt), None, ALU.mult), reads=[tsm, teS], writes=[tQ[0]])
                    kb.op("pe", lambda e: e.transpose(ps_b[:, 0:128], Qm[0][:], ident), reads=[tQ[0], self.tc], writes=[tp["b"]])
                    kb.op("act", lambda e: e.copy(PY[0][:, 0:128], ps_b[:, 0:128]), reads=[tp["b"]], writes=[tPY[0]])
                    kb.op("dve", lambda e: e.tensor_tensor(PY[0][:, 128:256], ps_b[:, 0:128], ident, ALU.add), reads=[tp["b"], self.tc], writes=[tPY[0]])
                    if P3STOP <= 3:
                        continue
                    for k in range(7):
                        a, b = k % 2, (k + 1) % 2
                        if k == 0:
                            kb.op("pe", lambda e: e.matmul(ps_a[:, 0:128], Qm[a][:], PY[a][:, 0:128], start=True, stop=True),
                                  reads=[tQ[a], tPY[a]], writes=[tp["a"]])
                        elif k < 6:
                            kb.op("pe", lambda e: e.matmul(ps_a[:, 0:256], Qm[a][:], PY[a][:, 0:256], start=True, stop=True),
                                  reads=[tQ[a], tPY[a]], writes=[tp["a"]])
                        else:
                            kb.op("pe", lambda e: e.matmul(ps_a[:, 128:256], Qm[a][:], PY[a][:, 128:256], start=True, stop=True),
                                  reads=[tQ[a], tPY[a]], writes=[tp["a"]])
                        if k < 6:
                            kb.op("pe", lambda e: e.matmul(ps_b[:, 0:128], PY[a][:, 0:128], Qm[a][:], start=True, stop=True),
                                  reads=[tQ[a], tPY[a]], writes=[tp["b"]])
                            kb.op("act", lambda e: e.copy(PY[b][:, 0:128], ps_a[:, 0:128]), reads=[tp["a"]], writes=[tPY[b]])
                            kb.op("act", lambda e: e.copy(Qm[b][:], ps_b[:, 0:128]), reads=[tp["b"]], writes=[tQ[b]])
                            if k == 0:
                                kb.op("dve", lambda e: e.tensor_copy(PY[b][:, 128:256], PY[a][:, 128:256]), reads=[tPY[a]], writes=[tPY[b]])
                            else:
                                kb.op("dve", lambda e: e.tensor_tensor(PY[b][:, 128:256], PY[a][:, 128:256], ps_a[:, 128:256], ALU.add),
                                      reads=[tPY[a], tp["a"]], writes=[tPY[b]])
                        else:
                            kb.op("dve", lambda e: e.tensor_tensor(TT[:], PY[a][:, 128:256], ps_a[:, 128:256], ALU.add),
                                  reads=[tPY[a], tp["a"]], writes=[tTT])
                    if P3STOP <= 4:
                        continue
                    kb.op("pe", lambda e: e.matmul(ps_kk[:, 0:128], KT[:, cols], QT[:, cols], start=True, stop=True), reads=[tld[0], tld[1]], writes=[tp["kk"]])
                    kb.op("dve", lambda e: e.tensor_tensor(AT[:], ps_kk[:, 0:128], eT[:], ALU.mult), reads=[tp["kk"], teT], writes=[tAT])
                    kb.op("pe", lambda e: e.matmul(ps_u, TT[:], vb_[:], start=True, stop=True), reads=[tTT, tvb], writes=[tp["u"]])
                    kb.op("pe", lambda e: e.matmul(ps_w, kbg[:], TT[:], start=True, stop=True), reads=[tTT, tkbg], writes=[tp["w"]])
                    kb.op("act", lambda e: e.copy(u_[:], ps_u), reads=[tp["u"]], writes=[tu])
                    kb.op("act", lambda e: e.copy(wT[:], ps_w), reads=[tp["w"]], writes=[twT])
                    kb.op("dve", lambda e: e.tensor_tensor(qdT[:], QT[:, cols], eg[:], ALU.mult), reads=[tld[1], teg], writes=[tqdT])
                    if P3STOP <= 5:
                        continue
                    kb.op("pe", lambda e: e.matmul(ps_v, wT[:], Sbf[:], start=True, stop=True), reads=[twT, tS], writes=[tp["v"]])
                    kb.op("dve", lambda e: e.tensor_tensor(vnew[:], u_[:], ps_v, ALU.subtract), reads=[tu, tp["v"]], writes=[tvn])

                    def fo(e):
                        e.matmul(ps_o, Sbf[:], qdT[:], start=True, stop=False)
                        return e.matmul(ps_o, vnew[:], AT[:], start=False, stop=True)
                    kb.op("pe", fo, reads=[tS, tqdT, tvn, tAT], writes=[tp["o"]])
                    kb.op("pe", lambda e: e.matmul(ps_s, ktl[:], vnew[:], start=True, stop=True), reads=[tktl, tvn], writes=[tp["s"]])
                    kb.op("dve", lambda e: e.tensor_scalar(Sst[:], Sst[:], col(sda), None, ALU.mult), reads=[tS, tsm], writes=[tS])
                    kb.op("dve", lambda e: e.tensor_tensor(Sst[:], Sst[:], ps_s, ALU.add), reads=[tS, tp["s"]], writes=[tS])
                    kb.op("act", lambda e: e.copy(Sbf[:], Sst[:]), reads=[tS], writes=[tS])
                    if P3STOP <= 6:
                        continue
                    kb.op("act", lambda e: e.activation(sq[:], ps_o, AF.Square), reads=[tp["o"]], writes=[tsq])
                    kb.op("pe", lambda e: e.matmul(ps_q, ones, sq[:], start=True, stop=True), reads=[tsq, self.tc], writes=[tp["q"]])
                    kb.op("act", lambda e: e.activation(rinv[:], ps_q, AF.Sqrt, bias=self.epsb[:, 1:2], scale=1.0), reads=[tp["q"], self.tc], writes=[tri])
                    kb.op("dve", lambda e: e.reciprocal(rinv[:], rinv[:]), reads=[tri], writes=[tri])
                    kb.op("dve", lambda e: e.tensor_tensor(y1[:], ps_o, rinv[:], ALU.mult), reads=[tp["o"], tri], writes=[ty1])
                    kb.op("dve", lambda e: e.scalar_tensor_tensor(ybst[:, cols], y1[:], nwc[:, 0:1], ZTs[:, cols], ALU.mult, ALU.mult),
                          reads=[ty1, tsm, tld[3]], writes=[tyb])
                kb.dma("sp", self.YBT.ap()[h], ybst, reads=[tyb])
            kb.barrier()


    def layernorm(self, pre, tpre, g, b, tgb, out, tout, st, mv, tst):
        kb = self.kb

        def f(e):
            e.bn_stats(st[:, 0:6], pre[:, 0:512])
            return e.bn_stats(st[:, 6:12], pre[:, 512:1024])
        kb.op("dve", f, reads=[tpre], writes=[tst])
        kb.op("dve", lambda e: e.bn_aggr(mv[:, 0:2], st[:, 0:12]), reads=[tst], writes=[tst])
        kb.op("act", lambda e: e.activation(mv[:, 2:3], mv[:, 1:2], AF.Sqrt, bias=self.epsb[:, 3:4]), reads=[tst, self.tc], writes=[tst])
        kb.op("dve", lambda e: e.reciprocal(mv[:, 2:3], mv[:, 2:3]), reads=[tst], writes=[tst])
        kb.op("dve", lambda e: e.tensor_scalar(out, pre, mv[:, 0:1], mv[:, 2:3], ALU.subtract, ALU.mult), reads=[tpre, tst], writes=[tout])
        kb.op("dve", lambda e: e.tensor_tensor(out, out, g, ALU.mult), reads=[tgb], writes=[tout])
        kb.op("dve", lambda e: e.tensor_tensor(out, out, b, ALU.add), reads=[tgb], writes=[tout])

    def phase4(self, L, s, xT, txT, gates, tgates):
        kb = self.kb
        c32 = self.c32
        xres_src = self.x_in.ap()[s * T:(s + 1) * T, :] if L == 0 else self.X2.ap()
        moe = (L == 1)
        with ExitStack() as es:
            Wa = self.sb(es, "p4wa", [128, 4, D], BF16)
            Wb = self.sb(es, "p4wb", [128, 8, D], BF16)
            Wo = self.sb(es, "p4wo", [128, 8, D], BF16)
            tW = Tok()
            stage = self.sb(es, "p4stg", [128, 8, 512], F32)
            tstage = Tok()
            for hf in range(2):
                hs = slice(hf * 512, (hf + 1) * 512)
                self.wload(stage[:, 0:4, :], tstage, Wa[:, :, hs], tW, self.w_a.ap()[L][:, hs].rearrange("(k p) n -> p k n", p=128), q="sp")
                self.wload(stage[:], tstage, Wb[:, :, hs], tW, self.w_b.ap()[L][:, hs].rearrange("(k p) n -> p k n", p=128), q="sp")
                self.wload(stage[:], tstage, Wo[:, :, hs], tW, self.w_o.ap()[L][:, hs].rearrange("(k p) n -> p k n", p=128), q="sp")
            lng = self.sb(es, "p4lng", [128, D], F32)
            lnb = self.sb(es, "p4lnb", [128, D], F32)
            tgb = Tok()
            kb.dma("sp", lng[:], self.bc_ap(self.ln1_g, L * D, D), writes=[tgb])
            kb.dma("sp", lnb[:], self.bc_ap(self.ln1_b, L * D, D), writes=[tgb])
            if moe:
                Wr = self.sb(es, "p4wr", [128, 8, NE], F32)
                kb.dma("sp", Wr[:], self.router.ap()[0].rearrange("(k p) n -> p k n", p=128), writes=[tgb])
                x32 = self.sb(es, "p4x32", [128, 8, 128], F32)
                tx32 = Tok()
                rt = self.sb(es, "p4rt", [128, 64], F32)
                trt = Tok()
            ya = self.sb(es, "p4ya", [128, 4, 512], BF16)
            yb = self.sb(es, "p4yb", [128, 8, 512], BF16)
            gt = self.sb(es, "p4gt", [128, 16, 512], BF16)
            xr = self.sb(es, "p4xr", [128, 4, D], F32)
            tya, tyb, tgt, txr = toks(4)
            mT = self.sb(es, "p4mT", [128, 8, 512], BF16)
            tmT = Tok()
            m1 = [self.sb(es, f"p4m1{i}", [128, 512], F32) for i in range(1)] * 2
            m2 = [self.sb(es, f"p4m2{i}", [128, 512], F32) for i in range(1)] * 2
            tm1, tm2 = toks(1) * 2, toks(1) * 2
            pre = [self.sb(es, f"p4pre{i}", [128, D], F32) for i in range(1)] * 2
            x1t = [self.sb(es, f"p4x1{i}", [128, D], F32) for i in range(2)]
            tpre, tx1 = toks(1) * 2, toks(2)
            st = self.sb(es, "p4st", [128, 12], F32)
            mv = self.sb(es, "p4mv", [128, 4], F32)
            tst = Tok()
            psA = [self.ps(es, f"p4pA{i}") for i in range(2)]
            psB = [self.ps(es, f"p4pB{i}") for i in range(2)]
            psO = [self.ps(es, f"p4pO{i}") for i in range(2)]
            psT = [self.ps(es, f"p4pT{i}") for i in range(2)]
            tpA, tpB, tpO, tpT = toks(2), toks(2), toks(2), toks(2)
            no = 0
            for tt in range(T // 512):
                cs = slice(tt * 512, (tt + 1) * 512)
                kb.dma("sp", ya[:], self.YAT.ap()[:, :, cs].rearrange("k p t -> p k t"), writes=[tya])
                kb.dma("sp", yb[:], self.YBT.ap()[:, :, cs].rearrange("k p t -> p k t"), writes=[tyb])
                kb.dma("sp", gt[:], self.GT.ap()[:, :, cs].rearrange("k p t -> p k t"), writes=[tgt])
                kb.dma("sp", xr[:], xres_src[tt * 512:(tt + 1) * 512, :].rearrange("(j p) d -> p j d", p=128), writes=[txr])
                kb.op("act", lambda e: e.mul(xr[:], xr[:], ALPHA), reads=[txr], writes=[txr])
                for dc in range(8):
                    i = dc % 2
                    ds_ = slice(dc * 128, (dc + 1) * 128)

                    def fa(e, i=i, ds_=ds_):
                        ins = None
                        for k in range(4):
                            ins = e.matmul(psA[i][:, :], Wa[:, k, ds_], ya[:, k, :], start=(k == 0), stop=(k == 3))
                        return ins

                    def fb(e, i=i, ds_=ds_):
                        ins = None
                        for k in range(8):
                            ins = e.matmul(psB[i][:, :], Wb[:, k, ds_], yb[:, k, :], start=(k == 0), stop=(k == 7))
                        return ins
                    kb.op("pe", fa, reads=[tW, tya], writes=[tpA[i]])
                    kb.op("pe", fb, reads=[tW, tyb], writes=[tpB[i]])
                    kb.op("dve", lambda e, i=i, dc=dc: e.tensor_tensor(m1[i][:], psA[i][:, :], gt[:, dc, :], ALU.mult), reads=[tpA[i], tgt], writes=[tm1[i]])
                    kb.op("dve", lambda e, i=i, dc=dc: e.tensor_tensor(m2[i][:], psB[i][:, :], gt[:, 8 + dc, :], ALU.mult), reads=[tpB[i], tgt], writes=[tm2[i]])
                    kb.op("pool", lambda e, i=i, dc=dc: e.tensor_tensor(mT[:, dc, :], m1[i][:], m2[i][:], ALU.add), reads=[tm1[i], tm2[i]], writes=[tmT])
                for sub in range(4):
                    b = no % 2
                    no += 1
                    tok0 = tt * 512 + sub * 128
                    for hf in range(2):
                        hs = slice(hf * 512, (hf + 1) * 512)

                        def fo(e, hf=hf, hs=hs, sub=sub):
                            ins = None
                            for k in range(8):
                                ins = e.matmul(psO[hf][:, :], mT[:, k, sub * 128:(sub + 1) * 128], Wo[:, k, hs], start=(k == 0), stop=(k == 7))
                            return ins
                        kb.op("pe", fo, reads=[tW, tmT], writes=[tpO[hf]])
                        kb.op("dve", lambda e, hf=hf, hs=hs, b=b, sub=sub: e.tensor_tensor(pre[b][:, hs], psO[hf][:, :], xr[:, sub, hs], ALU.add),
                              reads=[tpO[hf], txr], writes=[tpre[b]])
                    self.layernorm(pre[b][:], tpre[b], lng[:], lnb[:], tgb, x1t[b][:], tx1[b], st, mv, tst)
                    kb.dma("sp", self.X1.ap()[tok0:tok0 + 128, :], x1t[b][:], reads=[tx1[b]])
                    if moe:
                        self.transpose_rows(None, x1t[b], tx1[b], xT, txT, tok0, psT, tpT, 0, x32=x32, tx32=tx32)
                        self.router_gates(x32, tx32, Wr, tgb, rt, trt, psA[0], tpA[0], gates[:, tok0 // 128, :], tgates)
                    else:
                        self.transpose_rows(None, x1t[b], tx1[b], xT, txT, tok0, psT, tpT, 0)
            kb.barrier()

    def router_gates(self, x32, tx32, Wr, tWr, rt, trt, ps, tps, gout, tgout):
        kb = self.kb

        def f(e):
            ins = None
            for k in range(8):
                ins = e.matmul(ps[:, 0:NE], x32[:, k, :], Wr[:, k, :], start=(k == 0), stop=(k == 7))
            return ins
        kb.op("pe", f, reads=[tx32, tWr], writes=[tps])
        lg, eq1, lg2, eq2, g1 = (rt[:, i * 8:(i + 1) * 8] for i in range(5))
        m1, m2, d_, w1, w2 = (rt[:, 40 + i:41 + i] for i in range(5))
        kb.op("act", lambda e: e.copy(lg, ps[:, 0:NE]), reads=[tps], writes=[trt])
        kb.op("dve", lambda e: e.reduce_max(m1, lg, AX.X), reads=[trt], writes=[trt])
        kb.op("dve", lambda e: e.tensor_scalar(eq1, lg, m1, None, ALU.is_equal), reads=[trt], writes=[trt])
        kb.op("dve", lambda e: e.scalar_tensor_tensor(lg2, eq1, -1e30, lg, ALU.mult, ALU.add), reads=[trt], writes=[trt])
        kb.op("dve", lambda e: e.reduce_max(m2, lg2, AX.X), reads=[trt], writes=[trt])
        kb.op("dve", lambda e: e.tensor_scalar(eq2, lg2, m2, None, ALU.is_equal), reads=[trt], writes=[trt])
        kb.op("dve", lambda e: e.tensor_tensor(d_, m2, m1, ALU.subtract), reads=[trt], writes=[trt])
        kb.op("act", lambda e: e.activation(d_, d_, AF.Exp), reads=[trt], writes=[trt])
        kb.op("dve", lambda e: e.tensor_scalar(w1, d_, 1.0, None, ALU.add), reads=[trt], writes=[trt])
        kb.op("dve", lambda e: e.reciprocal(w1, w1), reads=[trt], writes=[trt])
        kb.op("dve", lambda e: e.tensor_tensor(w2, d_, w1, ALU.mult), reads=[trt], writes=[trt])
        kb.op("dve", lambda e: e.tensor_scalar(g1, eq1, w1, None, ALU.mult), reads=[trt], writes=[trt])
        kb.op("dve", lambda e: e.scalar_tensor_tensor(gout, eq2, w2, g1, ALU.mult, ALU.add), reads=[trt], writes=[tgout])

    def phase5(self, L, s, xT, txT, gates, tgates):
        kb = self.kb
        c32 = self.c32
        moe = (L == 1)
        ne = NE if moe else 1
        dff = DEX if moe else DFF
        GW = 256
        ngr = dff // GW
        TS = 2048
        last = (L == 1)

        def wsrc(which, e_, g_):
            c0 = g_ * GW
            if moe:
                base = {"g": self.moe_g, "u": self.moe_u, "d": self.moe_d}[which].ap()[0][e_]
            else:
                base = {"g": self.ffn_g, "u": self.ffn_u, "d": self.ffn_d}[which].ap()[0]
            if which == "d":
                return base[c0:c0 + GW, :].rearrange("(k p) n -> p k n", p=128)
            return base[:, c0:c0 + GW].rearrange("(k p) n -> p k n", p=128)
        with ExitStack() as es:
            yacc = self.sb(es, "p5acc", [128, TS // 128, D], F32)
            taccs = toks(2 * TS // 128)
            stg_g = self.sb(es, "p5sg", [128, 8, GW], F32)
            stg_u = self.sb(es, "p5su", [128, 8, GW], F32)
            stg_d = self.sb(es, "p5sd", [128, GW // 128, D], F32)
            tsg, tsu, tsd = toks(3)
            Wg = [self.sb(es, f"p5wg{i}", [128, 8, GW], BF16) for i in range(2)]
            Wu = [self.sb(es, f"p5wu{i}", [128, 8, GW], BF16) for i in range(2)]
            Wd = [self.sb(es, f"p5wd{i}", [128, GW // 128, D], BF16) for i in range(2)]
            tWg, tWu, tWd = toks(2), toks(2), toks(2)
            hT = [self.sb(es, f"p5h{i}", [128, GW // 128, 512], BF16) for i in range(2)]
            thT = toks(2)
            sg = [self.sb(es, f"p5s{i}", [128, 512], BF16) for i in range(2)]
            tsg_ = toks(2)
            tmp = [self.sb(es, f"p5t{i}", [128, 512], F32) for i in range(2)]
            ttmp = toks(2)
            lng = self.sb(es, "p5lng", [128, D], F32)
            lnb = self.sb(es, "p5lnb", [128, D], F32)
            tgb = Tok()
            kb.dma("sp", lng[:], self.bc_ap(self.ln2_g, L * D, D), writes=[tgb])
            kb.dma("sp", lnb[:], self.bc_ap(self.ln2_b, L * D, D), writes=[tgb])
            x1 = [self.sb(es, f"p5x1{i}", [128, D], F32) for i in range(1)] * 2
            tx1 = toks(1) * 2
            st = self.sb(es, "p5st", [128, 12], F32)
            mv = self.sb(es, "p5mv", [128, 4], F32)
            tst = Tok()
            psG = [self.ps(es, f"p5pG{i}") for i in range(2)]
            psU = [self.ps(es, f"p5pU{i}") for i in range(2)]
            psY = [self.ps(es, f"p5pY{i}") for i in range(4)]
            tpG, tpU, tpY = toks(2), toks(2), toks(4)
            work = [(e_, g_) for e_ in range(ne) for g_ in range(ngr)]

            def w_dma(wi):
                e_, g_ = work[wi]
                kb.dma("sp", stg_g[:], wsrc("g", e_, g_), writes=[tsg])
                kb.dma("sp", stg_u[:], wsrc("u", e_, g_), writes=[tsu])
                kb.dma("sp", stg_d[:], wsrc("d", e_, g_), writes=[tsd])

            def w_cast(wi):
                b = wi % 2
                kb.op("act", lambda e: e.copy(Wg[b][:], stg_g[:]), reads=[tsg], writes=[tWg[b]])
                kb.op("act", lambda e: e.copy(Wu[b][:], stg_u[:]), reads=[tsu], writes=[tWu[b]])
                kb.op("act", lambda e: e.copy(Wd[b][:], stg_d[:]), reads=[tsd], writes=[tWd[b]])

            def GU(st_i, wi, t4, hb):
                b = wi % 2
                c0 = st_i * TS + t4 * 512
                for fc in range(GW // 128):
                    i = self.ngc % 2
                    self.ngc += 1
                    fs = slice(fc * 128, (fc + 1) * 128)

                    def fg(e, i=i, fs=fs):
                        ins = None
                        for k in range(8):
                            ins = e.matmul(psG[i][:, :], Wg[b][:, k, fs], xT[:, k, c0:c0 + 512], start=(k == 0), stop=(k == 7))
                        return ins

                    def fu(e, i=i, fs=fs):
                        ins = None
                        for k in range(8):
                            ins = e.matmul(psU[i][:, :], Wu[b][:, k, fs], xT[:, k, c0:c0 + 512], start=(k == 0), stop=(k == 7))
                        return ins
                    kb.op("pe", fg, reads=[tWg[b], txT], writes=[tpG[i]])
                    kb.op("pe", fu, reads=[tWu[b], txT], writes=[tpU[i]])
                    kb.op("act", lambda e, i=i: e.activation(sg[i][:], psG[i][:, :], AF.Silu), reads=[tpG[i]], writes=[tsg_[i]])
                    kb.op("dve", lambda e, i=i, fc=fc: e.tensor_tensor(hT[hb][:, fc, :], sg[i][:], psU[i][:, :], ALU.mult),
                          reads=[tsg_[i], tpU[i]], writes=[thT[hb]])

            def YD(st_i, wi, t4, hb):
                b = wi % 2
                e_, g_ = work[wi]
                c0 = st_i * TS + t4 * 512
                for sub in range(4):
                    tsub = t4 * 4 + sub
                    gtile = (c0 + sub * 128) // 128
                    for hf in range(2):
                        j = self.nyc % 4
                        tb = self.nyc % 2
                        self.nyc += 1
                        hs = slice(hf * 512, (hf + 1) * 512)

                        def fy(e, j=j, sub=sub, hs=hs):
                            ins = None
                            nk = GW // 128
                            for k in range(nk):
                                ins = e.matmul(psY[j][:, :], hT[hb][:, k, sub * 128:(sub + 1) * 128], Wd[b][:, k, hs],
                                               start=(k == 0), stop=(k == nk - 1))
                            return ins
                        kb.op("pe", fy, reads=[thT[hb], tWd[b]], writes=[tpY[j]])
                        if moe:
                            kb.op("act", lambda e, j=j, tb=tb: e.activation(tmp[tb][:], psY[j][:, :], AF.Copy, scale=gates[:, gtile, e_:e_ + 1]),
                                  reads=[tpY[j], tgates], writes=[ttmp[tb]])
                        else:
                            kb.op("act", lambda e, j=j, tb=tb: e.copy(tmp[tb][:], psY[j][:, :]), reads=[tpY[j]], writes=[ttmp[tb]])
                        tk = taccs[tsub * 2 + hf]
                        kb.op("dve", lambda e, tb=tb, tsub=tsub, hs=hs: e.tensor_tensor(yacc[:, tsub, hs], yacc[:, tsub, hs], tmp[tb][:], ALU.add),
                              reads=[ttmp[tb], tk], writes=[tk])
            self.ngc, self.nyc = 0, 0
            NT4 = TS // 512
            for st_i in range(T // TS):
                kb.op("pool", lambda e: e.memset(yacc[:], 0.0), writes=taccs)
                w_dma(0)
                w_cast(0)
                units = [(wi, t4) for wi in range(len(work)) for t4 in range(NT4)]
                GU(st_i, 0, 0, 0)
                for ui, (wi, t4) in enumerate(units):
                    if t4 == 0 and wi + 1 < len(work):
                        w_dma(wi + 1)
                    if t4 == 2 and wi + 1 < len(work):
                        w_cast(wi + 1)
                    if ui + 1 < len(units):
                        GU(st_i, units[ui + 1][0], units[ui + 1][1], (ui + 1) % 2)
                    YD(st_i, wi, t4, ui % 2)
                for tsub in range(TS // 128):
                    b = tsub % 2
                    tok0 = st_i * TS + tsub * 128
                    kb.dma("sp", x1[b][:], self.X1.ap()[tok0:tok0 + 128, :], writes=[tx1[b]])
                    tacc = taccs[tsub * 2]
                    kb.op("dve", lambda e, b=b, tsub=tsub: e.scalar_tensor_tensor(yacc[:, tsub, :], x1[b][:], ALPHA, yacc[:, tsub, :], ALU.mult, ALU.add),
                          reads=[tx1[b], taccs[tsub * 2], taccs[tsub * 2 + 1]], writes=[tacc])
                    self.layernorm(yacc[:, tsub, :], tacc, lng[:], lnb[:], tgb, x1[b][:], tx1[b], st, mv, tst)
                    if last:
                        kb.dma("sp", self.y_out.ap()[s * T + tok0:s * T + tok0 + 128, :], x1[b][:], reads=[tx1[b]])
                    else:
                        kb.dma("sp", self.X2.ap()[tok0:tok0 + 128, :], x1[b][:], reads=[tx1[b]])
                        self.transpose_rows(None, x1[b], tx1[b], xT, txT, tok0, psG, tpG, 0)
            kb.barrier()

    def build(self):
        kb = self.kb
        with ExitStack() as es:
            self.load_consts(es)
            self.epsb = self.sb(es, "epsb", [128, 4], F32)
            kb.op("dve", lambda e: e.memset(self.epsb[:, 0:1], RMS_EPS), writes=[self.tc])
            kb.op("dve", lambda e: e.memset(self.epsb[:, 1:2], RMS_EPS * 128), writes=[self.tc])
            kb.op("dve", lambda e: e.memset(self.epsb[:, 2:3], 1.0), writes=[self.tc])
            kb.op("dve", lambda e: e.memset(self.epsb[:, 3:4], LN_EPS), writes=[self.tc])
            xT = self.sb(es, "xT", [128, 8, T], BF16)
            txT = Tok()
            gates = self.sb(es, "gates", [128, T // 128, NE], F32)
            tgates = Tok()
            kb.barrier()
            only = os.environ.get("ONLY")
            for s in range(self.nseq):
                if only:
                    ph, LL = only.split(":")
                    LL = int(LL)
                    kb.op("dve", lambda e: e.memset(xT[:], 0.0), writes=[txT])
                    kb.op("dve", lambda e: e.memset(gates[:], 0.0), writes=[tgates])
                    kb.barrier()
                    {"p1": lambda: self.phase1(LL, s, xT, txT), "p2": lambda: self.phase2(s, xT), "p3": lambda: self.phase3(LL, s, xT),
                     "p4": lambda: self.phase4(LL, s, xT, txT, gates, tgates), "p5": lambda: self.phase5(LL, s, xT, txT, gates, tgates)}[ph]()
                    continue
                self.phase0(s, xT, txT)
                if "XTd" in self.dbg:
                    xtd = self.scr("XTd", [128, 8, T], BF16)
                    kb.dma("sp", xtd.ap()[:, :, :], xT[:], reads=[txT])
                if self.stop_after == "p0":
                    break
                self.rope_tables(s)
                if self.stop_after == "rope":
                    break
                for L in range(2):
                    self.phase1(L, s, xT, txT)
                    if self.stop_after == "p1":
                        break
                    self.phase2(s, xT)
                    if self.stop_after == "p2":
                        break
                    self.phase3(L, s, xT)
                    if self.stop_after == "p3":
                        break
                    self.phase4(L, s, xT, txT, gates, tgates)
                    if self.stop_after == "p4":
                        break
                    self.phase5(L, s, xT, txT, gates, tgates)
                    if self.stop_after == f"p5_{L}":
                        break
                if self.stop_after:
                    break
            kb.barrier()
        return self.nc


_W_KEYS = ("w_in", "a_log", "dt_bias", "dn_norm_w", "w_branch_a", "w_branch_b", "w_out", "ln1_g", "ln1_b", "ln2_g", "ln2_b",
           "ffn_w_gate", "ffn_w_up", "ffn_w_down", "router_w", "moe_w_gate", "moe_w_up", "moe_w_down")


def core_maps(inputs, seq_groups):
    cst = make_consts()
    conv = np.ascontiguousarray(np.asarray(inputs["conv_w"], np.float32).reshape(2, 4, 24, 128).transpose(0, 3, 2, 1))
    ws = {k: np.ascontiguousarray(np.asarray(inputs[k], np.float32)) for k in _W_KEYS}
    x = np.asarray(inputs["x"], np.float32)
    pos = np.asarray(inputs["positions"], np.int32)
    maps = []
    for seqs in seq_groups:
        m = {"x": np.ascontiguousarray(x[seqs].reshape(-1, D)), "pos": np.ascontiguousarray(pos[seqs]), "cst": cst, "conv_w": conv}
        m.update(ws)
        maps.append(m)
    return maps


def kernel(**inputs):
    x = np.asarray(inputs["x"])
    B = x.shape[0]
    n_cores = 8
    per = B // n_cores
    groups = [list(range(c * per, (c + 1) * per)) for c in range(n_cores)]
    prog = Prog(per)
    nc = prog.build()
    res = run_bass_kernel_spmd(nc, core_maps(inputs, groups), core_ids=list(range(n_cores)))
    out = np.concatenate([np.asarray(r["y"], np.float32).reshape(per, T, D) for r in res.results], axis=0)
    return out
```

```python
import math
import os
from contextlib import ExitStack
import numpy as np
import concourse.bass as bass
import concourse.mybir as mybir
from concourse.bass_utils import run_bass_kernel_spmd

F32 = mybir.dt.float32
BF16 = mybir.dt.bfloat16
I32 = mybir.dt.int32
AF = mybir.ActivationFunctionType
ALU = mybir.AluOpType
AX = mybir.AxisListType

D = 1024
T = 4096
NIN = 10768
DFF = 2816
NE = 8
DEX = 3584
ALPHA = 4 ** 0.25
LN_EPS = 1e-5
RMS_EPS = 1e-6
PI = math.pi
A_PAIRS = ((128, 1), (512, 4), (2048, 16))
NDS = 8


class Tok:
    __slots__ = ("w", "r")

    def __init__(self):
        self.w = None
        self.r = {}


class KB:
    def __init__(self, nc):
        self.nc = nc
        self.E = {"pe": nc.tensor, "dve": nc.vector, "act": nc.scalar, "pool": nc.gpsimd, "sp": nc.sync}
        self.csem = {e: nc.alloc_semaphore("cs_" + e) for e in self.E}
        self.ccnt = {e: 0 for e in self.E}
        self.seen = {e: {} for e in self.E}
        self.dq = {q: [[nc.alloc_semaphore(f"ds_{q}{i}"), 0] for i in range(NDS)] for q in ("sp", "pool", "act")}
        self.dnext = {q: 0 for q in self.dq}
        self.ninst = 0

    def _sem(self, ev):
        if ev[0] == "c":
            return self.csem[ev[1]]
        return self.dq[ev[1]][ev[2]][0]

    def _wait(self, e, ev):
        key = ev[:-1]
        val = ev[-1]
        if self.seen[e].get(key, 0) >= val:
            return
        self.E[e].wait_ge(self._sem(ev), val)
        self.seen[e][key] = val
        self.ninst += 1

    def _deps(self, e, reads, writes):
        for t in reads:
            if t.w is not None:
                self._wait(e, t.w)
        for t in writes:
            if t.w is not None and not (e == "pe" and t.w[0] == "c" and t.w[1] == "pe"):
                self._wait(e, t.w)
            for k, ev in t.r.items():
                self._wait(e, ev)

    def _mark(self, ev, reads, writes):
        for t in reads:
            t.r[ev[:-1]] = ev
        for t in writes:
            t.w = ev
            t.r = {}

    def op(self, e, fn, reads=(), writes=()):
        self._deps(e, reads, writes)
        ins = fn(self.E[e])
        self.ccnt[e] += 1
        ins.then_inc(self.csem[e], 1)
        self.ninst += 1
        self._mark(("c", e, self.ccnt[e]), reads, writes)

    def dma(self, q, out, in_, reads=(), writes=()):
        self._deps(q, reads, writes)
        i = self.dnext[q]
        self.dnext[q] = (i + 1) % NDS
        slot = self.dq[q][i]
        if slot[1] > 0:
            self._wait(q, ("d", q, i, slot[1]))
        self.E[q].dma_start(out=out, in_=in_).then_inc(slot[0], 16)
        slot[1] += 16
        self.ninst += 1
        self._mark(("d", q, i, slot[1]), reads, writes)

    def barrier(self):
        for e in self.E:
            for f in self.E:
                if self.ccnt[f] > 0:
                    self._wait(e, ("c", f, self.ccnt[f]))
            for q in self.dq:
                for i, slot in enumerate(self.dq[q]):
                    if slot[1] > 0:
                        self._wait(e, ("d", q, i, slot[1]))


def ss(t0, n, d):
    return slice(t0, t0 + (n - 1) * d + 1, d)


def toks(n):
    return [Tok() for _ in range(n)]


C_ID, C_U, C_LI, C_LS, C_ONE, C_PERM, C_INVF, C_SIGN, C_MBS, C_MBT, C_N = 0, 128, 256, 384, 512, 640, 768, 769, 776, 904, 1032


def make_consts():
    c = np.zeros((128, C_N), np.float32)
    p = np.arange(128)
    c[:, C_ID:C_ID + 128] = np.eye(128)
    c[:, C_U:C_U + 128] = (p[:, None] <= p[None, :])
    c[:, C_LI:C_LI + 128] = (p[:, None] >= p[None, :])
    c[:, C_LS:C_LS + 128] = (p[:, None] > p[None, :])
    c[:, C_ONE:C_ONE + 128] = 1.0
    pm = np.zeros((32, 32), np.float32)
    for m in range(32):
        pm[(m + 16) % 32, m] = 1.0
    c[:32, C_PERM:C_PERM + 32] = pm
    invf = (500000.0 ** (-np.arange(0, 32, 2, dtype=np.float32) / 32)).astype(np.float32)
    c[:32, C_INVF] = np.concatenate([invf, invf])
    c[:16, C_SIGN] = -1.0
    c[16:32, C_SIGN] = 1.0
    c[:, C_MBS:C_MBS + 128] = 30000.0 * (1.0 - (p[:, None] > p[None, :]))
    c[:, C_MBT:C_MBT + 128] = -30000.0 * (1.0 - (p[:, None] <= p[None, :]))
    return c


class Prog:
    def __init__(self, nseq, dbg=None, stop_after=None):
        self.nseq = nseq
        self.dbg = dbg or ()
        self.stop_after = stop_after
        nc = bass.Bass("TRN2", target_bir_lowering=False)
        self.nc = nc
        self.kb = KB(nc)
        NT = nseq * T
        self.NT = NT
        dt = nc.dram_tensor
        I = "ExternalInput"
        self.x_in = dt("x", [NT, D], F32, kind=I)
        self.pos = dt("pos", [nseq, T], I32, kind=I)
        self.cst = dt("cst", [128, C_N], F32, kind=I)
        self.w_in = dt("w_in", [2, D, NIN], F32, kind=I)
        self.conv_w = dt("conv_w", [2, 128, 24, 4], F32, kind=I)
        self.a_log = dt("a_log", [2, 8], F32, kind=I)
        self.dt_bias = dt("dt_bias", [2, 8], F32, kind=I)
        self.dn_norm_w = dt("dn_norm_w", [2, 128], F32, kind=I)
        self.w_a = dt("w_branch_a", [2, 512, D], F32, kind=I)
        self.w_b = dt("w_branch_b", [2, D, D], F32, kind=I)
        self.w_o = dt("w_out", [2, D, D], F32, kind=I)
        self.ln1_g = dt("ln1_g", [2, D], F32, kind=I)
        self.ln1_b = dt("ln1_b", [2, D], F32, kind=I)
        self.ln2_g = dt("ln2_g", [2, D], F32, kind=I)
        self.ln2_b = dt("ln2_b", [2, D], F32, kind=I)
        self.ffn_g = dt("ffn_w_gate", [1, D, DFF], F32, kind=I)
        self.ffn_u = dt("ffn_w_up", [1, D, DFF], F32, kind=I)
        self.ffn_d = dt("ffn_w_down", [1, DFF, D], F32, kind=I)
        self.router = dt("router_w", [1, D, NE], F32, kind=I)
        self.moe_g = dt("moe_w_gate", [1, NE, D, DEX], F32, kind=I)
        self.moe_u = dt("moe_w_up", [1, NE, D, DEX], F32, kind=I)
        self.moe_d = dt("moe_w_down", [1, NE, DEX, D], F32, kind=I)
        self.y_out = dt("y", [NT, D], F32, kind="ExternalOutput")
        self.QKT = self.scr("QKT", [24, 128, T], BF16)
        self.VA = self.scr("VA", [3, 4, 128, 32, 128], BF16)
        self.GQT = self.scr("GQT", [24, 128, T], BF16)
        self.ZT = self.scr("ZT", [8, 128, T], BF16)
        self.GT = self.scr("GT", [16, 128, T], BF16)
        self.BG = self.scr("BG", [T, 16], F32)
        self.YAT = self.scr("YAT", [4, 128, T], BF16)
        self.YBT = self.scr("YBT", [8, 128, T], BF16)
        self.X1 = self.scr("X1", [T, D], F32)
        self.X2 = self.scr("X2", [T, D], F32)
        self.GATES = self.scr("GATES", [T, NE], F32)
        self.CSd = self.scr("CSd", [32, 2, T], F32)

    def scr(self, name, shape, dtype):
        kind = "ExternalOutput" if name in self.dbg else "Internal"
        return self.nc.dram_tensor(name, shape, dtype, kind=kind)

    def sb(self, es, name, shape, dtype):
        self.uid = getattr(self, "uid", 0) + 1
        return es.enter_context(self.nc.sbuf_tensor(f"{name}_{self.uid}", shape, dtype))

    def ps(self, es, name, shape=(128, 512), dtype=F32):
        self.uid = getattr(self, "uid", 0) + 1
        return es.enter_context(self.nc.psum_tensor(f"{name}_{self.uid}", list(shape), dtype))

    def bc_ap(self, handle, offset, n, parts=128):
        return bass.AP(handle, offset, [[0, parts], [1, n]])

    def wload(self, stage, tstage, dst, tdst, src, q="pool"):
        kb = self.kb
        kb.dma(q, stage, src, writes=[tstage])
        kb.op("pool", lambda e: e.tensor_copy(dst, stage), reads=[tstage], writes=[tdst])

    def load_consts(self, es):
        kb = self.kb
        self.c32 = self.sb(es, "c32", [128, C_N], F32)
        self.cbf = self.sb(es, "cbf", [128, C_N], BF16)
        self.tc = Tok()
        kb.dma("sp", self.c32[:], self.cst.ap()[:, :], writes=[self.tc])
        kb.op("dve", lambda e: e.tensor_copy(self.cbf[:], self.c32[:]), reads=[self.tc], writes=[self.tc])

    def transpose_rows(self, es_tag, src_sb, tsrc, xT, txT, col0, psT, tps, idx, x32=None, tx32=None):
        kb = self.kb
        ident = self.c32[:, C_ID:C_ID + 128]
        for hf in range(2):
            p = psT[(2 * idx + hf) % len(psT)]
            tp = tps[(2 * idx + hf) % len(psT)]

            def f(e, hf=hf, p=p):
                ins = None
                for j in range(4):
                    c = hf * 4 + j
                    ins = e.transpose(p[:, j * 128:(j + 1) * 128], src_sb[:, c * 128:(c + 1) * 128], ident)
                return ins
            kb.op("pe", f, reads=[tsrc, self.tc], writes=[tp])
            pv = p[:, :].rearrange("p (j t) -> p j t", j=4)
            if x32 is None:
                kb.op("act", lambda e, hf=hf, pv=pv: e.copy(xT[:, hf * 4:hf * 4 + 4, col0:col0 + 128], pv),
                      reads=[tp], writes=[txT])
            else:
                kb.op("act", lambda e, hf=hf, pv=pv: e.copy(x32[:, hf * 4:hf * 4 + 4, :], pv), reads=[tp], writes=[tx32])
                kb.op("dve", lambda e, hf=hf: e.tensor_copy(xT[:, hf * 4:hf * 4 + 4, col0:col0 + 128], x32[:, hf * 4:hf * 4 + 4, :]),
                      reads=[tx32], writes=[txT])

    def phase0(self, s, xT, txT):
        kb = self.kb
        with ExitStack() as es:
            xin = [self.sb(es, f"p0x{i}", [128, D], F32) for i in range(2)]
            tx = toks(2)
            psT = [self.ps(es, f"p0ps{i}") for i in range(4)]
            tps = toks(4)
            for t in range(T // 128):
                b = t % 2
                r0 = s * T + t * 128
                kb.dma("sp", xin[b][:], self.x_in.ap()[r0:r0 + 128, :], writes=[tx[b]])
                self.transpose_rows(None, xin[b], tx[b], xT, txT, t * 128, psT, tps, t)
            kb.barrier()

    def rope_tables(self, s):
        kb = self.kb
        with ExitStack() as es:
            pi_ = self.sb(es, "rp_i", [32, T], I32)
            ang = self.sb(es, "rp_a", [32, T], F32)
            kf = self.sb(es, "rp_k", [32, T], F32)
            ki = self.sb(es, "rp_ki", [32, T], I32)
            u = self.sb(es, "rp_u", [32, T], F32)
            cr = self.sb(es, "rp_c", [32, T], F32)
            CS = self.sb(es, "rp_CS", [32, 2, T], F32)
            tCS = Tok()
            t1 = Tok()
            kb.dma("sp", pi_[:], self.bc_ap(self.pos, s * T, T, 32), writes=[t1])
            kb.op("dve", lambda e: e.tensor_copy(ang[:], pi_[:]), reads=[t1], writes=[t1])
            kb.op("dve", lambda e: e.tensor_scalar(ang[:], ang[:], self.c32[0:32, C_INVF:C_INVF + 1], None, ALU.mult),
                  reads=[t1, self.tc], writes=[t1])
            t2 = Tok()
            for which in range(2):
                sh = PI / 2 if which == 0 else 0.0
                kb.op("dve", lambda e: e.tensor_scalar(kf[:], ang[:], 1.0 / (2 * PI), sh / (2 * PI), ALU.mult, ALU.add),
                      reads=[t1], writes=[t2])
                kb.op("dve", lambda e: e.tensor_copy(ki[:], kf[:]), reads=[t2], writes=[t2])
                kb.op("dve", lambda e: e.tensor_copy(kf[:], ki[:]), reads=[t2], writes=[t2])
                kb.op("dve", lambda e: e.scalar_tensor_tensor(u[:], kf[:], -2 * PI, ang[:], ALU.mult, ALU.add),
                      reads=[t2, t1], writes=[t2])
                if sh != 0.0:
                    kb.op("dve", lambda e: e.tensor_scalar(u[:], u[:], sh, None, ALU.add), reads=[t2], writes=[t2])
                kb.op("dve", lambda e: e.tensor_scalar(cr[:], u[:], PI, -2 * PI, ALU.is_gt, ALU.mult), reads=[t2], writes=[t2])
                kb.op("dve", lambda e: e.tensor_tensor(u[:], u[:], cr[:], ALU.add), reads=[t2], writes=[t2])
                kb.op("dve", lambda e: e.tensor_scalar(cr[:], u[:], -PI, 2 * PI, ALU.is_lt, ALU.mult), reads=[t2], writes=[t2])
                kb.op("dve", lambda e: e.tensor_tensor(u[:], u[:], cr[:], ALU.add), reads=[t2], writes=[t2])
                kb.op("dve", lambda e: e.tensor_scalar(u[:], u[:], PI, -PI, ALU.min, ALU.max), reads=[t2], writes=[t2])
                if which == 0:
                    kb.op("act", lambda e: e.activation(CS[0:32, 0, :], u[:], AF.Sin), reads=[t2], writes=[tCS])
                else:
                    kb.op("act", lambda e: e.activation(u[:], u[:], AF.Sin), reads=[t2], writes=[t2])
                    kb.op("dve", lambda e: e.tensor_scalar(CS[0:32, 1, :], u[:], self.c32[0:32, C_SIGN:C_SIGN + 1], None, ALU.mult),
                          reads=[t2, self.tc], writes=[tCS])
            kb.dma("sp", self.CSd.ap()[:, :, :], CS[:], reads=[tCS])
            kb.barrier()

    def phase1(self, L, s, xT, txT):
        kb = self.kb
        nc = self.nc
        win = self.w_in.ap()[L]
        NTT = T // 512
        with ExitStack() as es:
            wq = [self.sb(es, f"p1w{i}", [128, 8, 128], BF16) for i in range(2)]
            twq = toks(2)
            stg = [self.sb(es, f"p1s{i}", [128, T], BF16) for i in range(2)]
            tst = toks(2)
            raw = self.sb(es, "p1raw", [128, T + 3], F32)
            traw = Tok()
            acc = self.sb(es, "p1acc", [128, T], F32)
            tacc = Tok()
            h32 = [self.sb(es, f"p1h{i}", [128, 512], F32) for i in range(2)]
            th32 = toks(2)
            r1 = [self.sb(es, f"p1r{i}", [32, 512], F32) for i in range(2)]
            tr1 = toks(2)
            cw = self.sb(es, "p1cw", [128, 24, 4], F32)
            tcw = Tok()
            ps = [self.ps(es, f"p1ps{i}") for i in range(4)]
            tps = toks(4)
            ps2 = [self.ps(es, f"p1pq{i}") for i in range(2)]
            tps2 = toks(2)
            nps = [0]
            CS = self.sb(es, "p1CS", [32, 2, T], F32)
            tCS = Tok()
            kb.dma("sp", CS[:], self.CSd.ap()[:, :, :], writes=[tCS])

            kb.dma("sp", cw[:], self.conv_w.ap()[L], writes=[tcw])
            kb.op("dve", lambda e: e.memset(raw[:, 0:3], 0.0), writes=[traw])

            wst = [self.sb(es, f"p1wst{i}", [128, 8, 128], F32) for i in range(2)]
            twst = toks(2)
            wbig = self.sb(es, "p1wbig", [128, 8, 512], F32)
            twbig = Tok()
            dcols = [7680 + c * 128 for c in range(8)] + [8720 + c * 128 for c in range(16)]
            wcols = [c * 128 for c in range(24)]
            for c in range(24):
                wcols += [4608 + c * 128, dcols[c]]

            def w_dma(ci):
                b = ci % 2
                kb.dma("pool", wst[b][:], win[:, wcols[ci]:wcols[ci] + 128].rearrange("(k p) n -> p k n", p=128), writes=[twst[b]])

            def load_w(ci, col0):
                assert wcols[ci] == col0
                b = ci % 2
                if ci == 0:
                    w_dma(ci)
                if ci + 1 < len(wcols):
                    w_dma(ci + 1)
                kb.op("pool", lambda e: e.tensor_copy(wq[b][:], wst[b][:]), reads=[twst[b]], writes=[twq[b]])
                return wq[b], twq[b]

            def proj_tile(w, tw, tt, ncols=128):
                i = nps[0] % 4
                nps[0] += 1

                def f(e):
                    ins = None
                    for k in range(8):
                        ins = e.matmul(ps[i][0:ncols, :], w[:, k, 0:ncols], xT[:, k, tt * 512:(tt + 1) * 512],
                                       start=(k == 0), stop=(k == 7))
                    return ins
                kb.op("pe", f, reads=[tw, txT], writes=[tps[i]])
                return ps[i], tps[i]

            ci = 0
            tstA = [toks(NTT), toks(NTT)]
            pend = [None]

            def flush():
                if pend[0] is not None:
                    pend[0]()
                    pend[0] = None
            for c in range(24):
                w, tw = load_w(ci, c * 128)
                ci += 1
                sb_ = c % 2
                for tt in range(NTT):
                    p, tp = proj_tile(w, tw, tt)
                    cols = slice(tt * 512, (tt + 1) * 512)
                    hb = (c * NTT + tt) % 2
                    tk = tstA[sb_][tt]
                    kb.op("act", lambda e, p=p, cols=cols: e.copy(stg[sb_][:, cols], p[:, :]), reads=[tp], writes=[tk])
                    kb.op("act", lambda e, p=p, hb=hb: e.copy(h32[hb][0:32, :], p[0:32, :]), reads=[tp], writes=[th32[hb]])
                    flush()

                    def rot(hb=hb, cols=cols, tk=tk, sb_=sb_):
                        kb.op("pe", lambda e: e.matmul(ps2[hb][:, :], self.cbf[:, C_PERM:C_PERM + 128], stg[sb_][:, cols],
                                                       start=True, stop=True), reads=[tk, self.tc], writes=[tps2[hb]])
                        kb.op("dve", lambda e: e.tensor_tensor(r1[hb][:], h32[hb][0:32, :], CS[0:32, 0, cols], ALU.mult),
                              reads=[th32[hb], tCS], writes=[tr1[hb]])
                        kb.op("dve", lambda e: e.tensor_tensor(h32[hb][0:32, :], ps2[hb][0:32, :], CS[0:32, 1, cols], ALU.mult),
                              reads=[tps2[hb], tCS], writes=[th32[hb]])
                        kb.op("dve", lambda e: e.tensor_tensor(stg[sb_][0:32, cols], r1[hb][:], h32[hb][0:32, :], ALU.add),
                              reads=[tr1[hb], th32[hb]], writes=[tk])
                    pend[0] = rot
                flush()
                kb.dma("sp", self.QKT.ap()[c], stg[sb_][:], reads=tstA[sb_])
                tst[sb_].r.update({k_: v_ for t_ in tstA[sb_] for k_, v_ in t_.r.items()})
            wv = self.sb(es, "p1wv", [128, 8, 512], BF16)
            twv = Tok()
            vst = [self.sb(es, f"p1vs{i}", [128, 512], BF16) for i in range(2)]
            tvs = toks(2)
            nb_ = 0
            for g, (win_, dil) in enumerate(A_PAIRS):
                self.wload(wbig[:], twbig, wv[:], twv, win[:, 3072 + g * 512:3072 + (g + 1) * 512].rearrange("(k p) n -> p k n", p=128), q="sp")
                nblk = 32 // dil
                for r in range(dil):
                    for n in range(nblk):
                        blk = r * nblk + n
                        t0 = 128 * n * dil + r
                        i = nps[0] % 4
                        nps[0] += 1

                        def f(e, i=i, t0=t0, dil=dil):
                            ins = None
                            for k in range(8):
                                ins = e.matmul(ps[i][:, :], xT[:, k, ss(t0, 128, dil)], wv[:, k, :],
                                               start=(k == 0), stop=(k == 7))
                            return ins
                        kb.op("pe", f, reads=[twv, txT], writes=[tps[i]])
                        vb = nb_ % 2
                        nb_ += 1
                        kb.op("act", lambda e, i=i, vb=vb: e.copy(vst[vb][:], ps[i][:, :]), reads=[tps[i]], writes=[tvs[vb]])
                        kb.dma("sp", self.VA.ap()[g, :, :, blk, :].rearrange("h p e -> p h e"),
                               vst[vb][:, :].rearrange("p (h e) -> p h e", h=4), reads=[tvs[vb]])
            ci = 24
            for c in range(24):
                w, tw = load_w(ci, 4608 + c * 128)
                ci += 1
                sb_ = 0
                for tt in range(NTT):
                    p, tp = proj_tile(w, tw, tt)
                    kb.op("act", lambda e, p=p, tt=tt: e.copy(raw[:, 3 + tt * 512:3 + (tt + 1) * 512], p[:, :]),
                          reads=[tp], writes=[traw])
                wD, twD = load_w(ci, dcols[c])
                ci += 1

                def dchunk(c=c, w=wD, tw=twD):
                    sb_ = 1
                    fn = AF.Silu if c < 8 else AF.Sigmoid
                    for tt in range(NTT):
                        p, tp = proj_tile(w, tw, tt)
                        kb.op("act", lambda e, p=p, tt=tt, fn=fn: e.activation(stg[sb_][:, tt * 512:(tt + 1) * 512], p[:, :], fn),
                              reads=[tp], writes=[tst[sb_]])
                    dst = self.ZT.ap()[c] if c < 8 else self.GT.ap()[c - 8]
                    kb.dma("sp", dst, stg[sb_][:], reads=[tst[sb_]])
                dchunk()
                sb_ = 0
                for hh in range(2):
                    cs = slice(hh * 2048, (hh + 1) * 2048)
                    kb.op("dve", lambda e, cs=cs, hh=hh: e.tensor_scalar(acc[:, cs], raw[:, hh * 2048:hh * 2048 + 2048],
                                                                          cw[:, c, 0:1], None, ALU.mult),
                          reads=[traw, tcw], writes=[tacc])
                    for j in range(1, 4):
                        kb.op("dve", lambda e, cs=cs, hh=hh, j=j: e.scalar_tensor_tensor(
                            acc[:, cs], raw[:, hh * 2048 + j:hh * 2048 + j + 2048], cw[:, c, j:j + 1], acc[:, cs],
                            ALU.mult, ALU.add), reads=[traw, tcw, tacc], writes=[tacc])
                if c >= 16:
                    kb.op("act", lambda e: e.activation(stg[sb_][:], acc[:], AF.Silu), reads=[tacc], writes=[tst[sb_]])
                else:
                    kb.op("act", lambda e: e.activation(acc[:], acc[:], AF.Silu), reads=[tacc], writes=[tacc])
                    for tt in range(NTT):
                        cols = slice(tt * 512, (tt + 1) * 512)
                        hb = tt % 2
                        sq = raw
                        kb.op("dve", lambda e, cols=cols: e.tensor_tensor(raw[:, cols], acc[:, cols], acc[:, cols], ALU.mult),
                              reads=[tacc], writes=[traw])
                        i = nps[0] % 4
                        nps[0] += 1
                        kb.op("pe", lambda e, i=i, cols=cols: e.matmul(ps[i][:, :], self.c32[:, C_ONE:C_ONE + 128], raw[:, cols],
                                                                       start=True, stop=True),
                              reads=[traw, self.tc], writes=[tps[i]])
                        sc = (1.0 / 128) ** 0.5 if c < 8 else 1.0
                        kb.op("act", lambda e, i=i, cols=cols, sc=sc: e.activation(raw[:, cols], ps[i][:, :], AF.Sqrt,
                                                                                    bias=self.epsb[:, 0:1] if sc == 1.0 else self.epsb[:, 1:2],
                                                                                    scale=1.0 / (sc * sc)),
                              reads=[tps[i], self.tc], writes=[traw])
                        kb.op("dve", lambda e, cols=cols: e.reciprocal(raw[:, cols], raw[:, cols]), reads=[traw], writes=[traw])
                        kb.op("dve", lambda e, cols=cols: e.tensor_tensor(stg[sb_][:, cols], acc[:, cols], raw[:, cols], ALU.mult),
                              reads=[traw, tacc], writes=[tst[sb_]])
                    kb.op("dve", lambda e: e.memset(raw[:, 0:3], 0.0), reads=[traw], writes=[traw])
                kb.dma("sp", self.GQT.ap()[c], stg[sb_][:], reads=[tst[sb_]])
            wbd = self.sb(es, "p1wbd", [128, 8, 16], BF16)
            twbd = Tok()
            self.wload(wbig[:, :, 0:16], twbig, wbd[:], twbd, win[:, 8704:8720].rearrange("(k p) n -> p k n", p=128), q="sp")
            rows = self.sb(es, "p1rows", [128, 16], F32)
            trows = Tok()
            kb.dma("sp", rows[:, 0:8], self.bc_ap(self.dt_bias, L * 8, 8), writes=[trows])
            kb.dma("sp", rows[:, 8:16], self.bc_ap(self.a_log, L * 8, 8), writes=[trows])
            kb.op("act", lambda e: e.activation(rows[:, 8:16], rows[:, 8:16], AF.Exp), reads=[trows], writes=[trows])
            bg = self.sb(es, "p1bg", [128, 32, 16], F32)
            tbg = Tok()
            tmp = self.sb(es, "p1tmp", [128, 8], F32)
            ttmp = Tok()
            for t in range(32):
                i = nps[0] % 4
                nps[0] += 1

                def f(e, i=i, t=t):
                    ins = None
                    for k in range(8):
                        ins = e.matmul(ps[i][:, 0:16], xT[:, k, t * 128:(t + 1) * 128], wbd[:, k, :], start=(k == 0), stop=(k == 7))
                    return ins
                kb.op("pe", f, reads=[twbd, txT], writes=[tps[i]])
                kb.op("act", lambda e, i=i, t=t: e.activation(bg[:, t, 0:8], ps[i][:, 0:8], AF.Sigmoid), reads=[tps[i]], writes=[tbg])
                kb.op("dve", lambda e, i=i: e.tensor_tensor(tmp[:], ps[i][:, 8:16], rows[:, 0:8], ALU.add),
                      reads=[tps[i], trows], writes=[ttmp])
                kb.op("act", lambda e: e.activation(tmp[:], tmp[:], AF.Exp), reads=[ttmp], writes=[ttmp])
                kb.op("act", lambda e: e.activation(tmp[:], tmp[:], AF.Ln, bias=self.epsb[:, 2:3]), reads=[ttmp, self.tc], writes=[ttmp])
                kb.op("dve", lambda e, t=t: e.scalar_tensor_tensor(bg[:, t, 8:16], tmp[:], -1.0, rows[:, 8:16], ALU.mult, ALU.mult),
                      reads=[ttmp, trows], writes=[tbg])
            kb.dma("sp", self.BG.ap().rearrange("(t p) c -> p t c", p=128), bg[:], reads=[tbg])
            kb.barrier()


    def phase2(self, s, xT):
        kb = self.kb
        scale = 128.0 ** -0.5
        with ExitStack() as es:
            QT = [xT[:, g, :] for g in range(3)]
            KT = [xT[:, 3 + g, :] for g in range(3)]
            VV = [self.sb(es, f"p2v{g}", [128, 32, 128], BF16) for g in range(3)]
            tq, tk, tv = toks(3), toks(3), toks(3)
            num = self.sb(es, "p2num", [128, T], F32)
            den = self.sb(es, "p2den", [128, T], F32)
            tnum, tden = Tok(), Tok()
            PT = [self.sb(es, f"p2pt{i}", [128, 256], BF16) for i in range(2)]
            tpt = toks(2)
            yst = self.sb(es, "p2y", [128, T], BF16)
            tyst = Tok()
            psS = [self.ps(es, f"p2pS{i}") for i in range(2)]
            psN = [self.ps(es, f"p2pN{i}") for i in range(2)]
            psD = [self.ps(es, f"p2pD{i}") for i in range(2)]
            tS, tN, tD = toks(2), toks(2), toks(2)
            ones_bf = self.cbf[:, C_ONE:C_ONE + 128]
            maskcat = self.cbf[:, C_U:C_U + 256]
            kbi = 0
            for slot in range(4):
                for g in range(3):
                    kb.dma("sp", QT[g], self.QKT.ap()[g * 4 + slot], writes=[tq[g]])
                    kb.dma("sp", KT[g], self.QKT.ap()[12 + g * 4 + slot], writes=[tk[g]])
                    kb.dma("sp", VV[g][:], self.VA.ap()[g, slot], writes=[tv[g]])
                kb.op("dve", lambda e: e.memset(num[:], 0.0), writes=[tnum])
                kb.op("dve", lambda e: e.memset(den[:], 0.0), writes=[tden])
                for g, (win_, dil) in enumerate(A_PAIRS):
                    nblk = 32 // dil
                    for r in range(dil):
                        for n in range(nblk):
                            blk = r * nblk + n
                            nq = 2 if n + 1 < nblk else 1
                            t0 = 128 * n * dil + r
                            b = kbi % 2
                            kbi += 1
                            kb.op("pe", lambda e, b=b, g=g, t0=t0, nq=nq, dil=dil: e.matmul(
                                psS[b][:, 0:128 * nq], KT[g][:, ss(t0, 128, dil)], QT[g][:, ss(t0, 128 * nq, dil)],
                                start=True, stop=True), reads=[tk[g], tq[g]], writes=[tS[b]])
                            kb.op("act", lambda e, b=b, nq=nq: e.activation(PT[b][:, 0:128 * nq], psS[b][:, 0:128 * nq], AF.Exp,
                                                                            scale=scale), reads=[tS[b]], writes=[tpt[b]])
                            kb.op("dve", lambda e, b=b, nq=nq: e.tensor_tensor(PT[b][:, 0:128 * nq], PT[b][:, 0:128 * nq],
                                                                              maskcat[:, 0:128 * nq], ALU.mult),
                                  reads=[tpt[b], self.tc], writes=[tpt[b]])
                            for mo in range(nq):
                                a = (n + mo) % 2

                                def f(e, a=a, mo=mo, b=b, g=g, blk=blk, n=n):
                                    st = (mo == 1 or n == 0)
                                    sp_ = (mo == 0)
                                    e.matmul(psN[a][:, 0:128], VV[g][:, blk, :], PT[b][:, mo * 128:(mo + 1) * 128], start=st, stop=sp_)
                                    return e.matmul(psD[a][:, 0:128], ones_bf, PT[b][:, mo * 128:(mo + 1) * 128], start=st, stop=sp_)
                                kb.op("pe", f, reads=[tv[g], tpt[b], self.tc], writes=[tN[a], tD[a]])
                            a = n % 2
                            kb.op("dve", lambda e, a=a, t0=t0, dil=dil: e.tensor_tensor(
                                num[:, ss(t0, 128, dil)], num[:, ss(t0, 128, dil)], psN[a][:, 0:128], ALU.add),
                                reads=[tN[a], tnum], writes=[tnum])
                            kb.op("dve", lambda e, a=a, t0=t0, dil=dil: e.tensor_tensor(
                                den[:, ss(t0, 128, dil)], den[:, ss(t0, 128, dil)], psD[a][:, 0:128], ALU.add),
                                reads=[tD[a], tden], writes=[tden])
                kb.op("dve", lambda e: e.reciprocal(den[:], den[:]), reads=[tden], writes=[tden])
                kb.op("dve", lambda e: e.tensor_tensor(yst[:], num[:], den[:], ALU.mult), reads=[tnum, tden], writes=[tyst])
                kb.dma("sp", self.YAT.ap()[slot], yst[:], reads=[tyst])
            kb.barrier()


    def phase3(self, L, s, xT):
        kb = self.kb
        c32, cbf = self.c32, self.cbf
        ident = c32[:, C_ID:C_ID + 128]
        ident_bf = cbf[:, C_ID:C_ID + 128]
        ones = c32[:, C_ONE:C_ONE + 128]
        NCH = int(os.environ.get("P3NCH", "32"))
        NH = int(os.environ.get("P3NH", "8"))
        h2 = lambda ap: ap.rearrange("p (h e) -> p h e", h=2)
        with ExitStack() as es:
            KT2, QT2, VT2, ZT2 = (xT[:, 2 * i:2 * i + 2, :] for i in range(4))
            yb2 = self.sb(es, "p3yb", [128, 2, T], BF16)
            tld = toks(4)
            tyb = Tok()
            if NCH < 32:
                kb.op("dve", lambda e: e.memset(yb2[:], 0.0), writes=[tyb])
            bg = self.sb(es, "p3bg", [128, 32, 16], F32)
            gc, gl, egc, bge, etl, nbt, sda, ngc = (self.sb(es, "p3" + n_, [128, 32, 8], F32)
                                                    for n_ in ("gc", "gl", "egc", "bge", "etl", "nbt", "sda", "ngc"))
            nwc = self.sb(es, "p3nw", [128, 1], F32)
            ID2, MBS2, MBT2 = (self.sb(es, "p3" + n_, [128, 2, 128], F32) for n_ in ("id2", "mbs2", "mbt2"))
            tsm = Tok()
            pT, pG, pA, pB, pU, pV, pO = (self.ps(es, f"p3bk{i}") for i in range(7))
            tT, tG, tA, tB, tU, tV, tO = toks(7)
            for hh in range(2):
                kb.op("dve", lambda e: e.tensor_copy(ID2[:, hh, :], ident), reads=[self.tc], writes=[tsm])
                kb.op("dve", lambda e: e.tensor_copy(MBS2[:, hh, :], c32[:, C_MBS:C_MBS + 128]), reads=[self.tc], writes=[tsm])
                kb.op("dve", lambda e: e.tensor_copy(MBT2[:, hh, :], c32[:, C_MBT:C_MBT + 128]), reads=[self.tc], writes=[tsm])
            kb.dma("sp", bg[:], self.BG.ap().rearrange("(t p) c -> p t c", p=128), writes=[tsm])
            kb.dma("sp", nwc[:], bass.AP(self.dn_norm_w, L * 128, [[1, 128], [1, 1]]), writes=[tsm])
            gsl = bg[:, :, 8:16]
            bsl = bg[:, :, 0:8]
            v3 = lambda ap: ap.rearrange("p (c h) -> p c h", h=8)
            kb.op("pe", lambda e: e.matmul(v3(pT[:, 0:256]), c32[:, C_U:C_U + 128], gsl, start=True, stop=True), reads=[tsm, self.tc], writes=[tT])
            kb.op("pe", lambda e: e.matmul(v3(pG[:, 0:256]), ones, gsl, start=True, stop=True), reads=[tsm, self.tc], writes=[tG])
            kb.op("act", lambda e: e.copy(gc[:], v3(pT[:, 0:256])), reads=[tT], writes=[tsm])
            kb.op("act", lambda e: e.copy(gl[:], v3(pG[:, 0:256])), reads=[tG], writes=[tsm])
            kb.op("act", lambda e: e.activation(egc[:], gc[:], AF.Exp), reads=[tsm], writes=[tsm])
            kb.op("act", lambda e: e.activation(sda[:], gl[:], AF.Exp), reads=[tsm], writes=[tsm])
            kb.op("dve", lambda e: e.tensor_tensor(bge[:], egc[:], bsl, ALU.mult), reads=[tsm], writes=[tsm])
            kb.op("dve", lambda e: e.tensor_tensor(etl[:], gl[:], gc[:], ALU.subtract), reads=[tsm], writes=[tsm])
            kb.op("act", lambda e: e.activation(etl[:], etl[:], AF.Exp), reads=[tsm], writes=[tsm])
            kb.op("dve", lambda e: e.tensor_scalar(nbt[:], bsl, -1.0, None, ALU.mult), reads=[tsm], writes=[tsm])
            kb.op("dve", lambda e: e.tensor_scalar(ngc[:], gc[:], -1.0, None, ALU.mult), reads=[tsm], writes=[tsm])
            kb.op("dve", lambda e: e.tensor_scalar(nwc[:], nwc[:], 128.0 ** 0.5, None, ALU.mult), reads=[tsm], writes=[tsm])

            def S(name, shape, dtype):
                return self.sb(es, "p3" + name, shape, dtype)
            Sst, Ug, dmS, dmT, eS, eT, eg, u_, sq, rinv, y1 = (S(n_, [128, 2, 128], F32) for n_ in
                                                               ("S", "Ug", "dmS", "dmT", "eS", "eT", "eg", "u", "sq", "ri", "y1"))
            Qm = [S("Q0", [128, 2, 128], F32), S("Q1", [128, 2, 128], F32)]
            PY = [S("PY0", [128, 2, 256], F32), S("PY1", [128, 2, 256], F32)]
            Sbf, kbg, ktl, vb_, TT, AT, wT, qdT, vnew = (S(n_, [128, 2, 128], BF16) for n_ in
                                                         ("Sb", "kbg", "ktl", "vb", "TT", "AT", "wT", "qdT", "vn"))
            tS, tUg, tdmS, tdmT, teS, teT, teg, tkbg, tktl, tvb, tTT, tAT, twT, tqdT, tvn, tu, tsq, tri, ty1 = toks(19)
            tPY, tQ = toks(2), toks(2)
            pA3 = h2(pA[:, :])
            for h0 in range(0, NH, 2):
                hsl = lambda t, o: t.ap()[o + h0:o + h0 + 2].rearrange("h p t -> p h t")
                kb.dma("sp", KT2, hsl(self.GQT, 8), writes=[tld[0]])
                kb.dma("sp", QT2, hsl(self.GQT, 0), writes=[tld[1]])
                kb.dma("sp", VT2, hsl(self.GQT, 16), writes=[tld[2]])
                kb.dma("sp", ZT2, hsl(self.ZT, 0), writes=[tld[3]])
                kb.op("dve", lambda e: e.memset(Sst[:], 0.0), writes=[tS])
                kb.op("dve", lambda e: e.memset(Sbf[:], 0.0), writes=[tS])
                for c in range(NCH):
                    cols = slice(c * 128, (c + 1) * 128)
                    col = lambda t, hh: t[:, c, h0 + hh:h0 + hh + 1]
                    sl = lambda hh: slice(hh * 128, (hh + 1) * 128)
                    def ftr(e):
                        ins = None
                        for hh in range(2):
                            e.matmul(pT[:, sl(hh)], KT2[:, hh, cols], ident_bf, start=True, stop=True)
                            ins = e.matmul(pT[:, sl(2 + hh)], VT2[:, hh, cols], ident_bf, start=True, stop=True)
                        return ins
                    kb.op("pe", ftr, reads=[tld[0], tld[2], self.tc], writes=[tT])
                    for hh in range(2):
                        kb.op("act", lambda e: e.activation(kbg[:, hh, :], pT[:, sl(hh)], AF.Copy, scale=col(bge, hh)), reads=[tT, tsm], writes=[tkbg])
                        kb.op("act", lambda e: e.activation(ktl[:, hh, :], pT[:, sl(hh)], AF.Copy, scale=col(etl, hh)), reads=[tT, tsm], writes=[tktl])
                        kb.op("act", lambda e: e.activation(vb_[:, hh, :], pT[:, sl(2 + hh)], AF.Copy, scale=col(bsl, hh)), reads=[tT, tsm], writes=[tvb])
                    for hh in range(2):
                        kb.op("dve", lambda e: e.tensor_scalar(Ug[:, hh, :], c32[:, C_U:C_U + 128], col(gsl, hh), None, ALU.mult),
                              reads=[tsm, self.tc], writes=[tUg])

                    def fg(e):
                        e.matmul(pG[:, sl(0)], ones, Ug[:, 0, :], start=True, stop=True)
                        return e.matmul(pG[:, sl(1)], ones, Ug[:, 1, :], start=True, stop=True)
                    kb.op("pe", fg, reads=[tUg, self.tc], writes=[tG])
                    kb.op("act", lambda e: e.activation(eg[:], h2(pG[:, 0:256]), AF.Exp), reads=[tG], writes=[teg])
                    for hh in range(2):
                        kb.op("act", lambda e: e.activation(Ug[:, hh, :], pG[:, sl(hh)], AF.Identity, bias=col(ngc, hh), scale=1.0),
                              reads=[tG, tsm], writes=[tUg])
                    kb.op("dve", lambda e: e.tensor_tensor(dmS[:], Ug[:], MBS2[:], ALU.max), reads=[tUg, tsm], writes=[tdmS])
                    kb.op("dve", lambda e: e.tensor_tensor(dmT[:], Ug[:], MBT2[:], ALU.min), reads=[tUg, tsm], writes=[tdmT])
                    kb.op("act", lambda e: e.activation(eS[:], dmS[:], AF.Exp, scale=-1.0), reads=[tdmS], writes=[teS])
                    kb.op("act", lambda e: e.activation(eT[:], dmT[:], AF.Exp), reads=[tdmT], writes=[teT])

                    def fkk(e):
                        e.matmul(pG[:, sl(2)], KT2[:, 0, cols], KT2[:, 0, cols], start=True, stop=True)
                        return e.matmul(pG[:, sl(3)], KT2[:, 1, cols], KT2[:, 1, cols], start=True, stop=True)
                    kb.op("pe", fkk, reads=[tld[0]], writes=[tG])
                    kb.op("dve", lambda e: e.tensor_tensor(eS[:], h2(pG[:, 256:512]), eS[:], ALU.mult), reads=[tG, teS], writes=[teS])
                    for hh in range(2):
                        kb.op("dve", lambda e: e.tensor_scalar(Qm[0][:, hh, :], eS[:, hh, :], col(nbt, hh), None, ALU.mult), reads=[tsm, teS], writes=[tQ[0]])

                    def ftn(e):
                        e.transpose(pB[:, sl(0)], Qm[0][:, 0, :], ident)
                        return e.transpose(pB[:, sl(1)], Qm[0][:, 1, :], ident)
                    kb.op("pe", ftn, reads=[tQ[0], self.tc], writes=[tB])
                    kb.op("act", lambda e: e.copy(PY[0][:, :, 0:128], h2(pB[:, 0:256])), reads=[tB], writes=[tPY[0]])
                    kb.op("dve", lambda e: e.tensor_tensor(PY[0][:, :, 128:256], h2(pB[:, 0:256]), ID2[:], ALU.add), reads=[tB, tsm], writes=[tPY[0]])
                    for k in range(7):
                        a, b = k % 2, (k + 1) % 2

                        def fa_(e, k=k, a=a):
                            ins = None
                            for hh in range(2):
                                o = hh * 256
                                if k == 0:
                                    ins = e.matmul(pA[:, o:o + 128], Qm[a][:, hh, :], PY[a][:, hh, 0:128], start=True, stop=True)
                                elif k < 6:
                                    ins = e.matmul(pA[:, o:o + 256], Qm[a][:, hh, :], PY[a][:, hh, 0:256], start=True, stop=True)
                                else:
                                    ins = e.matmul(pA[:, o + 128:o + 256], Qm[a][:, hh, :], PY[a][:, hh, 128:256], start=True, stop=True)
                            return ins
                        kb.op("pe", fa_, reads=[tQ[a], tPY[a]], writes=[tA])
                        if k < 6:
                            def fb_(e, a=a):
                                e.matmul(pB[:, sl(0)], PY[a][:, 0, 0:128], Qm[a][:, 0, :], start=True, stop=True)
                                return e.matmul(pB[:, sl(1)], PY[a][:, 1, 0:128], Qm[a][:, 1, :], start=True, stop=True)
                            kb.op("pe", fb_, reads=[tQ[a], tPY[a]], writes=[tB])
                            kb.op("act", lambda e: e.copy(PY[b][:, :, 0:128], pA3[:, :, 0:128]), reads=[tA], writes=[tPY[b]])
                            kb.op("act", lambda e: e.copy(Qm[b][:], h2(pB[:, 0:256])), reads=[tB], writes=[tQ[b]])
                            if k == 0:
                                kb.op("dve", lambda e: e.tensor_copy(PY[b][:, :, 128:256], PY[a][:, :, 128:256]), reads=[tPY[a]], writes=[tPY[b]])
                            else:
                                kb.op("dve", lambda e: e.tensor_tensor(PY[b][:, :, 128:256], PY[a][:, :, 128:256], pA3[:, :, 128:256], ALU.add),
                                      reads=[tPY[a], tA], writes=[tPY[b]])
                        else:
                            kb.op("dve", lambda e: e.tensor_tensor(TT[:], PY[a][:, :, 128:256], pA3[:, :, 128:256], ALU.add),
                                  reads=[tPY[a], tA], writes=[tTT])

                    def fkq(e):
                        e.matmul(pG[:, sl(2)], KT2[:, 0, cols], QT2[:, 0, cols], start=True, stop=True)
                        return e.matmul(pG[:, sl(3)], KT2[:, 1, cols], QT2[:, 1, cols], start=True, stop=True)
                    kb.op("pe", fkq, reads=[tld[0], tld[1]], writes=[tG])
                    kb.op("dve", lambda e: e.tensor_tensor(AT[:], h2(pG[:, 256:512]), eT[:], ALU.mult), reads=[tG, teT], writes=[tAT])

                    def fuw(e):
                        ins = None
                        for hh in range(2):
                            e.matmul(pU[:, sl(hh)], TT[:, hh, :], vb_[:, hh, :], start=True, stop=True)
                            ins = e.matmul(pU[:, sl(2 + hh)], kbg[:, hh, :], TT[:, hh, :], start=True, stop=True)
                        return ins
                    kb.op("pe", fuw, reads=[tTT, tvb, tkbg], writes=[tU])
                    kb.op("act", lambda e: e.copy(u_[:], h2(pU[:, 0:256])), reads=[tU], writes=[tu])
                    kb.op("act", lambda e: e.copy(wT[:], h2(pU[:, 256:512])), reads=[tU], writes=[twT])
                    kb.op("dve", lambda e: e.tensor_tensor(qdT[:], QT2[:, :, cols], eg[:], ALU.mult), reads=[tld[1], teg], writes=[tqdT])

                    def fv(e):
                        e.matmul(pV[:, sl(0)], wT[:, 0, :], Sbf[:, 0, :], start=True, stop=True)
                        return e.matmul(pV[:, sl(1)], wT[:, 1, :], Sbf[:, 1, :], start=True, stop=True)
                    kb.op("pe", fv, reads=[twT, tS], writes=[tV])
                    kb.op("dve", lambda e: e.tensor_tensor(vnew[:], u_[:], h2(pV[:, 0:256]), ALU.subtract), reads=[tu, tV], writes=[tvn])

                    def fo(e):
                        ins = None
                        for hh in range(2):
                            e.matmul(pO[:, sl(hh)], Sbf[:, hh, :], qdT[:, hh, :], start=True, stop=False)
                            ins = e.matmul(pO[:, sl(hh)], vnew[:, hh, :], AT[:, hh, :], start=False, stop=True)
                        return ins
                    kb.op("pe", fo, reads=[tS, tqdT, tvn, tAT], writes=[tO])

                    def fs(e):
                        e.matmul(pV[:, sl(2)], ktl[:, 0, :], vnew[:, 0, :], start=True, stop=True)
                        return e.matmul(pV[:, sl(3)], ktl[:, 1, :], vnew[:, 1, :], start=True, stop=True)
                    kb.op("pe", fs, reads=[tktl, tvn], writes=[tV])
                    for hh in range(2):
                        kb.op("dve", lambda e: e.tensor_scalar(Sst[:, hh, :], Sst[:, hh, :], col(sda, hh), None, ALU.mult), reads=[tS, tsm], writes=[tS])
                    kb.op("dve", lambda e: e.tensor_tensor(Sst[:], Sst[:], h2(pV[:, 256:512]), ALU.add), reads=[tS, tV], writes=[tS])
                    kb.op("act", lambda e: e.copy(Sbf[:], Sst[:]), reads=[tS], writes=[tS])
                    kb.op("act", lambda e: e.activation(sq[:], h2(pO[:, 0:256]), AF.Square), reads=[tO], writes=[tsq])

                    def fq(e):
                        e.matmul(pO[:, sl(2)], ones, sq[:, 0, :], start=True, stop=True)
                        return e.matmul(pO[:, sl(3)], ones, sq[:, 1, :], start=True, stop=True)
                    kb.op("pe", fq, reads=[tsq, self.tc], writes=[tO])
                    kb.op("act", lambda e: e.activation(rinv[:], h2(pO[:, 256:512]), AF.Sqrt, bias=self.epsb[:, 1:2], scale=1.0), reads=[tO, self.tc], writes=[tri])
                    kb.op("dve", lambda e: e.reciprocal(rinv[:], rinv[:]), reads=[tri], writes=[tri])
                    kb.op("dve", lambda e: e.tensor_tensor(y1[:], h2(pO[:, 0:256]), rinv[:], ALU.mult), reads=[tO, tri], writes=[ty1])
                    kb.op("dve", lambda e: e.scalar_tensor_tensor(yb2[:, :, cols], y1[:], nwc[:, 0:1], ZT2[:, :, cols], ALU.mult, ALU.mult),
                          reads=[ty1, tsm, tld[3]], writes=[tyb])
                kb.dma("sp", self.YBT.ap()[h0:h0 + 2].rearrange("h p t -> p h t"), yb2[:], reads=[tyb])
            kb.barrier()

    def layernorm(self, pre, tpre, g, b, tgb, out, tout, st, mv, tst):
        kb = self.kb

        def f(e):
            e.bn_stats(st[:, 0:6], pre[:, 0:512])
            return e.bn_stats(st[:, 6:12], pre[:, 512:1024])
        kb.op("dve", f, reads=[tpre], writes=[tst])
        kb.op("dve", lambda e: e.bn_aggr(mv[:, 0:2], st[:, 0:12]), reads=[tst], writes=[tst])
        kb.op("act", lambda e: e.activation(mv[:, 2:3], mv[:, 1:2], AF.Sqrt, bias=self.epsb[:, 3:4]), reads=[tst, self.tc], writes=[tst])
        kb.op("dve", lambda e: e.reciprocal(mv[:, 2:3], mv[:, 2:3]), reads=[tst], writes=[tst])
        kb.op("dve", lambda e: e.tensor_scalar(out, pre, mv[:, 0:1], mv[:, 2:3], ALU.subtract, ALU.mult), reads=[tpre, tst], writes=[tout])
        kb.op("dve", lambda e: e.tensor_tensor(out, out, g, ALU.mult), reads=[tgb], writes=[tout])
        kb.op("dve", lambda e: e.tensor_tensor(out, out, b, ALU.add), reads=[tgb], writes=[tout])

    def phase4(self, L, s, xT, txT, gates, tgates):
        kb = self.kb
        c32 = self.c32
        xres_src = self.x_in.ap()[s * T:(s + 1) * T, :] if L == 0 else self.X2.ap()
        moe = (L == 1)
        with ExitStack() as es:
            Wa = self.sb(es, "p4wa", [128, 4, D], BF16)
            Wb = self.sb(es, "p4wb", [128, 8, D], BF16)
            Wo = self.sb(es, "p4wo", [128, 8, D], BF16)
            tW = Tok()
            stage = self.sb(es, "p4stg", [128, 8, 512], F32)
            tstage = Tok()
            for hf in range(2):
                hs = slice(hf * 512, (hf + 1) * 512)
                self.wload(stage[:, 0:4, :], tstage, Wa[:, :, hs], tW, self.w_a.ap()[L][:, hs].rearrange("(k p) n -> p k n", p=128), q="sp")
                self.wload(stage[:], tstage, Wb[:, :, hs], tW, self.w_b.ap()[L][:, hs].rearrange("(k p) n -> p k n", p=128), q="sp")
                self.wload(stage[:], tstage, Wo[:, :, hs], tW, self.w_o.ap()[L][:, hs].rearrange("(k p) n -> p k n", p=128), q="sp")
            lng = self.sb(es, "p4lng", [128, D], F32)
            lnb = self.sb(es, "p4lnb", [128, D], F32)
            tgb = Tok()
            kb.dma("sp", lng[:], self.bc_ap(self.ln1_g, L * D, D), writes=[tgb])
            kb.dma("sp", lnb[:], self.bc_ap(self.ln1_b, L * D, D), writes=[tgb])
            if moe:
                Wr = self.sb(es, "p4wr", [128, 8, NE], F32)
                kb.dma("sp", Wr[:], self.router.ap()[0].rearrange("(k p) n -> p k n", p=128), writes=[tgb])
                x32 = self.sb(es, "p4x32", [128, 8, 128], F32)
                tx32 = Tok()
                rt = self.sb(es, "p4rt", [128, 64], F32)
                trt = Tok()
            ya = self.sb(es, "p4ya", [128, 4, 512], BF16)
            yb = self.sb(es, "p4yb", [128, 8, 512], BF16)
            gt = self.sb(es, "p4gt", [128, 16, 512], BF16)
            xr = self.sb(es, "p4xr", [128, 4, D], F32)
            tya, tyb, tgt, txr = toks(4)
            mT = self.sb(es, "p4mT", [128, 8, 512], BF16)
            tmT = Tok()
            m1 = [self.sb(es, f"p4m1{i}", [128, 512], F32) for i in range(1)] * 2
            m2 = [self.sb(es, f"p4m2{i}", [128, 512], F32) for i in range(1)] * 2
            tm1, tm2 = toks(1) * 2, toks(1) * 2
            pre = [self.sb(es, f"p4pre{i}", [128, D], F32) for i in range(1)] * 2
            x1t = [self.sb(es, f"p4x1{i}", [128, D], F32) for i in range(2)]
            tpre, tx1 = toks(1) * 2, toks(2)
            st = self.sb(es, "p4st", [128, 12], F32)
            mv = self.sb(es, "p4mv", [128, 4], F32)
            tst = Tok()
            psA = [self.ps(es, f"p4pA{i}") for i in range(2)]
            psB = [self.ps(es, f"p4pB{i}") for i in range(2)]
            psO = [self.ps(es, f"p4pO{i}") for i in range(2)]
            psT = [self.ps(es, f"p4pT{i}") for i in range(2)]
            tpA, tpB, tpO, tpT = toks(2), toks(2), toks(2), toks(2)
            no = 0
            for tt in range(T // 512):
                cs = slice(tt * 512, (tt + 1) * 512)
                kb.dma("sp", ya[:], self.YAT.ap()[:, :, cs].rearrange("k p t -> p k t"), writes=[tya])
                kb.dma("sp", yb[:], self.YBT.ap()[:, :, cs].rearrange("k p t -> p k t"), writes=[tyb])
                kb.dma("sp", gt[:], self.GT.ap()[:, :, cs].rearrange("k p t -> p k t"), writes=[tgt])
                kb.dma("sp", xr[:], xres_src[tt * 512:(tt + 1) * 512, :].rearrange("(j p) d -> p j d", p=128), writes=[txr])
                kb.op("act", lambda e: e.mul(xr[:], xr[:], ALPHA), reads=[txr], writes=[txr])
                for dc in range(8):
                    i = dc % 2
                    ds_ = slice(dc * 128, (dc + 1) * 128)

                    def fa(e, i=i, ds_=ds_):
                        ins = None
                        for k in range(4):
                            ins = e.matmul(psA[i][:, :], Wa[:, k, ds_], ya[:, k, :], start=(k == 0), stop=(k == 3))
                        return ins

                    def fb(e, i=i, ds_=ds_):
                        ins = None
                        for k in range(8):
                            ins = e.matmul(psB[i][:, :], Wb[:, k, ds_], yb[:, k, :], start=(k == 0), stop=(k == 7))
                        return ins
                    kb.op("pe", fa, reads=[tW, tya], writes=[tpA[i]])
                    kb.op("pe", fb, reads=[tW, tyb], writes=[tpB[i]])
                    kb.op("dve", lambda e, i=i, dc=dc: e.tensor_tensor(m1[i][:], psA[i][:, :], gt[:, dc, :], ALU.mult), reads=[tpA[i], tgt], writes=[tm1[i]])
                    kb.op("dve", lambda e, i=i, dc=dc: e.tensor_tensor(m2[i][:], psB[i][:, :], gt[:, 8 + dc, :], ALU.mult), reads=[tpB[i], tgt], writes=[tm2[i]])
                    kb.op("pool", lambda e, i=i, dc=dc: e.tensor_tensor(mT[:, dc, :], m1[i][:], m2[i][:], ALU.add), reads=[tm1[i], tm2[i]], writes=[tmT])
                for sub in range(4):
                    b = no % 2
                    no += 1
                    tok0 = tt * 512 + sub * 128
                    for hf in range(2):
                        hs = slice(hf * 512, (hf + 1) * 512)

                        def fo(e, hf=hf, hs=hs, sub=sub):
                            ins = None
                            for k in range(8):
                                ins = e.matmul(psO[hf][:, :], mT[:, k, sub * 128:(sub + 1) * 128], Wo[:, k, hs], start=(k == 0), stop=(k == 7))
                            return ins
                        kb.op("pe", fo, reads=[tW, tmT], writes=[tpO[hf]])
                        kb.op("dve", lambda e, hf=hf, hs=hs, b=b, sub=sub: e.tensor_tensor(pre[b][:, hs], psO[hf][:, :], xr[:, sub, hs], ALU.add),
                              reads=[tpO[hf], txr], writes=[tpre[b]])
                    self.layernorm(pre[b][:], tpre[b], lng[:], lnb[:], tgb, x1t[b][:], tx1[b], st, mv, tst)
                    kb.dma("sp", self.X1.ap()[tok0:tok0 + 128, :], x1t[b][:], reads=[tx1[b]])
                    if moe:
                        self.transpose_rows(None, x1t[b], tx1[b], xT, txT, tok0, psT, tpT, 0, x32=x32, tx32=tx32)
                        self.router_gates(x32, tx32, Wr, tgb, rt, trt, psA[0], tpA[0], gates[:, tok0 // 128, :], tgates)
                    else:
                        self.transpose_rows(None, x1t[b], tx1[b], xT, txT, tok0, psT, tpT, 0)
            kb.barrier()

    def router_gates(self, x32, tx32, Wr, tWr, rt, trt, ps, tps, gout, tgout):
        kb = self.kb

        def f(e):
            ins = None
            for k in range(8):
                ins = e.matmul(ps[:, 0:NE], x32[:, k, :], Wr[:, k, :], start=(k == 0), stop=(k == 7))
            return ins
        kb.op("pe", f, reads=[tx32, tWr], writes=[tps])
        lg, eq1, lg2, eq2, g1 = (rt[:, i * 8:(i + 1) * 8] for i in range(5))
        m1, m2, d_, w1, w2 = (rt[:, 40 + i:41 + i] for i in range(5))
        kb.op("act", lambda e: e.copy(lg, ps[:, 0:NE]), reads=[tps], writes=[trt])
        kb.op("dve", lambda e: e.reduce_max(m1, lg, AX.X), reads=[trt], writes=[trt])
        kb.op("dve", lambda e: e.tensor_scalar(eq1, lg, m1, None, ALU.is_equal), reads=[trt], writes=[trt])
        kb.op("dve", lambda e: e.scalar_tensor_tensor(lg2, eq1, -1e30, lg, ALU.mult, ALU.add), reads=[trt], writes=[trt])
        kb.op("dve", lambda e: e.reduce_max(m2, lg2, AX.X), reads=[trt], writes=[trt])
        kb.op("dve", lambda e: e.tensor_scalar(eq2, lg2, m2, None, ALU.is_equal), reads=[trt], writes=[trt])
        kb.op("dve", lambda e: e.tensor_tensor(d_, m2, m1, ALU.subtract), reads=[trt], writes=[trt])
        kb.op("act", lambda e: e.activation(d_, d_, AF.Exp), reads=[trt], writes=[trt])
        kb.op("dve", lambda e: e.tensor_scalar(w1, d_, 1.0, None, ALU.add), reads=[trt], writes=[trt])
        kb.op("dve", lambda e: e.reciprocal(w1, w1), reads=[trt], writes=[trt])
        kb.op("dve", lambda e: e.tensor_tensor(w2, d_, w1, ALU.mult), reads=[trt], writes=[trt])
        kb.op("dve", lambda e: e.tensor_scalar(g1, eq1, w1, None, ALU.mult), reads=[trt], writes=[trt])
        kb.op("dve", lambda e: e.scalar_tensor_tensor(gout, eq2, w2, g1, ALU.mult, ALU.add), reads=[trt], writes=[tgout])

    def phase5(self, L, s, xT, txT, gates, tgates):
        kb = self.kb
        c32 = self.c32
        moe = (L == 1)
        ne = NE if moe else 1
        dff = DEX if moe else DFF
        GW = 256
        ngr = dff // GW
        TS = 2048
        last = (L == 1)

        def wsrc(which, e_, g_):
            c0 = g_ * GW
            if moe:
                base = {"g": self.moe_g, "u": self.moe_u, "d": self.moe_d}[which].ap()[0][e_]
            else:
                base = {"g": self.ffn_g, "u": self.ffn_u, "d": self.ffn_d}[which].ap()[0]
            if which == "d":
                return base[c0:c0 + GW, :].rearrange("(k p) n -> p k n", p=128)
            return base[:, c0:c0 + GW].rearrange("(k p) n -> p k n", p=128)
        with ExitStack() as es:
            yacc = self.sb(es, "p5acc", [128, TS // 128, D], F32)
            taccs = toks(2 * TS // 128)
            stg_g = self.sb(es, "p5sg", [128, 8, GW], F32)
            stg_u = self.sb(es, "p5su", [128, 8, GW], F32)
            stg_d = self.sb(es, "p5sd", [128, GW // 128, D], F32)
            tsg, tsu, tsd = toks(3)
            Wg = [self.sb(es, f"p5wg{i}", [128, 8, GW], BF16) for i in range(2)]
            Wu = [self.sb(es, f"p5wu{i}", [128, 8, GW], BF16) for i in range(2)]
            Wd = [self.sb(es, f"p5wd{i}", [128, GW // 128, D], BF16) for i in range(2)]
            tWg, tWu, tWd = toks(2), toks(2), toks(2)
            hT = [self.sb(es, f"p5h{i}", [128, GW // 128, 512], BF16) for i in range(2)]
            thT = toks(2)
            sg = [self.sb(es, f"p5s{i}", [128, 512], BF16) for i in range(2)]
            tsg_ = toks(2)
            tmp = [self.sb(es, f"p5t{i}", [128, 512], F32) for i in range(2)]
            ttmp = toks(2)
            lng = self.sb(es, "p5lng", [128, D], F32)
            lnb = self.sb(es, "p5lnb", [128, D], F32)
            tgb = Tok()
            kb.dma("sp", lng[:], self.bc_ap(self.ln2_g, L * D, D), writes=[tgb])
            kb.dma("sp", lnb[:], self.bc_ap(self.ln2_b, L * D, D), writes=[tgb])
            x1 = [self.sb(es, f"p5x1{i}", [128, D], F32) for i in range(1)] * 2
            tx1 = toks(1) * 2
            st = self.sb(es, "p5st", [128, 12], F32)
            mv = self.sb(es, "p5mv", [128, 4], F32)
            tst = Tok()
            psG = [self.ps(es, f"p5pG{i}") for i in range(2)]
            psU = [self.ps(es, f"p5pU{i}") for i in range(2)]
            psY = [self.ps(es, f"p5pY{i}") for i in range(4)]
            tpG, tpU, tpY = toks(2), toks(2), toks(4)
            work = [(e_, g_) for e_ in range(ne) for g_ in range(ngr)]

            def w_dma(wi):
                e_, g_ = work[wi]
                kb.dma("sp", stg_g[:], wsrc("g", e_, g_), writes=[tsg])
                kb.dma("sp", stg_u[:], wsrc("u", e_, g_), writes=[tsu])
                kb.dma("sp", stg_d[:], wsrc("d", e_, g_), writes=[tsd])

            def w_cast(wi):
                b = wi % 2
                kb.op("act", lambda e: e.copy(Wg[b][:], stg_g[:]), reads=[tsg], writes=[tWg[b]])
                kb.op("act", lambda e: e.copy(Wu[b][:], stg_u[:]), reads=[tsu], writes=[tWu[b]])
                kb.op("act", lambda e: e.copy(Wd[b][:], stg_d[:]), reads=[tsd], writes=[tWd[b]])

            def GU(st_i, wi, t4, hb):
                b = wi % 2
                c0 = st_i * TS + t4 * 512
                for fc in range(GW // 128):
                    i = self.ngc % 2
                    self.ngc += 1
                    fs = slice(fc * 128, (fc + 1) * 128)

                    def fg(e, i=i, fs=fs):
                        ins = None
                        for k in range(8):
                            ins = e.matmul(psG[i][:, :], Wg[b][:, k, fs], xT[:, k, c0:c0 + 512], start=(k == 0), stop=(k == 7))
                        return ins

                    def fu(e, i=i, fs=fs):
                        ins = None
                        for k in range(8):
                            ins = e.matmul(psU[i][:, :], Wu[b][:, k, fs], xT[:, k, c0:c0 + 512], start=(k == 0), stop=(k == 7))
                        return ins
                    kb.op("pe", fg, reads=[tWg[b], txT], writes=[tpG[i]])
                    kb.op("pe", fu, reads=[tWu[b], txT], writes=[tpU[i]])
                    kb.op("act", lambda e, i=i: e.activation(sg[i][:], psG[i][:, :], AF.Silu), reads=[tpG[i]], writes=[tsg_[i]])
                    kb.op("dve", lambda e, i=i, fc=fc: e.tensor_tensor(hT[hb][:, fc, :], sg[i][:], psU[i][:, :], ALU.mult),
                          reads=[tsg_[i], tpU[i]], writes=[thT[hb]])

            def YD(st_i, wi, t4, hb):
                b = wi % 2
                e_, g_ = work[wi]
                c0 = st_i * TS + t4 * 512
                for sub in range(4):
                    tsub = t4 * 4 + sub
                    gtile = (c0 + sub * 128) // 128
                    for hf in range(2):
                        j = self.nyc % 4
                        tb = self.nyc % 2
                        self.nyc += 1
                        hs = slice(hf * 512, (hf + 1) * 512)

                        def fy(e, j=j, sub=sub, hs=hs):
                            ins = None
                            nk = GW // 128
                            for k in range(nk):
                                ins = e.matmul(psY[j][:, :], hT[hb][:, k, sub * 128:(sub + 1) * 128], Wd[b][:, k, hs],
                                               start=(k == 0), stop=(k == nk - 1))
                            return ins
                        kb.op("pe", fy, reads=[thT[hb], tWd[b]], writes=[tpY[j]])
                        if moe:
                            kb.op("act", lambda e, j=j, tb=tb: e.activation(tmp[tb][:], psY[j][:, :], AF.Copy, scale=gates[:, gtile, e_:e_ + 1]),
                                  reads=[tpY[j], tgates], writes=[ttmp[tb]])
                        else:
                            kb.op("act", lambda e, j=j, tb=tb: e.copy(tmp[tb][:], psY[j][:, :]), reads=[tpY[j]], writes=[ttmp[tb]])
                        tk = taccs[tsub * 2 + hf]
                        kb.op("dve", lambda e, tb=tb, tsub=tsub, hs=hs: e.tensor_tensor(yacc[:, tsub, hs], yacc[:, tsub, hs], tmp[tb][:], ALU.add),
                              reads=[ttmp[tb], tk], writes=[tk])
            self.ngc, self.nyc = 0, 0
            NT4 = TS // 512
            for st_i in range(T // TS):
                kb.op("pool", lambda e: e.memset(yacc[:], 0.0), writes=taccs)
                w_dma(0)
                w_cast(0)
                units = [(wi, t4) for wi in range(len(work)) for t4 in range(NT4)]
                GU(st_i, 0, 0, 0)
                for ui, (wi, t4) in enumerate(units):
                    if t4 == 0 and wi + 1 < len(work):
                        w_dma(wi + 1)
                    if t4 == 2 and wi + 1 < len(work):
                        w_cast(wi + 1)
                    if ui + 1 < len(units):
                        GU(st_i, units[ui + 1][0], units[ui + 1][1], (ui + 1) % 2)
                    YD(st_i, wi, t4, ui % 2)
                for tsub in range(TS // 128):
                    b = tsub % 2
                    tok0 = st_i * TS + tsub * 128
                    kb.dma("sp", x1[b][:], self.X1.ap()[tok0:tok0 + 128, :], writes=[tx1[b]])
                    tacc = taccs[tsub * 2]
                    kb.op("dve", lambda e, b=b, tsub=tsub: e.scalar_tensor_tensor(yacc[:, tsub, :], x1[b][:], ALPHA, yacc[:, tsub, :], ALU.mult, ALU.add),
                          reads=[tx1[b], taccs[tsub * 2], taccs[tsub * 2 + 1]], writes=[tacc])
                    self.layernorm(yacc[:, tsub, :], tacc, lng[:], lnb[:], tgb, x1[b][:], tx1[b], st, mv, tst)
                    if last:
                        kb.dma("sp", self.y_out.ap()[s * T + tok0:s * T + tok0 + 128, :], x1[b][:], reads=[tx1[b]])
                    else:
                        kb.dma("sp", self.X2.ap()[tok0:tok0 + 128, :], x1[b][:], reads=[tx1[b]])
                        self.transpose_rows(None, x1[b], tx1[b], xT, txT, tok0, psG, tpG, 0)
            kb.barrier()

    def build(self):
        kb = self.kb
        with ExitStack() as es:
            self.load_consts(es)
            self.epsb = self.sb(es, "epsb", [128, 4], F32)
            kb.op("dve", lambda e: e.memset(self.epsb[:, 0:1], RMS_EPS), writes=[self.tc])
            kb.op("dve", lambda e: e.memset(self.epsb[:, 1:2], RMS_EPS * 128), writes=[self.tc])
            kb.op("dve", lambda e: e.memset(self.epsb[:, 2:3], 1.0), writes=[self.tc])
            kb.op("dve", lambda e: e.memset(self.epsb[:, 3:4], LN_EPS), writes=[self.tc])
            xT = self.sb(es, "xT", [128, 8, T], BF16)
            txT = Tok()
            gates = self.sb(es, "gates", [128, T // 128, NE], F32)
            tgates = Tok()
            kb.barrier()
            only = os.environ.get("ONLY")
            for s in range(self.nseq):
                if only:
                    ph, LL = only.split(":")
                    LL = int(LL)
                    kb.op("dve", lambda e: e.memset(xT[:], 0.0), writes=[txT])
                    kb.op("dve", lambda e: e.memset(gates[:], 0.0), writes=[tgates])
                    kb.barrier()
                    {"p1": lambda: self.phase1(LL, s, xT, txT), "p2": lambda: self.phase2(s, xT), "p3": lambda: self.phase3(LL, s, xT),
                     "p4": lambda: self.phase4(LL, s, xT, txT, gates, tgates), "p5": lambda: self.phase5(LL, s, xT, txT, gates, tgates)}[ph]()
                    continue
                self.phase0(s, xT, txT)
                if "XTd" in self.dbg:
                    xtd = self.scr("XTd", [128, 8, T], BF16)
                    kb.dma("sp", xtd.ap()[:, :, :], xT[:], reads=[txT])
                if self.stop_after == "p0":
                    break
                self.rope_tables(s)
                if self.stop_after == "rope":
                    break
                for L in range(2):
                    self.phase1(L, s, xT, txT)
                    if self.stop_after == "p1":
                        break
                    self.phase2(s, xT)
                    if self.stop_after == "p2":
                        break
                    self.phase3(L, s, xT)
                    if self.stop_after == "p3":
                        break
                    self.phase4(L, s, xT, txT, gates, tgates)
                    if self.stop_after == "p4":
                        break
                    self.phase5(L, s, xT, txT, gates, tgates)
                    if self.stop_after == f"p5_{L}":
                        break
                if self.stop_after:
                    break
            kb.barrier()
        return self.nc


_W_KEYS = ("w_in", "a_log", "dt_bias", "dn_norm_w", "w_branch_a", "w_branch_b", "w_out", "ln1_g", "ln1_b", "ln2_g", "ln2_b",
           "ffn_w_gate", "ffn_w_up", "ffn_w_down", "router_w", "moe_w_gate", "moe_w_up", "moe_w_down")


def core_maps(inputs, seq_groups):
    cst = make_consts()
    conv = np.ascontiguousarray(np.asarray(inputs["conv_w"], np.float32).reshape(2, 4, 24, 128).transpose(0, 3, 2, 1))
    ws = {k: np.ascontiguousarray(np.asarray(inputs[k], np.float32)) for k in _W_KEYS}
    x = np.asarray(inputs["x"], np.float32)
    pos = np.asarray(inputs["positions"], np.int32)
    maps = []
    for seqs in seq_groups:
        m = {"x": np.ascontiguousarray(x[seqs].reshape(-1, D)), "pos": np.ascontiguousarray(pos[seqs]), "cst": cst, "conv_w": conv}
        m.update(ws)
        maps.append(m)
    return maps


def kernel(**inputs):
    x = np.asarray(inputs["x"])
    B = x.shape[0]
    n_cores = 8
    per = B // n_cores
    groups = [list(range(c * per, (c + 1) * per)) for c in range(n_cores)]
    prog = Prog(per)
    nc = prog.build()
    res = run_bass_kernel_spmd(nc, core_maps(inputs, groups), core_ids=list(range(n_cores)))
    out = np.concatenate([np.asarray(r["y"], np.float32).reshape(per, T, D) for r in res.results], axis=0)
    return out
```

```python
import math
import os
from contextlib import ExitStack
import numpy as np
import concourse.bass as bass
import concourse.mybir as mybir
from concourse.bass_utils import run_bass_kernel_spmd

F32 = mybir.dt.float32
F32R = mybir.dt.float32r
BF16 = mybir.dt.bfloat16
I32 = mybir.dt.int32
AF = mybir.ActivationFunctionType
ALU = mybir.AluOpType
AX = mybir.AxisListType

D = 1024
T = 4096
NIN = 10768
DFF = 2816
NE = 8
DEX = 3584
ALPHA = 4 ** 0.25
LN_EPS = 1e-5
RMS_EPS = 1e-6
PI = math.pi
A_PAIRS = ((128, 1), (512, 4), (2048, 16))
NDS = 8


class Tok:
    __slots__ = ("w", "r")

    def __init__(self):
        self.w = None
        self.r = {}


class KB:
    def __init__(self, nc):
        self.nc = nc
        self.E = {"pe": nc.tensor, "dve": nc.vector, "act": nc.scalar, "pool": nc.gpsimd, "sp": nc.sync}
        self.csem = {e: nc.alloc_semaphore("cs_" + e) for e in self.E}
        self.ccnt = {e: 0 for e in self.E}
        self.seen = {e: {} for e in self.E}
        self.dq = {q: [[nc.alloc_semaphore(f"ds_{q}{i}"), 0] for i in range(NDS)] for q in ("sp", "pool", "act")}
        self.dnext = {q: 0 for q in self.dq}
        self.ninst = 0

    def _sem(self, ev):
        if ev[0] == "c":
            return self.csem[ev[1]]
        return self.dq[ev[1]][ev[2]][0]

    def _wait(self, e, ev):
        key = ev[:-1]
        val = ev[-1]
        if self.seen[e].get(key, 0) >= val:
            return
        self.E[e].wait_ge(self._sem(ev), val)
        self.seen[e][key] = val
        self.ninst += 1

    def _deps(self, e, reads, writes):
        for t in reads:
            if t.w is not None:
                self._wait(e, t.w)
        for t in writes:
            if t.w is not None and not (e == "pe" and t.w[0] == "c" and t.w[1] == "pe"):
                self._wait(e, t.w)
            for k, ev in t.r.items():
                self._wait(e, ev)

    def _mark(self, ev, reads, writes):
        for t in reads:
            t.r[ev[:-1]] = ev
        for t in writes:
            t.w = ev
            t.r = {}

    def op(self, e, fn, reads=(), writes=()):
        self._deps(e, reads, writes)
        ins = fn(self.E[e])
        self.ccnt[e] += 1
        ins.then_inc(self.csem[e], 1)
        self.ninst += 1
        self._mark(("c", e, self.ccnt[e]), reads, writes)

    def dma(self, q, out, in_, reads=(), writes=()):
        self._deps(q, reads, writes)
        i = self.dnext[q]
        self.dnext[q] = (i + 1) % NDS
        slot = self.dq[q][i]
        if slot[1] > 0:
            self._wait(q, ("d", q, i, slot[1]))
        self.E[q].dma_start(out=out, in_=in_).then_inc(slot[0], 16)
        slot[1] += 16
        self.ninst += 1
        self._mark(("d", q, i, slot[1]), reads, writes)

    def barrier(self):
        for e in self.E:
            for f in self.E:
                if self.ccnt[f] > 0:
                    self._wait(e, ("c", f, self.ccnt[f]))
            for q in self.dq:
                for i, slot in enumerate(self.dq[q]):
                    if slot[1] > 0:
                        self._wait(e, ("d", q, i, slot[1]))


def ss(t0, n, d):
    return slice(t0, t0 + (n - 1) * d + 1, d)


def toks(n):
    return [Tok() for _ in range(n)]


C_ID, C_U, C_LI, C_LS, C_ONE, C_PERM, C_INVF, C_SIGN, C_MBS, C_MBT, C_N = 0, 128, 256, 384, 512, 640, 768, 769, 776, 904, 1032


def make_consts():
    c = np.zeros((128, C_N), np.float32)
    p = np.arange(128)
    c[:, C_ID:C_ID + 128] = np.eye(128)
    c[:, C_U:C_U + 128] = (p[:, None] <= p[None, :])
    c[:, C_LI:C_LI + 128] = (p[:, None] >= p[None, :])
    c[:, C_LS:C_LS + 128] = (p[:, None] > p[None, :])
    c[:, C_ONE:C_ONE + 128] = 1.0
    pm = np.zeros((32, 32), np.float32)
    for m in range(32):
        pm[(m + 16) % 32, m] = 1.0
    c[:32, C_PERM:C_PERM + 32] = pm
    invf = (500000.0 ** (-np.arange(0, 32, 2, dtype=np.float32) / 32)).astype(np.float32)
    c[:32, C_INVF] = np.concatenate([invf, invf])
    c[:16, C_SIGN] = -1.0
    c[16:32, C_SIGN] = 1.0
    c[:, C_MBS:C_MBS + 128] = 30000.0 * (1.0 - (p[:, None] > p[None, :]))
    c[:, C_MBT:C_MBT + 128] = -30000.0 * (1.0 - (p[:, None] <= p[None, :]))
    return c


class Prog:
    def __init__(self, nseq, dbg=None, stop_after=None):
        self.nseq = nseq
        self.dbg = dbg or ()
        self.stop_after = stop_after
        nc = bass.Bass("TRN2", target_bir_lowering=False)
        self.nc = nc
        self.kb = KB(nc)
        NT = nseq * T
        self.NT = NT
        dt = nc.dram_tensor
        I = "ExternalInput"
        self.x_in = dt("x", [NT, D], F32, kind=I)
        self.pos = dt("pos", [nseq, T], I32, kind=I)
        self.cst = dt("cst", [128, C_N], F32, kind=I)
        self.w_in = dt("w_in", [2, D, NIN], F32, kind=I)
        self.conv_w = dt("conv_w", [2, 128, 24, 4], F32, kind=I)
        self.a_log = dt("a_log", [2, 8], F32, kind=I)
        self.dt_bias = dt("dt_bias", [2, 8], F32, kind=I)
        self.dn_norm_w = dt("dn_norm_w", [2, 128], F32, kind=I)
        self.w_a = dt("w_branch_a", [2, 512, D], F32, kind=I)
        self.w_b = dt("w_branch_b", [2, D, D], F32, kind=I)
        self.w_o = dt("w_out", [2, D, D], F32, kind=I)
        self.ln1_g = dt("ln1_g", [2, D], F32, kind=I)
        self.ln1_b = dt("ln1_b", [2, D], F32, kind=I)
        self.ln2_g = dt("ln2_g", [2, D], F32, kind=I)
        self.ln2_b = dt("ln2_b", [2, D], F32, kind=I)
        self.ffn_g = dt("ffn_w_gate", [1, D, DFF], F32, kind=I)
        self.ffn_u = dt("ffn_w_up", [1, D, DFF], F32, kind=I)
        self.ffn_d = dt("ffn_w_down", [1, DFF, D], F32, kind=I)
        self.router = dt("router_w", [1, D, NE], F32, kind=I)
        self.moe_g = dt("moe_w_gate", [1, NE, D, DEX], F32, kind=I)
        self.moe_u = dt("moe_w_up", [1, NE, D, DEX], F32, kind=I)
        self.moe_d = dt("moe_w_down", [1, NE, DEX, D], F32, kind=I)
        self.y_out = dt("y", [NT, D], F32, kind="ExternalOutput")
        self.QKT = self.scr("QKT", [24, 128, T], BF16)
        self.VA = self.scr("VA", [3, 4, 128, 32, 128], BF16)
        self.GQT = self.scr("GQT", [24, 128, T], BF16)
        self.ZT = self.scr("ZT", [8, 128, T], BF16)
        self.GT = self.scr("GT", [16, 128, T], BF16)
        self.BG = self.scr("BG", [T, 16], F32)
        self.YAT = self.scr("YAT", [4, 128, T], BF16)
        self.YBT = self.scr("YBT", [8, 128, T], BF16)
        self.X1 = self.scr("X1", [T, D], F32)
        self.X2 = self.scr("X2", [T, D], F32)
        self.GATES = self.scr("GATES", [T, NE], F32)
        self.CSd = self.scr("CSd", [32, 2, T], F32)

    def scr(self, name, shape, dtype):
        kind = "ExternalOutput" if name in self.dbg else "Internal"
        return self.nc.dram_tensor(name, shape, dtype, kind=kind)

    def sb(self, es, name, shape, dtype):
        self.uid = getattr(self, "uid", 0) + 1
        return es.enter_context(self.nc.sbuf_tensor(f"{name}_{self.uid}", shape, dtype))

    def ps(self, es, name, shape=(128, 512), dtype=F32):
        self.uid = getattr(self, "uid", 0) + 1
        return es.enter_context(self.nc.psum_tensor(f"{name}_{self.uid}", list(shape), dtype))

    def bc_ap(self, handle, offset, n, parts=128):
        return bass.AP(handle, offset, [[0, parts], [1, n]])

    def wload(self, stage, tstage, dst, tdst, src, q="pool"):
        kb = self.kb
        kb.dma(q, stage, src, writes=[tstage])
        kb.op("pool", lambda e: e.tensor_copy(dst, stage), reads=[tstage], writes=[tdst])

    def load_consts(self, es):
        kb = self.kb
        self.c32 = self.sb(es, "c32", [128, C_N], F32)
        self.cbf = self.sb(es, "cbf", [128, C_N], BF16)
        self.tc = Tok()
        kb.dma("sp", self.c32[:], self.cst.ap()[:, :], writes=[self.tc])
        kb.op("dve", lambda e: e.tensor_copy(self.cbf[:], self.c32[:]), reads=[self.tc], writes=[self.tc])

    def transpose_rows(self, es_tag, src_sb, tsrc, xT, txT, col0, psT, tps, idx, x32=None, tx32=None):
        kb = self.kb
        ident = self.c32[:, C_ID:C_ID + 128]
        for hf in range(2):
            p = psT[(2 * idx + hf) % len(psT)]
            tp = tps[(2 * idx + hf) % len(psT)]

            def f(e, hf=hf, p=p):
                ins = None
                for j in range(4):
                    c = hf * 4 + j
                    ins = e.transpose(p[:, j * 128:(j + 1) * 128], src_sb[:, c * 128:(c + 1) * 128], ident)
                return ins
            kb.op("pe", f, reads=[tsrc, self.tc], writes=[tp])
            pv = p[:, :].rearrange("p (j t) -> p j t", j=4)
            if x32 is None:
                kb.op("act", lambda e, hf=hf, pv=pv: e.copy(xT[:, hf * 4:hf * 4 + 4, col0:col0 + 128], pv),
                      reads=[tp], writes=[txT])
            else:
                kb.op("act", lambda e, hf=hf, pv=pv: e.copy(x32[:, hf * 4:hf * 4 + 4, :], pv), reads=[tp], writes=[tx32])
                kb.op("dve", lambda e, hf=hf: e.tensor_copy(xT[:, hf * 4:hf * 4 + 4, col0:col0 + 128], x32[:, hf * 4:hf * 4 + 4, :]),
                      reads=[tx32], writes=[txT])

    def phase0(self, s, xT, txT):
        kb = self.kb
        with ExitStack() as es:
            xin = [self.sb(es, f"p0x{i}", [128, D], F32) for i in range(2)]
            tx = toks(2)
            psT = [self.ps(es, f"p0ps{i}") for i in range(4)]
            tps = toks(4)
            for t in range(T // 128):
                b = t % 2
                r0 = s * T + t * 128
                kb.dma("sp", xin[b][:], self.x_in.ap()[r0:r0 + 128, :], writes=[tx[b]])
                self.transpose_rows(None, xin[b], tx[b], xT, txT, t * 128, psT, tps, t)
            kb.barrier()

    def rope_tables(self, s):
        kb = self.kb
        with ExitStack() as es:
            pi_ = self.sb(es, "rp_i", [32, T], I32)
            ang = self.sb(es, "rp_a", [32, T], F32)
            kf = self.sb(es, "rp_k", [32, T], F32)
            ki = self.sb(es, "rp_ki", [32, T], I32)
            u = self.sb(es, "rp_u", [32, T], F32)
            cr = self.sb(es, "rp_c", [32, T], F32)
            CS = self.sb(es, "rp_CS", [32, 2, T], F32)
            tCS = Tok()
            t1 = Tok()
            kb.dma("sp", pi_[:], self.bc_ap(self.pos, s * T, T, 32), writes=[t1])
            kb.op("dve", lambda e: e.tensor_copy(ang[:], pi_[:]), reads=[t1], writes=[t1])
            kb.op("dve", lambda e: e.tensor_scalar(ang[:], ang[:], self.c32[0:32, C_INVF:C_INVF + 1], None, ALU.mult),
                  reads=[t1, self.tc], writes=[t1])
            t2 = Tok()
            for which in range(2):
                sh = PI / 2 if which == 0 else 0.0
                kb.op("dve", lambda e: e.tensor_scalar(kf[:], ang[:], 1.0 / (2 * PI), sh / (2 * PI), ALU.mult, ALU.add),
                      reads=[t1], writes=[t2])
                kb.op("dve", lambda e: e.tensor_copy(ki[:], kf[:]), reads=[t2], writes=[t2])
                kb.op("dve", lambda e: e.tensor_copy(kf[:], ki[:]), reads=[t2], writes=[t2])
                kb.op("dve", lambda e: e.scalar_tensor_tensor(u[:], kf[:], -2 * PI, ang[:], ALU.mult, ALU.add),
                      reads=[t2, t1], writes=[t2])
                if sh != 0.0:
                    kb.op("dve", lambda e: e.tensor_scalar(u[:], u[:], sh, None, ALU.add), reads=[t2], writes=[t2])
                kb.op("dve", lambda e: e.tensor_scalar(cr[:], u[:], PI, -2 * PI, ALU.is_gt, ALU.mult), reads=[t2], writes=[t2])
                kb.op("dve", lambda e: e.tensor_tensor(u[:], u[:], cr[:], ALU.add), reads=[t2], writes=[t2])
                kb.op("dve", lambda e: e.tensor_scalar(cr[:], u[:], -PI, 2 * PI, ALU.is_lt, ALU.mult), reads=[t2], writes=[t2])
                kb.op("dve", lambda e: e.tensor_tensor(u[:], u[:], cr[:], ALU.add), reads=[t2], writes=[t2])
                kb.op("dve", lambda e: e.tensor_scalar(u[:], u[:], PI, -PI, ALU.min, ALU.max), reads=[t2], writes=[t2])
                if which == 0:
                    kb.op("act", lambda e: e.activation(CS[0:32, 0, :], u[:], AF.Sin), reads=[t2], writes=[tCS])
                else:
                    kb.op("act", lambda e: e.activation(u[:], u[:], AF.Sin), reads=[t2], writes=[t2])
                    kb.op("dve", lambda e: e.tensor_scalar(CS[0:32, 1, :], u[:], self.c32[0:32, C_SIGN:C_SIGN + 1], None, ALU.mult),
                          reads=[t2, self.tc], writes=[tCS])
            kb.dma("sp", self.CSd.ap()[:, :, :], CS[:], reads=[tCS])
            kb.barrier()

    def phase1(self, L, s, xT, txT):
        kb = self.kb
        nc = self.nc
        win = self.w_in.ap()[L]
        NTT = T // 512
        with ExitStack() as es:
            wq = [self.sb(es, f"p1w{i}", [128, 8, 128], BF16) for i in range(2)]
            twq = toks(2)
            stg = [self.sb(es, f"p1s{i}", [128, T], BF16) for i in range(2)]
            tst = toks(2)
            raw = self.sb(es, "p1raw", [128, T + 3], F32)
            traw = Tok()
            acc = self.sb(es, "p1acc", [128, T], F32)
            tacc = Tok()
            h32 = [self.sb(es, f"p1h{i}", [128, 512], F32) for i in range(2)]
            th32 = toks(2)
            r1 = [self.sb(es, f"p1r{i}", [32, 512], F32) for i in range(2)]
            tr1 = toks(2)
            cw = self.sb(es, "p1cw", [128, 24, 4], F32)
            tcw = Tok()
            ps = [self.ps(es, f"p1ps{i}") for i in range(4)]
            tps = toks(4)
            ps2 = [self.ps(es, f"p1pq{i}") for i in range(2)]
            tps2 = toks(2)
            nps = [0]
            CS = self.sb(es, "p1CS", [32, 2, T], F32)
            tCS = Tok()
            kb.dma("sp", CS[:], self.CSd.ap()[:, :, :], writes=[tCS])

            kb.dma("sp", cw[:], self.conv_w.ap()[L], writes=[tcw])
            kb.op("dve", lambda e: e.memset(raw[:, 0:3], 0.0), writes=[traw])

            wst = [self.sb(es, f"p1wst{i}", [128, 8, 128], F32) for i in range(2)]
            twst = toks(2)
            wbig = self.sb(es, "p1wbig", [128, 8, 512], F32)
            twbig = Tok()
            dcols = [7680 + c * 128 for c in range(8)] + [8720 + c * 128 for c in range(16)]
            wcols = [c * 128 for c in range(24)]
            for c in range(24):
                wcols += [4608 + c * 128, dcols[c]]

            def w_dma(ci):
                b = ci % 2
                kb.dma("pool", wst[b][:], win[:, wcols[ci]:wcols[ci] + 128].rearrange("(k p) n -> p k n", p=128), writes=[twst[b]])

            def load_w(ci, col0):
                assert wcols[ci] == col0
                b = ci % 2
                if ci == 0:
                    w_dma(ci)
                if ci + 1 < len(wcols):
                    w_dma(ci + 1)
                kb.op("pool", lambda e: e.tensor_copy(wq[b][:], wst[b][:]), reads=[twst[b]], writes=[twq[b]])
                return wq[b], twq[b]

            def proj_tile(w, tw, tt, ncols=128):
                i = nps[0] % 4
                nps[0] += 1

                def f(e):
                    ins = None
                    for k in range(8):
                        ins = e.matmul(ps[i][0:ncols, :], w[:, k, 0:ncols], xT[:, k, tt * 512:(tt + 1) * 512],
                                       start=(k == 0), stop=(k == 7))
                    return ins
                kb.op("pe", f, reads=[tw, txT], writes=[tps[i]])
                return ps[i], tps[i]

            ci = 0
            tstA = [toks(NTT), toks(NTT)]
            pend = [None]

            def flush():
                if pend[0] is not None:
                    pend[0]()
                    pend[0] = None
            for c in range(24):
                w, tw = load_w(ci, c * 128)
                ci += 1
                sb_ = c % 2
                for tt in range(NTT):
                    p, tp = proj_tile(w, tw, tt)
                    cols = slice(tt * 512, (tt + 1) * 512)
                    hb = (c * NTT + tt) % 2
                    tk = tstA[sb_][tt]
                    kb.op("act", lambda e, p=p, cols=cols: e.copy(stg[sb_][:, cols], p[:, :]), reads=[tp], writes=[tk])
                    kb.op("act", lambda e, p=p, hb=hb: e.copy(h32[hb][0:32, :], p[0:32, :]), reads=[tp], writes=[th32[hb]])
                    flush()

                    def rot(hb=hb, cols=cols, tk=tk, sb_=sb_):
                        kb.op("pe", lambda e: e.matmul(ps2[hb][:, :], self.cbf[:, C_PERM:C_PERM + 128], stg[sb_][:, cols],
                                                       start=True, stop=True), reads=[tk, self.tc], writes=[tps2[hb]])
                        kb.op("dve", lambda e: e.tensor_tensor(r1[hb][:], h32[hb][0:32, :], CS[0:32, 0, cols], ALU.mult),
                              reads=[th32[hb], tCS], writes=[tr1[hb]])
                        kb.op("dve", lambda e: e.tensor_tensor(h32[hb][0:32, :], ps2[hb][0:32, :], CS[0:32, 1, cols], ALU.mult),
                              reads=[tps2[hb], tCS], writes=[th32[hb]])
                        kb.op("dve", lambda e: e.tensor_tensor(stg[sb_][0:32, cols], r1[hb][:], h32[hb][0:32, :], ALU.add),
                              reads=[tr1[hb], th32[hb]], writes=[tk])
                    pend[0] = rot
                flush()
                kb.dma("sp", self.QKT.ap()[c], stg[sb_][:], reads=tstA[sb_])
                tst[sb_].r.update({k_: v_ for t_ in tstA[sb_] for k_, v_ in t_.r.items()})
            wv = self.sb(es, "p1wv", [128, 8, 512], BF16)
            twv = Tok()
            vst = [self.sb(es, f"p1vs{i}", [128, 512], BF16) for i in range(2)]
            tvs = toks(2)
            nb_ = 0
            for g, (win_, dil) in enumerate(A_PAIRS):
                self.wload(wbig[:], twbig, wv[:], twv, win[:, 3072 + g * 512:3072 + (g + 1) * 512].rearrange("(k p) n -> p k n", p=128), q="sp")
                nblk = 32 // dil
                for r in range(dil):
                    for n in range(nblk):
                        blk = r * nblk + n
                        t0 = 128 * n * dil + r
                        i = nps[0] % 4
                        nps[0] += 1

                        def f(e, i=i, t0=t0, dil=dil):
                            ins = None
                            for k in range(8):
                                ins = e.matmul(ps[i][:, :], xT[:, k, ss(t0, 128, dil)], wv[:, k, :],
                                               start=(k == 0), stop=(k == 7))
                            return ins
                        kb.op("pe", f, reads=[twv, txT], writes=[tps[i]])
                        vb = nb_ % 2
                        nb_ += 1
                        kb.op("act", lambda e, i=i, vb=vb: e.copy(vst[vb][:], ps[i][:, :]), reads=[tps[i]], writes=[tvs[vb]])
                        kb.dma("sp", self.VA.ap()[g, :, :, blk, :].rearrange("h p e -> p h e"),
                               vst[vb][:, :].rearrange("p (h e) -> p h e", h=4), reads=[tvs[vb]])
            ci = 24
            for c in range(24):
                w, tw = load_w(ci, 4608 + c * 128)
                ci += 1
                sb_ = 0
                for tt in range(NTT):
                    p, tp = proj_tile(w, tw, tt)
                    kb.op("act", lambda e, p=p, tt=tt: e.copy(raw[:, 3 + tt * 512:3 + (tt + 1) * 512], p[:, :]),
                          reads=[tp], writes=[traw])
                wD, twD = load_w(ci, dcols[c])
                ci += 1

                def dchunk(c=c, w=wD, tw=twD):
                    sb_ = 1
                    fn = AF.Silu if c < 8 else AF.Sigmoid
                    for tt in range(NTT):
                        p, tp = proj_tile(w, tw, tt)
                        kb.op("act", lambda e, p=p, tt=tt, fn=fn: e.activation(stg[sb_][:, tt * 512:(tt + 1) * 512], p[:, :], fn),
                              reads=[tp], writes=[tst[sb_]])
                    dst = self.ZT.ap()[c] if c < 8 else self.GT.ap()[c - 8]
                    kb.dma("sp", dst, stg[sb_][:], reads=[tst[sb_]])
                dchunk()
                sb_ = 0
                for hh in range(2):
                    cs = slice(hh * 2048, (hh + 1) * 2048)
                    kb.op("dve", lambda e, cs=cs, hh=hh: e.tensor_scalar(acc[:, cs], raw[:, hh * 2048:hh * 2048 + 2048],
                                                                          cw[:, c, 0:1], None, ALU.mult),
                          reads=[traw, tcw], writes=[tacc])
                    for j in range(1, 4):
                        kb.op("dve", lambda e, cs=cs, hh=hh, j=j: e.scalar_tensor_tensor(
                            acc[:, cs], raw[:, hh * 2048 + j:hh * 2048 + j + 2048], cw[:, c, j:j + 1], acc[:, cs],
                            ALU.mult, ALU.add), reads=[traw, tcw, tacc], writes=[tacc])
                if c >= 16:
                    kb.op("act", lambda e: e.activation(stg[sb_][:], acc[:], AF.Silu), reads=[tacc], writes=[tst[sb_]])
                else:
                    kb.op("act", lambda e: e.activation(acc[:], acc[:], AF.Silu), reads=[tacc], writes=[tacc])
                    for tt in range(NTT):
                        cols = slice(tt * 512, (tt + 1) * 512)
                        hb = tt % 2
                        sq = raw
                        kb.op("dve", lambda e, cols=cols: e.tensor_tensor(raw[:, cols], acc[:, cols], acc[:, cols], ALU.mult),
                              reads=[tacc], writes=[traw])
                        i = nps[0] % 4
                        nps[0] += 1
                        kb.op("pe", lambda e, i=i, cols=cols: e.matmul(ps[i][:, :], self.c32[:, C_ONE:C_ONE + 128], raw[:, cols],
                                                                       start=True, stop=True),
                              reads=[traw, self.tc], writes=[tps[i]])
                        sc = (1.0 / 128) ** 0.5 if c < 8 else 1.0
                        kb.op("act", lambda e, i=i, cols=cols, sc=sc: e.activation(raw[:, cols], ps[i][:, :], AF.Sqrt,
                                                                                    bias=self.epsb[:, 0:1] if sc == 1.0 else self.epsb[:, 1:2],
                                                                                    scale=1.0 / (sc * sc)),
                              reads=[tps[i], self.tc], writes=[traw])
                        kb.op("dve", lambda e, cols=cols: e.reciprocal(raw[:, cols], raw[:, cols]), reads=[traw], writes=[traw])
                        kb.op("dve", lambda e, cols=cols: e.tensor_tensor(stg[sb_][:, cols], acc[:, cols], raw[:, cols], ALU.mult),
                              reads=[traw, tacc], writes=[tst[sb_]])
                    kb.op("dve", lambda e: e.memset(raw[:, 0:3], 0.0), reads=[traw], writes=[traw])
                kb.dma("sp", self.GQT.ap()[c], stg[sb_][:], reads=[tst[sb_]])
            wbd = self.sb(es, "p1wbd", [128, 8, 16], BF16)
            twbd = Tok()
            self.wload(wbig[:, :, 0:16], twbig, wbd[:], twbd, win[:, 8704:8720].rearrange("(k p) n -> p k n", p=128), q="sp")
            rows = self.sb(es, "p1rows", [128, 16], F32)
            trows = Tok()
            kb.dma("sp", rows[:, 0:8], self.bc_ap(self.dt_bias, L * 8, 8), writes=[trows])
            kb.dma("sp", rows[:, 8:16], self.bc_ap(self.a_log, L * 8, 8), writes=[trows])
            kb.op("act", lambda e: e.activation(rows[:, 8:16], rows[:, 8:16], AF.Exp), reads=[trows], writes=[trows])
            bg = self.sb(es, "p1bg", [128, 32, 16], F32)
            tbg = Tok()
            tmp = self.sb(es, "p1tmp", [128, 8], F32)
            ttmp = Tok()
            for t in range(32):
                i = nps[0] % 4
                nps[0] += 1

                def f(e, i=i, t=t):
                    ins = None
                    for k in range(8):
                        ins = e.matmul(ps[i][:, 0:16], xT[:, k, t * 128:(t + 1) * 128], wbd[:, k, :], start=(k == 0), stop=(k == 7))
                    return ins
                kb.op("pe", f, reads=[twbd, txT], writes=[tps[i]])
                kb.op("act", lambda e, i=i, t=t: e.activation(bg[:, t, 0:8], ps[i][:, 0:8], AF.Sigmoid), reads=[tps[i]], writes=[tbg])
                kb.op("dve", lambda e, i=i: e.tensor_tensor(tmp[:], ps[i][:, 8:16], rows[:, 0:8], ALU.add),
                      reads=[tps[i], trows], writes=[ttmp])
                kb.op("act", lambda e: e.activation(tmp[:], tmp[:], AF.Exp), reads=[ttmp], writes=[ttmp])
                kb.op("act", lambda e: e.activation(tmp[:], tmp[:], AF.Ln, bias=self.epsb[:, 2:3]), reads=[ttmp, self.tc], writes=[ttmp])
                kb.op("dve", lambda e, t=t: e.scalar_tensor_tensor(bg[:, t, 8:16], tmp[:], -1.0, rows[:, 8:16], ALU.mult, ALU.mult),
                      reads=[ttmp, trows], writes=[tbg])
            kb.dma("sp", self.BG.ap().rearrange("(t p) c -> p t c", p=128), bg[:], reads=[tbg])
            kb.barrier()


    def phase2(self, s, xT):
        kb = self.kb
        scale = 128.0 ** -0.5
        with ExitStack() as es:
            QT = [xT[:, g, :] for g in range(3)]
            KT = [xT[:, 3 + g, :] for g in range(3)]
            VV = [self.sb(es, f"p2v{g}", [128, 32, 128], BF16) for g in range(3)]
            tq, tk, tv = toks(3), toks(3), toks(3)
            num = self.sb(es, "p2num", [128, T], F32)
            den = self.sb(es, "p2den", [128, T], F32)
            tnum, tden = Tok(), Tok()
            PT = [self.sb(es, f"p2pt{i}", [128, 256], BF16) for i in range(2)]
            tpt = toks(2)
            yst = self.sb(es, "p2y", [128, T], BF16)
            tyst = Tok()
            psS = [self.ps(es, f"p2pS{i}") for i in range(2)]
            psN = [self.ps(es, f"p2pN{i}") for i in range(2)]
            psD = [self.ps(es, f"p2pD{i}") for i in range(2)]
            tS, tN, tD = toks(2), toks(2), toks(2)
            ones_bf = self.cbf[:, C_ONE:C_ONE + 128]
            maskcat = self.cbf[:, C_U:C_U + 256]
            kbi = 0
            for slot in range(4):
                for g in range(3):
                    kb.dma("sp", QT[g], self.QKT.ap()[g * 4 + slot], writes=[tq[g]])
                    kb.dma("sp", KT[g], self.QKT.ap()[12 + g * 4 + slot], writes=[tk[g]])
                    kb.dma("sp", VV[g][:], self.VA.ap()[g, slot], writes=[tv[g]])
                kb.op("dve", lambda e: e.memset(num[:], 0.0), writes=[tnum])
                kb.op("dve", lambda e: e.memset(den[:], 0.0), writes=[tden])
                for g, (win_, dil) in enumerate(A_PAIRS):
                    nblk = 32 // dil
                    for r in range(dil):
                        for n in range(nblk):
                            blk = r * nblk + n
                            nq = 2 if n + 1 < nblk else 1
                            t0 = 128 * n * dil + r
                            b = kbi % 2
                            kbi += 1
                            kb.op("pe", lambda e, b=b, g=g, t0=t0, nq=nq, dil=dil: e.matmul(
                                psS[b][:, 0:128 * nq], KT[g][:, ss(t0, 128, dil)], QT[g][:, ss(t0, 128 * nq, dil)],
                                start=True, stop=True), reads=[tk[g], tq[g]], writes=[tS[b]])
                            kb.op("act", lambda e, b=b, nq=nq: e.activation(PT[b][:, 0:128 * nq], psS[b][:, 0:128 * nq], AF.Exp,
                                                                            scale=scale), reads=[tS[b]], writes=[tpt[b]])
                            kb.op("dve", lambda e, b=b, nq=nq: e.tensor_tensor(PT[b][:, 0:128 * nq], PT[b][:, 0:128 * nq],
                                                                              maskcat[:, 0:128 * nq], ALU.mult),
                                  reads=[tpt[b], self.tc], writes=[tpt[b]])
                            for mo in range(nq):
                                a = (n + mo) % 2

                                def f(e, a=a, mo=mo, b=b, g=g, blk=blk, n=n):
                                    st = (mo == 1 or n == 0)
                                    sp_ = (mo == 0)
                                    e.matmul(psN[a][:, 0:128], VV[g][:, blk, :], PT[b][:, mo * 128:(mo + 1) * 128], start=st, stop=sp_)
                                    return e.matmul(psD[a][:, 0:128], ones_bf, PT[b][:, mo * 128:(mo + 1) * 128], start=st, stop=sp_)
                                kb.op("pe", f, reads=[tv[g], tpt[b], self.tc], writes=[tN[a], tD[a]])
                            a = n % 2
                            kb.op("dve", lambda e, a=a, t0=t0, dil=dil: e.tensor_tensor(
                                num[:, ss(t0, 128, dil)], num[:, ss(t0, 128, dil)], psN[a][:, 0:128], ALU.add),
                                reads=[tN[a], tnum], writes=[tnum])
                            kb.op("dve", lambda e, a=a, t0=t0, dil=dil: e.tensor_tensor(
                                den[:, ss(t0, 128, dil)], den[:, ss(t0, 128, dil)], psD[a][:, 0:128], ALU.add),
                                reads=[tD[a], tden], writes=[tden])
                kb.op("dve", lambda e: e.reciprocal(den[:], den[:]), reads=[tden], writes=[tden])
                kb.op("dve", lambda e: e.tensor_tensor(yst[:], num[:], den[:], ALU.mult), reads=[tnum, tden], writes=[tyst])
                kb.dma("sp", self.YAT.ap()[slot], yst[:], reads=[tyst])
            kb.barrier()


    def phase3(self, L, s, xT):
        kb = self.kb
        c32, cbf = self.c32, self.cbf
        ident = c32[:, C_ID:C_ID + 128]
        ident_bf = cbf[:, C_ID:C_ID + 128]
        ones = c32[:, C_ONE:C_ONE + 128]
        NCH = int(os.environ.get("P3NCH", "32"))
        NH = int(os.environ.get("P3NH", "8"))
        h2 = lambda ap: ap.rearrange("p (h e) -> p h e", h=2)
        with ExitStack() as es:
            KT2, QT2, VT2, ZT2 = (xT[:, 2 * i:2 * i + 2, :] for i in range(4))
            yb2 = self.sb(es, "p3yb", [128, 2, T], BF16)
            tld = toks(4)
            tyb = Tok()
            if NCH < 32:
                kb.op("dve", lambda e: e.memset(yb2[:], 0.0), writes=[tyb])
            bg = self.sb(es, "p3bg", [128, 32, 16], F32)
            gc, gl, egc, bge, etl, nbt, sda, ngc = (self.sb(es, "p3" + n_, [128, 32, 8], F32)
                                                    for n_ in ("gc", "gl", "egc", "bge", "etl", "nbt", "sda", "ngc"))
            nwc = self.sb(es, "p3nw", [128, 1], F32)
            ID2, MBS2, MBT2 = (self.sb(es, "p3" + n_, [128, 2, 128], F32) for n_ in ("id2", "mbs2", "mbt2"))
            tsm = Tok()
            pT, pG, pA, pB, pU, pV, pO = (self.ps(es, f"p3bk{i}") for i in range(7))
            tT, tG, tA, tB, tU, tV, tO = toks(7)
            for hh in range(2):
                kb.op("dve", lambda e: e.tensor_copy(ID2[:, hh, :], ident), reads=[self.tc], writes=[tsm])
                kb.op("dve", lambda e: e.tensor_copy(MBS2[:, hh, :], c32[:, C_MBS:C_MBS + 128]), reads=[self.tc], writes=[tsm])
                kb.op("dve", lambda e: e.tensor_copy(MBT2[:, hh, :], c32[:, C_MBT:C_MBT + 128]), reads=[self.tc], writes=[tsm])
            kb.dma("sp", bg[:], self.BG.ap().rearrange("(t p) c -> p t c", p=128), writes=[tsm])
            kb.dma("sp", nwc[:], bass.AP(self.dn_norm_w, L * 128, [[1, 128], [1, 1]]), writes=[tsm])
            gsl = bg[:, :, 8:16]
            bsl = bg[:, :, 0:8]
            v3 = lambda ap: ap.rearrange("p (c h) -> p c h", h=8)
            kb.op("pe", lambda e: e.matmul(v3(pT[:, 0:256]), c32[:, C_U:C_U + 128], gsl, start=True, stop=True), reads=[tsm, self.tc], writes=[tT])
            kb.op("pe", lambda e: e.matmul(v3(pG[:, 0:256]), ones, gsl, start=True, stop=True), reads=[tsm, self.tc], writes=[tG])
            kb.op("act", lambda e: e.copy(gc[:], v3(pT[:, 0:256])), reads=[tT], writes=[tsm])
            kb.op("act", lambda e: e.copy(gl[:], v3(pG[:, 0:256])), reads=[tG], writes=[tsm])
            kb.op("act", lambda e: e.activation(egc[:], gc[:], AF.Exp), reads=[tsm], writes=[tsm])
            kb.op("act", lambda e: e.activation(sda[:], gl[:], AF.Exp), reads=[tsm], writes=[tsm])
            kb.op("dve", lambda e: e.tensor_tensor(bge[:], egc[:], bsl, ALU.mult), reads=[tsm], writes=[tsm])
            kb.op("dve", lambda e: e.tensor_tensor(etl[:], gl[:], gc[:], ALU.subtract), reads=[tsm], writes=[tsm])
            kb.op("act", lambda e: e.activation(etl[:], etl[:], AF.Exp), reads=[tsm], writes=[tsm])
            kb.op("dve", lambda e: e.tensor_scalar(nbt[:], bsl, -1.0, None, ALU.mult), reads=[tsm], writes=[tsm])
            kb.op("dve", lambda e: e.tensor_scalar(ngc[:], gc[:], -1.0, None, ALU.mult), reads=[tsm], writes=[tsm])
            kb.op("dve", lambda e: e.tensor_scalar(nwc[:], nwc[:], 128.0 ** 0.5, None, ALU.mult), reads=[tsm], writes=[tsm])

            def S(name, shape, dtype):
                return self.sb(es, "p3" + name, shape, dtype)
            Sst, Ug, dmS, dmT, eS, eT, eg, u_, sq, rinv, y1 = (S(n_, [128, 2, 128], F32) for n_ in
                                                               ("S", "Ug", "dmS", "dmT", "eS", "eT", "eg", "u", "sq", "ri", "y1"))
            Qm = [S("Q0", [128, 2, 128], F32), S("Q1", [128, 2, 128], F32)]
            PY = [S("PY0", [128, 2, 256], F32), S("PY1", [128, 2, 256], F32)]
            Sbf, kbg, ktl, vb_, TT, AT, wT, qdT, vnew = (S(n_, [128, 2, 128], BF16) for n_ in
                                                         ("Sb", "kbg", "ktl", "vb", "TT", "AT", "wT", "qdT", "vn"))
            tS, tUg, tdmS, tdmT, teS, teT, teg, tkbg, tktl, tvb, tTT, tAT, twT, tqdT, tvn, tu, tsq, tri, ty1 = toks(19)
            tPY, tQ = toks(2), toks(2)
            pA3 = h2(pA[:, :])
            for h0 in range(0, NH, 2):
                hsl = lambda t, o: t.ap()[o + h0:o + h0 + 2].rearrange("h p t -> p h t")
                kb.dma("sp", KT2, hsl(self.GQT, 8), writes=[tld[0]])
                kb.dma("sp", QT2, hsl(self.GQT, 0), writes=[tld[1]])
                kb.dma("sp", VT2, hsl(self.GQT, 16), writes=[tld[2]])
                kb.dma("sp", ZT2, hsl(self.ZT, 0), writes=[tld[3]])
                kb.op("dve", lambda e: e.memset(Sst[:], 0.0), writes=[tS])
                kb.op("dve", lambda e: e.memset(Sbf[:], 0.0), writes=[tS])
                for c in range(NCH):
                    cols = slice(c * 128, (c + 1) * 128)
                    col = lambda t, hh: t[:, c, h0 + hh:h0 + hh + 1]
                    sl = lambda hh: slice(hh * 128, (hh + 1) * 128)
                    def ftr(e):
                        ins = None
                        for hh in range(2):
                            e.matmul(pT[:, sl(hh)], KT2[:, hh, cols], ident_bf, start=True, stop=True)
                            ins = e.matmul(pT[:, sl(2 + hh)], VT2[:, hh, cols], ident_bf, start=True, stop=True)
                        return ins
                    kb.op("pe", ftr, reads=[tld[0], tld[2], self.tc], writes=[tT])
                    for hh in range(2):
                        kb.op("act", lambda e: e.activation(kbg[:, hh, :], pT[:, sl(hh)], AF.Copy, scale=col(bge, hh)), reads=[tT, tsm], writes=[tkbg])
                        kb.op("act", lambda e: e.activation(ktl[:, hh, :], pT[:, sl(hh)], AF.Copy, scale=col(etl, hh)), reads=[tT, tsm], writes=[tktl])
                        kb.op("act", lambda e: e.activation(vb_[:, hh, :], pT[:, sl(2 + hh)], AF.Copy, scale=col(bsl, hh)), reads=[tT, tsm], writes=[tvb])
                    for hh in range(2):
                        kb.op("dve", lambda e: e.tensor_scalar(Ug[:, hh, :], c32[:, C_U:C_U + 128], col(gsl, hh), None, ALU.mult),
                              reads=[tsm, self.tc], writes=[tUg])

                    def fg(e):
                        e.matmul(pG[:, sl(0)], ones, Ug[:, 0, :], start=True, stop=True)
                        return e.matmul(pG[:, sl(1)], ones, Ug[:, 1, :], start=True, stop=True)
                    kb.op("pe", fg, reads=[tUg, self.tc], writes=[tG])
                    kb.op("act", lambda e: e.activation(eg[:], h2(pG[:, 0:256]), AF.Exp), reads=[tG], writes=[teg])
                    for hh in range(2):
                        kb.op("act", lambda e: e.activation(Ug[:, hh, :], pG[:, sl(hh)], AF.Identity, bias=col(ngc, hh), scale=1.0),
                              reads=[tG, tsm], writes=[tUg])
                    kb.op("dve", lambda e: e.tensor_tensor(dmS[:], Ug[:], MBS2[:], ALU.max), reads=[tUg, tsm], writes=[tdmS])
                    kb.op("dve", lambda e: e.tensor_tensor(dmT[:], Ug[:], MBT2[:], ALU.min), reads=[tUg, tsm], writes=[tdmT])
                    kb.op("act", lambda e: e.activation(eS[:], dmS[:], AF.Exp, scale=-1.0), reads=[tdmS], writes=[teS])
                    kb.op("act", lambda e: e.activation(eT[:], dmT[:], AF.Exp), reads=[tdmT], writes=[teT])

                    def fkk(e):
                        e.matmul(pG[:, sl(2)], KT2[:, 0, cols], KT2[:, 0, cols], start=True, stop=True)
                        return e.matmul(pG[:, sl(3)], KT2[:, 1, cols], KT2[:, 1, cols], start=True, stop=True)
                    kb.op("pe", fkk, reads=[tld[0]], writes=[tG])
                    kb.op("dve", lambda e: e.tensor_tensor(eS[:], h2(pG[:, 256:512]), eS[:], ALU.mult), reads=[tG, teS], writes=[teS])
                    for hh in range(2):
                        kb.op("dve", lambda e: e.tensor_scalar(Qm[0][:, hh, :].bitcast(F32R), eS[:, hh, :], col(nbt, hh), None, ALU.mult), reads=[tsm, teS], writes=[tQ[0]])

                    def ftn(e):
                        e.transpose(pB[:, sl(0)], Qm[0][:, 0, :], ident)
                        return e.transpose(pB[:, sl(1)], Qm[0][:, 1, :], ident)
                    kb.op("pe", ftn, reads=[tQ[0], self.tc], writes=[tB])
                    kb.op("act", lambda e: e.copy(PY[0][:, :, 0:128].bitcast(F32R), h2(pB[:, 0:256])), reads=[tB], writes=[tPY[0]])
                    kb.op("dve", lambda e: e.tensor_tensor(PY[0][:, :, 128:256].bitcast(F32R), h2(pB[:, 0:256]), ID2[:], ALU.add), reads=[tB, tsm], writes=[tPY[0]])
                    for k in range(7):
                        a, b = k % 2, (k + 1) % 2

                        def fa_(e, k=k, a=a):
                            ins = None
                            for hh in range(2):
                                o = hh * 256
                                q_ = Qm[a][:, hh, :].bitcast(F32R)
                                if k == 0:
                                    ins = e.matmul(pA[:, o:o + 128], q_, PY[a][:, hh, 0:128].bitcast(F32R), start=True, stop=True)
                                elif k < 6:
                                    ins = e.matmul(pA[:, o:o + 256], q_, PY[a][:, hh, 0:256].bitcast(F32R), start=True, stop=True)
                                else:
                                    ins = e.matmul(pA[:, o + 128:o + 256], q_, PY[a][:, hh, 128:256].bitcast(F32R), start=True, stop=True)
                            return ins
                        kb.op("pe", fa_, reads=[tQ[a], tPY[a]], writes=[tA])
                        if k < 6:
                            def fb_(e, a=a):
                                e.matmul(pB[:, sl(0)], PY[a][:, 0, 0:128].bitcast(F32R), Qm[a][:, 0, :].bitcast(F32R), start=True, stop=True)
                                return e.matmul(pB[:, sl(1)], PY[a][:, 1, 0:128].bitcast(F32R), Qm[a][:, 1, :].bitcast(F32R), start=True, stop=True)
                            kb.op("pe", fb_, reads=[tQ[a], tPY[a]], writes=[tB])
                            kb.op("act", lambda e: e.copy(PY[b][:, :, 0:128].bitcast(F32R), pA3[:, :, 0:128]), reads=[tA], writes=[tPY[b]])
                            kb.op("act", lambda e: e.copy(Qm[b][:].bitcast(F32R), h2(pB[:, 0:256])), reads=[tB], writes=[tQ[b]])
                            if k == 0:
                                kb.op("dve", lambda e: e.tensor_copy(PY[b][:, :, 128:256].bitcast(F32R), PY[a][:, :, 128:256]), reads=[tPY[a]], writes=[tPY[b]])
                            else:
                                kb.op("dve", lambda e: e.tensor_tensor(PY[b][:, :, 128:256].bitcast(F32R), PY[a][:, :, 128:256], pA3[:, :, 128:256], ALU.add),
                                      reads=[tPY[a], tA], writes=[tPY[b]])
                        else:
                            kb.op("dve", lambda e: e.tensor_tensor(TT[:], PY[a][:, :, 128:256], pA3[:, :, 128:256], ALU.add),
                                  reads=[tPY[a], tA], writes=[tTT])

                    def fkq(e):
                        e.matmul(pG[:, sl(2)], KT2[:, 0, cols], QT2[:, 0, cols], start=True, stop=True)
                        return e.matmul(pG[:, sl(3)], KT2[:, 1, cols], QT2[:, 1, cols], start=True, stop=True)
                    kb.op("pe", fkq, reads=[tld[0], tld[1]], writes=[tG])
                    kb.op("dve", lambda e: e.tensor_tensor(AT[:], h2(pG[:, 256:512]), eT[:], ALU.mult), reads=[tG, teT], writes=[tAT])

                    def fuw(e):
                        ins = None
                        for hh in range(2):
                            e.matmul(pU[:, sl(hh)], TT[:, hh, :], vb_[:, hh, :], start=True, stop=True)
                            ins = e.matmul(pU[:, sl(2 + hh)], kbg[:, hh, :], TT[:, hh, :], start=True, stop=True)
                        return ins
                    kb.op("pe", fuw, reads=[tTT, tvb, tkbg], writes=[tU])
                    kb.op("act", lambda e: e.copy(u_[:], h2(pU[:, 0:256])), reads=[tU], writes=[tu])
                    kb.op("act", lambda e: e.copy(wT[:], h2(pU[:, 256:512])), reads=[tU], writes=[twT])
                    kb.op("dve", lambda e: e.tensor_tensor(qdT[:], QT2[:, :, cols], eg[:], ALU.mult), reads=[tld[1], teg], writes=[tqdT])

                    def fv(e):
                        e.matmul(pV[:, sl(0)], wT[:, 0, :], Sbf[:, 0, :], start=True, stop=True)
                        return e.matmul(pV[:, sl(1)], wT[:, 1, :], Sbf[:, 1, :], start=True, stop=True)
                    kb.op("pe", fv, reads=[twT, tS], writes=[tV])
                    kb.op("dve", lambda e: e.tensor_tensor(vnew[:], u_[:], h2(pV[:, 0:256]), ALU.subtract), reads=[tu, tV], writes=[tvn])

                    def fo(e):
                        ins = None
                        for hh in range(2):
                            e.matmul(pO[:, sl(hh)], Sbf[:, hh, :], qdT[:, hh, :], start=True, stop=False)
                            ins = e.matmul(pO[:, sl(hh)], vnew[:, hh, :], AT[:, hh, :], start=False, stop=True)
                        return ins
                    kb.op("pe", fo, reads=[tS, tqdT, tvn, tAT], writes=[tO])

                    def fs(e):
                        e.matmul(pV[:, sl(2)], ktl[:, 0, :], vnew[:, 0, :], start=True, stop=True)
                        return e.matmul(pV[:, sl(3)], ktl[:, 1, :], vnew[:, 1, :], start=True, stop=True)
                    kb.op("pe", fs, reads=[tktl, tvn], writes=[tV])
                    for hh in range(2):
                        kb.op("dve", lambda e: e.tensor_scalar(Sst[:, hh, :], Sst[:, hh, :], col(sda, hh), None, ALU.mult), reads=[tS, tsm], writes=[tS])
                    kb.op("dve", lambda e: e.tensor_tensor(Sst[:], Sst[:], h2(pV[:, 256:512]), ALU.add), reads=[tS, tV], writes=[tS])
                    kb.op("act", lambda e: e.copy(Sbf[:], Sst[:]), reads=[tS], writes=[tS])
                    kb.op("act", lambda e: e.activation(sq[:], h2(pO[:, 0:256]), AF.Square), reads=[tO], writes=[tsq])

                    def fq(e):
                        e.matmul(pO[:, sl(2)], ones, sq[:, 0, :], start=True, stop=True)
                        return e.matmul(pO[:, sl(3)], ones, sq[:, 1, :], start=True, stop=True)
                    kb.op("pe", fq, reads=[tsq, self.tc], writes=[tO])
                    kb.op("act", lambda e: e.activation(rinv[:], h2(pO[:, 256:512]), AF.Sqrt, bias=self.epsb[:, 1:2], scale=1.0), reads=[tO, self.tc], writes=[tri])
                    kb.op("dve", lambda e: e.reciprocal(rinv[:], rinv[:]), reads=[tri], writes=[tri])
                    kb.op("dve", lambda e: e.tensor_tensor(y1[:], h2(pO[:, 0:256]), rinv[:], ALU.mult), reads=[tO, tri], writes=[ty1])
                    kb.op("dve", lambda e: e.scalar_tensor_tensor(yb2[:, :, cols], y1[:], nwc[:, 0:1], ZT2[:, :, cols], ALU.mult, ALU.mult),
                          reads=[ty1, tsm, tld[3]], writes=[tyb])
                kb.dma("sp", self.YBT.ap()[h0:h0 + 2].rearrange("h p t -> p h t"), yb2[:], reads=[tyb])
            kb.barrier()

    def layernorm(self, pre, tpre, g, b, tgb, out, tout, st, mv, tst):
        kb = self.kb

        def f(e):
            e.bn_stats(st[:, 0:6], pre[:, 0:512])
            return e.bn_stats(st[:, 6:12], pre[:, 512:1024])
        kb.op("dve", f, reads=[tpre], writes=[tst])
        kb.op("dve", lambda e: e.bn_aggr(mv[:, 0:2], st[:, 0:12]), reads=[tst], writes=[tst])
        kb.op("act", lambda e: e.activation(mv[:, 2:3], mv[:, 1:2], AF.Sqrt, bias=self.epsb[:, 3:4]), reads=[tst, self.tc], writes=[tst])
        kb.op("dve", lambda e: e.reciprocal(mv[:, 2:3], mv[:, 2:3]), reads=[tst], writes=[tst])
        kb.op("dve", lambda e: e.tensor_scalar(out, pre, mv[:, 0:1], mv[:, 2:3], ALU.subtract, ALU.mult), reads=[tpre, tst], writes=[tout])
        kb.op("dve", lambda e: e.tensor_tensor(out, out, g, ALU.mult), reads=[tgb], writes=[tout])
        kb.op("dve", lambda e: e.tensor_tensor(out, out, b, ALU.add), reads=[tgb], writes=[tout])

    def phase4(self, L, s, xT, txT, gates, tgates):
        kb = self.kb
        c32 = self.c32
        xres_src = self.x_in.ap()[s * T:(s + 1) * T, :] if L == 0 else self.X2.ap()
        moe = (L == 1)
        with ExitStack() as es:
            Wa = self.sb(es, "p4wa", [128, 4, D], BF16)
            Wb = self.sb(es, "p4wb", [128, 8, D], BF16)
            Wo = self.sb(es, "p4wo", [128, 8, D], BF16)
            tW = Tok()
            stage = self.sb(es, "p4stg", [128, 8, 512], F32)
            tstage = Tok()
            for hf in range(2):
                hs = slice(hf * 512, (hf + 1) * 512)
                self.wload(stage[:, 0:4, :], tstage, Wa[:, :, hs], tW, self.w_a.ap()[L][:, hs].rearrange("(k p) n -> p k n", p=128), q="sp")
                self.wload(stage[:], tstage, Wb[:, :, hs], tW, self.w_b.ap()[L][:, hs].rearrange("(k p) n -> p k n", p=128), q="sp")
                self.wload(stage[:], tstage, Wo[:, :, hs], tW, self.w_o.ap()[L][:, hs].rearrange("(k p) n -> p k n", p=128), q="sp")
            lng = self.sb(es, "p4lng", [128, D], F32)
            lnb = self.sb(es, "p4lnb", [128, D], F32)
            tgb = Tok()
            kb.dma("sp", lng[:], self.bc_ap(self.ln1_g, L * D, D), writes=[tgb])
            kb.dma("sp", lnb[:], self.bc_ap(self.ln1_b, L * D, D), writes=[tgb])
            if moe:
                Wr = self.sb(es, "p4wr", [128, 8, NE], F32)
                kb.dma("sp", Wr[:], self.router.ap()[0].rearrange("(k p) n -> p k n", p=128), writes=[tgb])
                x32 = self.sb(es, "p4x32", [128, 8, 128], F32)
                tx32 = Tok()
                rt = self.sb(es, "p4rt", [128, 64], F32)
                trt = Tok()
            ya = self.sb(es, "p4ya", [128, 4, 512], BF16)
            yb = self.sb(es, "p4yb", [128, 8, 512], BF16)
            gt = self.sb(es, "p4gt", [128, 16, 512], BF16)
            xr = self.sb(es, "p4xr", [128, 4, D], F32)
            tya, tyb, tgt, txr = toks(4)
            mT = self.sb(es, "p4mT", [128, 8, 512], BF16)
            tmT = Tok()
            m1 = [self.sb(es, f"p4m1{i}", [128, 512], F32) for i in range(1)] * 2
            m2 = [self.sb(es, f"p4m2{i}", [128, 512], F32) for i in range(1)] * 2
            tm1, tm2 = toks(1) * 2, toks(1) * 2
            pre = [self.sb(es, f"p4pre{i}", [128, D], F32) for i in range(1)] * 2
            x1t = [self.sb(es, f"p4x1{i}", [128, D], F32) for i in range(2)]
            tpre, tx1 = toks(1) * 2, toks(2)
            st = self.sb(es, "p4st", [128, 12], F32)
            mv = self.sb(es, "p4mv", [128, 4], F32)
            tst = Tok()
            psA = [self.ps(es, f"p4pA{i}") for i in range(2)]
            psB = [self.ps(es, f"p4pB{i}") for i in range(2)]
            psO = [self.ps(es, f"p4pO{i}") for i in range(2)]
            psT = [self.ps(es, f"p4pT{i}") for i in range(2)]
            tpA, tpB, tpO, tpT = toks(2), toks(2), toks(2), toks(2)
            no = 0
            for tt in range(T // 512):
                cs = slice(tt * 512, (tt + 1) * 512)
                kb.dma("sp", ya[:], self.YAT.ap()[:, :, cs].rearrange("k p t -> p k t"), writes=[tya])
                kb.dma("sp", yb[:], self.YBT.ap()[:, :, cs].rearrange("k p t -> p k t"), writes=[tyb])
                kb.dma("sp", gt[:], self.GT.ap()[:, :, cs].rearrange("k p t -> p k t"), writes=[tgt])
                kb.dma("sp", xr[:], xres_src[tt * 512:(tt + 1) * 512, :].rearrange("(j p) d -> p j d", p=128), writes=[txr])
                kb.op("act", lambda e: e.mul(xr[:], xr[:], ALPHA), reads=[txr], writes=[txr])
                for dc in range(8):
                    i = dc % 2
                    ds_ = slice(dc * 128, (dc + 1) * 128)

                    def fa(e, i=i, ds_=ds_):
                        ins = None
                        for k in range(4):
                            ins = e.matmul(psA[i][:, :], Wa[:, k, ds_], ya[:, k, :], start=(k == 0), stop=(k == 3))
                        return ins

                    def fb(e, i=i, ds_=ds_):
                        ins = None
                        for k in range(8):
                            ins = e.matmul(psB[i][:, :], Wb[:, k, ds_], yb[:, k, :], start=(k == 0), stop=(k == 7))
                        return ins
                    kb.op("pe", fa, reads=[tW, tya], writes=[tpA[i]])
                    kb.op("pe", fb, reads=[tW, tyb], writes=[tpB[i]])
                    kb.op("dve", lambda e, i=i, dc=dc: e.tensor_tensor(m1[i][:], psA[i][:, :], gt[:, dc, :], ALU.mult), reads=[tpA[i], tgt], writes=[tm1[i]])
                    kb.op("dve", lambda e, i=i, dc=dc: e.tensor_tensor(m2[i][:], psB[i][:, :], gt[:, 8 + dc, :], ALU.mult), reads=[tpB[i], tgt], writes=[tm2[i]])
                    kb.op("pool", lambda e, i=i, dc=dc: e.tensor_tensor(mT[:, dc, :], m1[i][:], m2[i][:], ALU.add), reads=[tm1[i], tm2[i]], writes=[tmT])
                for sub in range(4):
                    b = no % 2
                    no += 1
                    tok0 = tt * 512 + sub * 128
                    for hf in range(2):
                        hs = slice(hf * 512, (hf + 1) * 512)

                        def fo(e, hf=hf, hs=hs, sub=sub):
                            ins = None
                            for k in range(8):
                                ins = e.matmul(psO[hf][:, :], mT[:, k, sub * 128:(sub + 1) * 128], Wo[:, k, hs], start=(k == 0), stop=(k == 7))
                            return ins
                        kb.op("pe", fo, reads=[tW, tmT], writes=[tpO[hf]])
                        kb.op("dve", lambda e, hf=hf, hs=hs, b=b, sub=sub: e.tensor_tensor(pre[b][:, hs], psO[hf][:, :], xr[:, sub, hs], ALU.add),
                              reads=[tpO[hf], txr], writes=[tpre[b]])
                    self.layernorm(pre[b][:], tpre[b], lng[:], lnb[:], tgb, x1t[b][:], tx1[b], st, mv, tst)
                    kb.dma("sp", self.X1.ap()[tok0:tok0 + 128, :], x1t[b][:], reads=[tx1[b]])
                    if moe:
                        self.transpose_rows(None, x1t[b], tx1[b], xT, txT, tok0, psT, tpT, 0, x32=x32, tx32=tx32)
                        self.router_gates(x32, tx32, Wr, tgb, rt, trt, psA[0], tpA[0], gates[:, tok0 // 128, :], tgates)
                    else:
                        self.transpose_rows(None, x1t[b], tx1[b], xT, txT, tok0, psT, tpT, 0)
            kb.barrier()

    def router_gates(self, x32, tx32, Wr, tWr, rt, trt, ps, tps, gout, tgout):
        kb = self.kb

        def f(e):
            ins = None
            for k in range(8):
                ins = e.matmul(ps[:, 0:NE], x32[:, k, :], Wr[:, k, :], start=(k == 0), stop=(k == 7))
            return ins
        kb.op("pe", f, reads=[tx32, tWr], writes=[tps])
        lg, eq1, lg2, eq2, g1 = (rt[:, i * 8:(i + 1) * 8] for i in range(5))
        m1, m2, d_, w1, w2 = (rt[:, 40 + i:41 + i] for i in range(5))
        kb.op("act", lambda e: e.copy(lg, ps[:, 0:NE]), reads=[tps], writes=[trt])
        kb.op("dve", lambda e: e.reduce_max(m1, lg, AX.X), reads=[trt], writes=[trt])
        kb.op("dve", lambda e: e.tensor_scalar(eq1, lg, m1, None, ALU.is_equal), reads=[trt], writes=[trt])
        kb.op("dve", lambda e: e.scalar_tensor_tensor(lg2, eq1, -1e30, lg, ALU.mult, ALU.add), reads=[trt], writes=[trt])
        kb.op("dve", lambda e: e.reduce_max(m2, lg2, AX.X), reads=[trt], writes=[trt])
        kb.op("dve", lambda e: e.tensor_scalar(eq2, lg2, m2, None, ALU.is_equal), reads=[trt], writes=[trt])
        kb.op("dve", lambda e: e.tensor_tensor(d_, m2, m1, ALU.subtract), reads=[trt], writes=[trt])
        kb.op("act", lambda e: e.activation(d_, d_, AF.Exp), reads=[trt], writes=[trt])
        kb.op("dve", lambda e: e.tensor_scalar(w1, d_, 1.0, None, ALU.add), reads=[trt], writes=[trt])
        kb.op("dve", lambda e: e.reciprocal(w1, w1), reads=[trt], writes=[trt])
        kb.op("dve", lambda e: e.tensor_tensor(w2, d_, w1, ALU.mult), reads=[trt], writes=[trt])
        kb.op("dve", lambda e: e.tensor_scalar(g1, eq1, w1, None, ALU.mult), reads=[trt], writes=[trt])
        kb.op("dve", lambda e: e.scalar_tensor_tensor(gout, eq2, w2, g1, ALU.mult, ALU.add), reads=[trt], writes=[tgout])

    def phase5(self, L, s, xT, txT, gates, tgates):
        kb = self.kb
        c32 = self.c32
        moe = (L == 1)
        ne = NE if moe else 1
        dff = DEX if moe else DFF
        GW = 256
        ngr = dff // GW
        TS = 2048
        last = (L == 1)

        def wsrc(which, e_, g_):
            c0 = g_ * GW
            if moe:
                base = {"g": self.moe_g, "u": self.moe_u, "d": self.moe_d}[which].ap()[0][e_]
            else:
                base = {"g": self.ffn_g, "u": self.ffn_u, "d": self.ffn_d}[which].ap()[0]
            if which == "d":
                return base[c0:c0 + GW, :].rearrange("(k p) n -> p k n", p=128)
            return base[:, c0:c0 + GW].rearrange("(k p) n -> p k n", p=128)
        with ExitStack() as es:
            yacc = self.sb(es, "p5acc", [128, TS // 128, D], F32)
            taccs = toks(2 * TS // 128)
            stg_g = self.sb(es, "p5sg", [128, 8, GW], F32)
            stg_u = self.sb(es, "p5su", [128, 8, GW], F32)
            stg_d = self.sb(es, "p5sd", [128, GW // 128, D], F32)
            tsg, tsu, tsd = toks(3)
            Wg = [self.sb(es, f"p5wg{i}", [128, 8, GW], BF16) for i in range(2)]
            Wu = [self.sb(es, f"p5wu{i}", [128, 8, GW], BF16) for i in range(2)]
            Wd = [self.sb(es, f"p5wd{i}", [128, GW // 128, D], BF16) for i in range(2)]
            tWg, tWu, tWd = toks(2), toks(2), toks(2)
            hT = [self.sb(es, f"p5h{i}", [128, GW // 128, 512], BF16) for i in range(2)]
            thT = toks(2)
            sg = [self.sb(es, f"p5s{i}", [128, 512], BF16) for i in range(2)]
            tsg_ = toks(2)
            tmp = [self.sb(es, f"p5t{i}", [128, 512], F32) for i in range(2)]
            ttmp = toks(2)
            lng = self.sb(es, "p5lng", [128, D], F32)
            lnb = self.sb(es, "p5lnb", [128, D], F32)
            tgb = Tok()
            kb.dma("sp", lng[:], self.bc_ap(self.ln2_g, L * D, D), writes=[tgb])
            kb.dma("sp", lnb[:], self.bc_ap(self.ln2_b, L * D, D), writes=[tgb])
            x1 = [self.sb(es, f"p5x1{i}", [128, D], F32) for i in range(1)] * 2
            tx1 = toks(1) * 2
            st = self.sb(es, "p5st", [128, 12], F32)
            mv = self.sb(es, "p5mv", [128, 4], F32)
            tst = Tok()
            psG = [self.ps(es, f"p5pG{i}") for i in range(2)]
            psU = [self.ps(es, f"p5pU{i}") for i in range(2)]
            psY = [self.ps(es, f"p5pY{i}") for i in range(4)]
            tpG, tpU, tpY = toks(2), toks(2), toks(4)
            work = [(e_, g_) for e_ in range(ne) for g_ in range(ngr)]

            def w_dma(wi):
                e_, g_ = work[wi]
                kb.dma("sp", stg_g[:], wsrc("g", e_, g_), writes=[tsg])
                kb.dma("sp", stg_u[:], wsrc("u", e_, g_), writes=[tsu])
                kb.dma("sp", stg_d[:], wsrc("d", e_, g_), writes=[tsd])

            def w_cast(wi):
                b = wi % 2
                kb.op("act", lambda e: e.copy(Wg[b][:], stg_g[:]), reads=[tsg], writes=[tWg[b]])
                kb.op("act", lambda e: e.copy(Wu[b][:], stg_u[:]), reads=[tsu], writes=[tWu[b]])
                kb.op("act", lambda e: e.copy(Wd[b][:], stg_d[:]), reads=[tsd], writes=[tWd[b]])

            def GU(st_i, wi, t4, hb):
                b = wi % 2
                c0 = st_i * TS + t4 * 512
                for fc in range(GW // 128):
                    i = self.ngc % 2
                    self.ngc += 1
                    fs = slice(fc * 128, (fc + 1) * 128)

                    def fg(e, i=i, fs=fs):
                        ins = None
                        for k in range(8):
                            ins = e.matmul(psG[i][:, :], Wg[b][:, k, fs], xT[:, k, c0:c0 + 512], start=(k == 0), stop=(k == 7))
                        return ins

                    def fu(e, i=i, fs=fs):
                        ins = None
                        for k in range(8):
                            ins = e.matmul(psU[i][:, :], Wu[b][:, k, fs], xT[:, k, c0:c0 + 512], start=(k == 0), stop=(k == 7))
                        return ins
                    kb.op("pe", fg, reads=[tWg[b], txT], writes=[tpG[i]])
                    kb.op("pe", fu, reads=[tWu[b], txT], writes=[tpU[i]])
                    kb.op("act", lambda e, i=i: e.activation(sg[i][:], psG[i][:, :], AF.Silu), reads=[tpG[i]], writes=[tsg_[i]])
                    kb.op("dve", lambda e, i=i, fc=fc: e.tensor_tensor(hT[hb][:, fc, :], sg[i][:], psU[i][:, :], ALU.mult),
                          reads=[tsg_[i], tpU[i]], writes=[thT[hb]])

            def YD(st_i, wi, t4, hb):
                b = wi % 2
                e_, g_ = work[wi]
                c0 = st_i * TS + t4 * 512
                for sub in range(4):
                    tsub = t4 * 4 + sub
                    gtile = (c0 + sub * 128) // 128
                    for hf in range(2):
                        j = self.nyc % 4
                        tb = self.nyc % 2
                        self.nyc += 1
                        hs = slice(hf * 512, (hf + 1) * 512)

                        def fy(e, j=j, sub=sub, hs=hs):
                            ins = None
                            nk = GW // 128
                            for k in range(nk):
                                ins = e.matmul(psY[j][:, :], hT[hb][:, k, sub * 128:(sub + 1) * 128], Wd[b][:, k, hs],
                                               start=(k == 0), stop=(k == nk - 1))
                            return ins
                        kb.op("pe", fy, reads=[thT[hb], tWd[b]], writes=[tpY[j]])
                        if moe:
                            kb.op("act", lambda e, j=j, tb=tb: e.activation(tmp[tb][:], psY[j][:, :], AF.Copy, scale=gates[:, gtile, e_:e_ + 1]),
                                  reads=[tpY[j], tgates], writes=[ttmp[tb]])
                        else:
                            kb.op("act", lambda e, j=j, tb=tb: e.copy(tmp[tb][:], psY[j][:, :]), reads=[tpY[j]], writes=[ttmp[tb]])
                        tk = taccs[tsub * 2 + hf]
                        kb.op("dve", lambda e, tb=tb, tsub=tsub, hs=hs: e.tensor_tensor(yacc[:, tsub, hs], yacc[:, tsub, hs], tmp[tb][:], ALU.add),
                              reads=[ttmp[tb], tk], writes=[tk])
            self.ngc, self.nyc = 0, 0
            NT4 = TS // 512
            for st_i in range(T // TS):
                kb.op("pool", lambda e: e.memset(yacc[:], 0.0), writes=taccs)
                w_dma(0)
                w_cast(0)
                units = [(wi, t4) for wi in range(len(work)) for t4 in range(NT4)]
                GU(st_i, 0, 0, 0)
                for ui, (wi, t4) in enumerate(units):
                    if t4 == 0 and wi + 1 < len(work):
                        w_dma(wi + 1)
                    if t4 == 2 and wi + 1 < len(work):
                        w_cast(wi + 1)
                    if ui + 1 < len(units):
                        GU(st_i, units[ui + 1][0], units[ui + 1][1], (ui + 1) % 2)
                    YD(st_i, wi, t4, ui % 2)
                for tsub in range(TS // 128):
                    b = tsub % 2
                    tok0 = st_i * TS + tsub * 128
                    kb.dma("sp", x1[b][:], self.X1.ap()[tok0:tok0 + 128, :], writes=[tx1[b]])
                    tacc = taccs[tsub * 2]
                    kb.op("dve", lambda e, b=b, tsub=tsub: e.scalar_tensor_tensor(yacc[:, tsub, :], x1[b][:], ALPHA, yacc[:, tsub, :], ALU.mult, ALU.add),
                          reads=[tx1[b], taccs[tsub * 2], taccs[tsub * 2 + 1]], writes=[tacc])
                    self.layernorm(yacc[:, tsub, :], tacc, lng[:], lnb[:], tgb, x1[b][:], tx1[b], st, mv, tst)
                    if last:
                        kb.dma("sp", self.y_out.ap()[s * T + tok0:s * T + tok0 + 128, :], x1[b][:], reads=[tx1[b]])
                    else:
                        kb.dma("sp", self.X2.ap()[tok0:tok0 + 128, :], x1[b][:], reads=[tx1[b]])
                        self.transpose_rows(None, x1[b], tx1[b], xT, txT, tok0, psG, tpG, 0)
            kb.barrier()

    def build(self):
        kb = self.kb
        with ExitStack() as es:
            self.load_consts(es)
            self.epsb = self.sb(es, "epsb", [128, 4], F32)
            kb.op("dve", lambda e: e.memset(self.epsb[:, 0:1], RMS_EPS), writes=[self.tc])
            kb.op("dve", lambda e: e.memset(self.epsb[:, 1:2], RMS_EPS * 128), writes=[self.tc])
            kb.op("dve", lambda e: e.memset(self.epsb[:, 2:3], 1.0), writes=[self.tc])
            kb.op("dve", lambda e: e.memset(self.epsb[:, 3:4], LN_EPS), writes=[self.tc])
            xT = self.sb(es, "xT", [128, 8, T], BF16)
            txT = Tok()
            gates = self.sb(es, "gates", [128, T // 128, NE], F32)
            tgates = Tok()
            kb.barrier()
            only = os.environ.get("ONLY")
            for s in range(self.nseq):
                if only:
                    ph, LL = only.split(":")
                    LL = int(LL)
                    kb.op("dve", lambda e: e.memset(xT[:], 0.0), writes=[txT])
                    kb.op("dve", lambda e: e.memset(gates[:], 0.0), writes=[tgates])
                    kb.barrier()
                    {"p1": lambda: self.phase1(LL, s, xT, txT), "p2": lambda: self.phase2(s, xT), "p3": lambda: self.phase3(LL, s, xT),
                     "p4": lambda: self.phase4(LL, s, xT, txT, gates, tgates), "p5": lambda: self.phase5(LL, s, xT, txT, gates, tgates)}[ph]()
                    continue
                self.phase0(s, xT, txT)
                if "XTd" in self.dbg:
                    xtd = self.scr("XTd", [128, 8, T], BF16)
                    kb.dma("sp", xtd.ap()[:, :, :], xT[:], reads=[txT])
                if self.stop_after == "p0":
                    break
                self.rope_tables(s)
                if self.stop_after == "rope":
                    break
                for L in range(2):
                    self.phase1(L, s, xT, txT)
                    if self.stop_after == "p1":
                        break
                    self.phase2(s, xT)
                    if self.stop_after == "p2":
                        break
                    self.phase3(L, s, xT)
                    if self.stop_after == "p3":
                        break
                    self.phase4(L, s, xT, txT, gates, tgates)
                    if self.stop_after == "p4":
                        break
                    self.phase5(L, s, xT, txT, gates, tgates)
                    if self.stop_after == f"p5_{L}":
                        break
                if self.stop_after:
                    break
            kb.barrier()
        return self.nc


_W_KEYS = ("w_in", "a_log", "dt_bias", "dn_norm_w", "w_branch_a", "w_branch_b", "w_out", "ln1_g", "ln1_b", "ln2_g", "ln2_b",
           "ffn_w_gate", "ffn_w_up", "ffn_w_down", "router_w", "moe_w_gate", "moe_w_up", "moe_w_down")


def core_maps(inputs, seq_groups):
    cst = make_consts()
    conv = np.ascontiguousarray(np.asarray(inputs["conv_w"], np.float32).reshape(2, 4, 24, 128).transpose(0, 3, 2, 1))
    ws = {k: np.ascontiguousarray(np.asarray(inputs[k], np.float32)) for k in _W_KEYS}
    x = np.asarray(inputs["x"], np.float32)
    pos = np.asarray(inputs["positions"], np.int32)
    maps = []
    for seqs in seq_groups:
        m = {"x": np.ascontiguousarray(x[seqs].reshape(-1, D)), "pos": np.ascontiguousarray(pos[seqs]), "cst": cst, "conv_w": conv}
        m.update(ws)
        maps.append(m)
    return maps


def kernel(**inputs):
    x = np.asarray(inputs["x"])
    B = x.shape[0]
    n_cores = 8
    per = B // n_cores
    groups = [list(range(c * per, (c + 1) * per)) for c in range(n_cores)]
    prog = Prog(per)
    nc = prog.build()
    res = run_bass_kernel_spmd(nc, core_maps(inputs, groups), core_ids=list(range(n_cores)))
    out = np.concatenate([np.asarray(r["y"], np.float32).reshape(per, T, D) for r in res.results], axis=0)
    return out
```
